# Optimizing a Trainium2 kernel written in Bass

```python
import math
import jax
import jax.numpy as jnp
from jax import lax
import numpy as np

D_MODEL = 1024
BATCH = 4
SEQ = 8192
DEPTH = 1

GRID_W = 64
CTX_LEN = 256
S5_WIDTH = 256
S5_GROUP = 16
S5_GROUPS = S5_WIDTH // S5_GROUP
S5_STATE = 64
DT_MIN = 1e-3
DT_MAX = 1e-1
HG_HEADS = 6
HG_DK = 128
HG_DV = 128
HG_WIDTH = HG_HEADS * HG_DK
HG_CHUNK = 64
N_BRANCH = 2
IN_WIDTH = S5_WIDTH + 5 * HG_WIDTH + N_BRANCH * D_MODEL
N_EXPERTS = 64
ROUTE_GROUPS = 8
TOPK_GROUPS = 4
TOP_K = 8
EXPERT_HIDDEN = 256
SHARED_HIDDEN = 256
ROUTED_SCALE = 2.5
MOE_BLOCK = 128
EPS = 1e-6

kernel_name = 'hybrid_s5_hgrn2_moe_prefix_block'


def rms_norm(t, g):
    tf = t.astype(jnp.float32)
    y = tf * lax.rsqrt(jnp.mean(tf * tf, axis=-1, keepdims=True) + EPS)
    return (y * g.astype(jnp.float32)).astype(t.dtype)


def to_colmajor(t, rows):
    b, l, f = t.shape
    return t.reshape(b, rows, GRID_W, f).transpose(0, 2, 1, 3).reshape(b, l, f)


def from_colmajor(t, rows):
    b, l, f = t.shape
    return t.reshape(b, GRID_W, rows, f).transpose(0, 2, 1, 3).reshape(b, l, f)


def split_in(z):
    sizes = (S5_WIDTH,) + (HG_WIDTH,) * 5 + (D_MODEL,) * N_BRANCH
    idx = [int(v) for v in np.cumsum(sizes)[:-1]]
    return jnp.split(z, idx, axis=-1)


def s5_discretise(lam_re, lam_im, log_dt, b_re, b_im):
    lam = lax.complex(jnp.minimum(lam_re.astype(jnp.float32), -1e-4), lam_im.astype(jnp.float32))
    dt = jnp.exp(log_dt.astype(jnp.float32))[:, None]
    lam_dt = lam * dt
    b = lax.complex(b_re.astype(jnp.float32), b_im.astype(jnp.float32))
    b_bar = ((jnp.exp(lam_dt) - 1.0) / lam)[..., None] * b
    return lam_dt, b_bar


def s5_prepare(lp):
    lam_f, bbar_f = s5_discretise(lp['s5_lam_re'][0], lp['s5_lam_im'][0], lp['s5_log_dt'][0],
                                  lp['s5_b_re'][0], lp['s5_b_im'][0])
    lam_b, bbar_b = s5_discretise(lp['s5_lam_re'][1], lp['s5_lam_im'][1], lp['s5_log_dt'][1],
                                  lp['s5_b_re'][1], lp['s5_b_im'][1])
    return {'lam_f': lam_f, 'bbar_f': bbar_f, 'lam_b': lam_b, 'bbar_b': bbar_b,
            'c': lax.complex(lp['s5_c_re'].astype(jnp.float32), lp['s5_c_im'].astype(jnp.float32)),
            'd': lp['s5_d'].astype(jnp.float32).reshape(S5_GROUPS, S5_GROUP)}


def s5_drive(u, b_bar):
    return jnp.einsum('blgc,gpc->blgp', u.astype(jnp.complex64), b_bar)


def _linear_recurrence_op(e1, e2):
    a1, b1 = e1
    a2, b2 = e2
    return a2 * a1, a2 * b1 + b2


def s5_scan(bu, lam_dt, h0, reverse):
    l = bu.shape[1]
    a = jnp.broadcast_to(jnp.exp(lam_dt), bu.shape)
    _, states = lax.associative_scan(_linear_recurrence_op, (a, bu), axis=1, reverse=reverse)
    pos = jnp.arange(l, dtype=jnp.float32)
    steps = (l - pos) if reverse else (pos + 1.0)
    return states + jnp.exp(steps[:, None, None] * lam_dt)[None] * h0[:, None]


def s5_final_state(bu, lam_dt, reverse):
    l = bu.shape[1]
    pos = jnp.arange(l, dtype=jnp.float32)
    steps = pos if reverse else (l - 1.0) - pos
    decay = jnp.exp(steps[:, None, None] * lam_dt)
    return jnp.einsum('lgp,blgp->bgp', decay, bu)


def hgrn_forget(zf, lb):
    f = lb + (1.0 - lb) * jax.nn.sigmoid(zf)
    return jnp.log(f), (1.0 - lb) * jax.nn.sigmoid(-zf)


def split_heads(t):
    b, l, _ = t.shape
    return t.reshape(b, l, HG_HEADS, -1).transpose(0, 2, 1, 3)


def merge_heads(t):
    b, h, l, d = t.shape
    return t.transpose(0, 2, 1, 3).reshape(b, l, h * d)


def gla_chunkwise(q, k, v, log_f, s0):
    b, h, l, dk = q.shape
    n = l // HG_CHUNK
    rs = lambda t: t.reshape(b, h, n, HG_CHUNK, t.shape[-1])
    q, k, v, log_f = rs(q), rs(k), rs(v), rs(log_f)
    cum = jnp.cumsum(log_f, axis=3)
    ref = cum[:, :, :, HG_CHUNK // 2:HG_CHUNK // 2 + 1]
    qi = q * jnp.exp(cum - ref)
    ki = k * jnp.exp(ref - cum)
    mask = jnp.tril(jnp.ones((HG_CHUNK, HG_CHUNK), dtype=bool))
    scores = jnp.where(mask, jnp.einsum('bhnti,bhnsi->bhnts', qi, ki), 0.0)
    o_intra = jnp.einsum('bhnts,bhnsv->bhntv', scores, v)
    total = cum[:, :, :, -1]
    q_in = q * jnp.exp(cum)
    kv = jnp.einsum('bhnsi,bhnsv->bhniv', k * jnp.exp(total[:, :, :, None] - cum), v)

    def step(s, inp):
        q_c, dec, kv_c = inp
        o = jnp.einsum('bhti,bhiv->bhtv', q_c, s)
        return dec[..., None] * s + kv_c, o

    _, o_inter = lax.scan(step, s0, (jnp.moveaxis(q_in, 2, 0), jnp.moveaxis(jnp.exp(total), 2, 0),
                                     jnp.moveaxis(kv, 2, 0)))
    o = o_intra + jnp.moveaxis(o_inter, 0, 2)
    return o.reshape(b, h, l, v.shape[-1])


def gla_final_state(k, v, log_f):
    cum = jnp.cumsum(log_f, axis=2)
    return jnp.einsum('bhli,bhlv->bhiv', k * jnp.exp(cum[:, :, -1:] - cum), v)


def context_states(zc, s5, lb):
    u, _, ff, fb, i, _, _, _ = split_in(zc)
    b, l, _ = zc.shape
    uc = u.astype(jnp.float32).reshape(b, l, S5_GROUPS, S5_GROUP)
    h_f = s5_final_state(s5_drive(uc, s5['bbar_f']), s5['lam_f'], reverse=False)
    h_b = s5_final_state(s5_drive(uc, s5['bbar_b']), s5['lam_b'], reverse=True)
    logf_f, k_f = hgrn_forget(ff.astype(jnp.float32), lb)
    logf_b, k_b = hgrn_forget(fb.astype(jnp.float32), lb)
    vh = split_heads(i.astype(jnp.float32))
    s_f = gla_final_state(split_heads(k_f), vh, split_heads(logf_f))
    flip = lambda t: jnp.flip(t, axis=2)
    s_b = gla_final_state(flip(split_heads(k_b)), flip(vh), flip(split_heads(logf_b)))
    return h_f, h_b, s_f, s_b


def token_mixer(z, states, s5, lp, lb, rows):
    h_f, h_b, s_f, s_b = states
    u, q, ff, fb, i, g_out, gate_a, gate_b = split_in(z)
    b, l, _ = z.shape
    uf = u.astype(jnp.float32).reshape(b, l, S5_GROUPS, S5_GROUP)
    xs = (s5_scan(s5_drive(uf, s5['bbar_f']), s5['lam_f'], h_f, reverse=False)
          + s5_scan(s5_drive(uf, s5['bbar_b']), s5['lam_b'], h_b, reverse=True))
    y = jnp.einsum('blgp,gcp->blgc', xs, s5['c']).real + s5['d'] * uf
    y = jax.nn.gelu(y.reshape(b, l, S5_WIDTH)).astype(z.dtype)
    y_a = y * jax.nn.sigmoid(y @ lp['s5_w_glu'])
    q, ff, fb, i = [t.astype(jnp.float32) for t in (q, ff, fb, i)]
    if rows is not None:
        q, ff, fb, i = [to_colmajor(t, rows) for t in (q, ff, fb, i)]
    logf_f, k_f = hgrn_forget(ff, lb)
    logf_b, k_b = hgrn_forget(fb, lb)
    qh, vh = split_heads(q), split_heads(i)
    flip = lambda t: jnp.flip(t, axis=2)
    o = gla_chunkwise(qh, split_heads(k_f), vh, split_heads(logf_f), s_f)
    o = o + flip(gla_chunkwise(flip(qh), flip(split_heads(k_b)), flip(vh), flip(split_heads(logf_b)), s_b))
    o = merge_heads(rms_norm(o, lp['hg_norm_g']))
    if rows is not None:
        o = from_colmajor(o, rows)
    y_b = o.astype(z.dtype) * jax.nn.silu(g_out)
    merged = jax.nn.sigmoid(gate_a) * (y_a @ lp['p_a']) + jax.nn.sigmoid(gate_b) * (y_b @ lp['p_b'])
    return merged @ lp['w_out']


def moe_ffn(h, lp):
    b, l, dm = h.shape
    xt = h.reshape(-1, dm)
    n_tok = xt.shape[0]
    scores = jax.nn.sigmoid((xt @ lp['moe_w_router']).astype(jnp.float32))
    biased = scores + lp['moe_b_router'].astype(jnp.float32)
    per_group = N_EXPERTS // ROUTE_GROUPS
    grp_score = lax.top_k(biased.reshape(n_tok, ROUTE_GROUPS, per_group), 2)[0].sum(-1)
    _, top_groups = lax.top_k(grp_score, TOPK_GROUPS)
    group_ok = jnp.any(top_groups[:, :, None] == jnp.arange(ROUTE_GROUPS)[None, None, :], axis=1)
    expert_ok = jnp.repeat(group_ok, per_group, axis=1)
    _, eidx = lax.top_k(jnp.where(expert_ok, biased, -jnp.inf), TOP_K)
    wts = jnp.take_along_axis(scores, eidx, axis=1)
    wts = wts / jnp.sum(wts, axis=-1, keepdims=True) * ROUTED_SCALE
    n_assign = n_tok * TOP_K
    e_flat = eidx.reshape(-1).astype(jnp.int32)
    tok_flat = jnp.arange(n_assign, dtype=jnp.int32) // TOP_K
    w_flat = wts.reshape(-1)
    order = jnp.argsort(e_flat)
    e_sorted = e_flat[order]
    counts = jnp.bincount(e_flat, length=N_EXPERTS)
    starts = jnp.cumsum(counts) - counts
    padded = (counts + MOE_BLOCK - 1) // MOE_BLOCK * MOE_BLOCK
    pends = jnp.cumsum(padded)
    pstarts = pends - padded
    dest = pstarts[e_sorted] + jnp.arange(n_assign, dtype=jnp.int32) - starts[e_sorted]
    n_blocks = (n_assign + N_EXPERTS * (MOE_BLOCK - 1) + MOE_BLOCK - 1) // MOE_BLOCK
    cap = n_blocks * MOE_BLOCK
    row_tok = jnp.full((cap,), n_tok, jnp.int32).at[dest].set(tok_flat[order])
    row_w = jnp.zeros((cap,), jnp.float32).at[dest].set(w_flat[order])
    block_e = jnp.minimum(jnp.searchsorted(pends, jnp.arange(n_blocks) * MOE_BLOCK, side='right'),
                          N_EXPERTS - 1).astype(jnp.int32)
    x_pad = jnp.concatenate([xt, jnp.zeros((1, dm), xt.dtype)], axis=0)
    w1, w3, w2 = lp['moe_w1'], lp['moe_w3'], lp['moe_w2']

    def expert_block(acc, blk):
        toks, wr, e = blk
        xb = x_pad[toks]
        hid = jax.nn.silu(xb @ w1[e]) * (xb @ w3[e])
        return acc.at[toks].add((hid @ w2[e]) * wr[:, None].astype(xb.dtype)), None

    acc, _ = lax.scan(expert_block, jnp.zeros((n_tok + 1, dm), xt.dtype),
                      (row_tok.reshape(n_blocks, MOE_BLOCK), row_w.reshape(n_blocks, MOE_BLOCK), block_e))
    shared = (jax.nn.silu(xt @ lp['moe_ws1']) * (xt @ lp['moe_ws3'])) @ lp['moe_ws2']
    return (acc[:n_tok] + shared).reshape(b, l, dm)


def layer_forward(x, ctx, c, c_ctx, lp, lb, rows, last):
    mod = (jax.nn.silu(c) @ lp['w_ada'] + lp['b_ada'])[:, None, :]
    mod_c = jax.nn.silu(c_ctx) @ lp['w_ada'] + lp['b_ada']
    sh1, sc1, g1, sh2, sc2, g2 = jnp.split(mod, 6, axis=-1)
    csh1, csc1, cg1, csh2, csc2, cg2 = jnp.split(mod_c, 6, axis=-1)
    s5 = s5_prepare(lp)
    h = rms_norm(x, lp['norm1_g']) * (1.0 + sc1) + sh1
    hc = rms_norm(ctx, lp['norm1_g']) * (1.0 + csc1) + csh1
    z = h @ lp['w_in']
    zc = hc @ lp['w_in']
    states = context_states(zc, s5, lb)
    x = x + g1 * token_mixer(z, states, s5, lp, lb, rows)
    x = x + g2 * moe_ffn(rms_norm(x, lp['norm2_g']) * (1.0 + sc2) + sh2, lp)
    if not last:
        b = ctx.shape[0]
        zero_states = (jnp.zeros((b, S5_GROUPS, S5_STATE), jnp.complex64),
                       jnp.zeros((b, S5_GROUPS, S5_STATE), jnp.complex64),
                       jnp.zeros((b, HG_HEADS, HG_DK, HG_DV), jnp.float32),
                       jnp.zeros((b, HG_HEADS, HG_DK, HG_DV), jnp.float32))
        ctx = ctx + cg1 * token_mixer(zc, zero_states, s5, lp, lb, None)
        ctx = ctx + cg2 * moe_ffn(rms_norm(ctx, lp['norm2_g']) * (1.0 + csc2) + csh2, lp)
    return x, ctx


def setup_inputs(seed: int = 0) -> dict:
    key = jax.random.key(seed)
    ks = iter(jax.random.split(key, 40))

    def nrm(shape, scale):
        return jax.random.normal(next(ks), shape, jnp.float32) * scale

    d = D_MODEL
    g, cc, p = S5_GROUPS, S5_GROUP, S5_STATE
    e, f = N_EXPERTS, EXPERT_HIDDEN
    n_idx = jnp.arange(p, dtype=jnp.float32)
    return {
        'x': nrm((BATCH, SEQ, d), 1.0),
        'c': nrm((BATCH, d), 1.0),
        'ctx': nrm((BATCH, CTX_LEN, d), 1.0),
        'c_ctx': nrm((d,), 1.0),
        'w_ada': nrm((DEPTH, d, 6 * d), 0.5 * d ** -0.5),
        'b_ada': nrm((DEPTH, 6 * d), 0.01),
        'norm1_g': 1.0 + nrm((DEPTH, d), 0.01),
        'norm2_g': 1.0 + nrm((DEPTH, d), 0.01),
        'w_in': nrm((DEPTH, d, IN_WIDTH), d ** -0.5),
        's5_lam_re': -0.5 + nrm((DEPTH, 2, g, p), 0.01),
        's5_lam_im': math.pi * n_idx + nrm((DEPTH, 2, g, p), 0.01),
        's5_log_dt': jax.random.uniform(next(ks), (DEPTH, 2, g), jnp.float32,
                                        math.log(DT_MIN), math.log(DT_MAX)),
        's5_b_re': nrm((DEPTH, 2, g, p, cc), (2 * cc) ** -0.5),
        's5_b_im': nrm((DEPTH, 2, g, p, cc), (2 * cc) ** -0.5),
        's5_c_re': nrm((DEPTH, g, cc, p), (2 * p) ** -0.5),
        's5_c_im': nrm((DEPTH, g, cc, p), (2 * p) ** -0.5),
        's5_d': nrm((DEPTH, S5_WIDTH), 1.0),
        's5_w_glu': nrm((DEPTH, S5_WIDTH, S5_WIDTH), S5_WIDTH ** -0.5),
        'hg_lb_logits': nrm((DEPTH + 1, HG_WIDTH), 0.1),
        'hg_norm_g': 1.0 + nrm((DEPTH, HG_DV), 0.01),
        'p_a': nrm((DEPTH, S5_WIDTH, d), S5_WIDTH ** -0.5),
        'p_b': nrm((DEPTH, HG_WIDTH, d), HG_WIDTH ** -0.5),
        'w_out': nrm((DEPTH, d, d), d ** -0.5),
        'moe_w_router': nrm((DEPTH, d, e), d ** -0.5),
        'moe_b_router': nrm((DEPTH, e), 0.01),
        'moe_w1': nrm((DEPTH, e, d, f), d ** -0.5),
        'moe_w3': nrm((DEPTH, e, d, f), d ** -0.5),
        'moe_w2': nrm((DEPTH, e, f, d), f ** -0.5),
        'moe_ws1': nrm((DEPTH, d, SHARED_HIDDEN), d ** -0.5),
        'moe_ws3': nrm((DEPTH, d, SHARED_HIDDEN), d ** -0.5),
        'moe_ws2': nrm((DEPTH, SHARED_HIDDEN, d), SHARED_HIDDEN ** -0.5),
        'final_norm_g': 1.0 + nrm((d,), 0.01),
    }


def reference(x, c, ctx, c_ctx, w_ada, b_ada, norm1_g, norm2_g, w_in, s5_lam_re, s5_lam_im,
              s5_log_dt, s5_b_re, s5_b_im, s5_c_re, s5_c_im, s5_d, s5_w_glu, hg_lb_logits,
              hg_norm_g, p_a, p_b, w_out, moe_w_router, moe_b_router, moe_w1, moe_w3, moe_w2,
              moe_ws1, moe_ws3, moe_ws2, final_norm_g):
    rows = x.shape[1] // GRID_W
    lb_all = jnp.cumsum(jax.nn.softmax(hg_lb_logits.astype(jnp.float32), axis=0), axis=0)
    for layer in range(DEPTH):
        lp = {'w_ada': w_ada[layer], 'b_ada': b_ada[layer], 'norm1_g': norm1_g[layer],
              'norm2_g': norm2_g[layer], 'w_in': w_in[layer],
              's5_lam_re': s5_lam_re[layer], 's5_lam_im': s5_lam_im[layer], 's5_log_dt': s5_log_dt[layer],
              's5_b_re': s5_b_re[layer], 's5_b_im': s5_b_im[layer], 's5_c_re': s5_c_re[layer],
              's5_c_im': s5_c_im[layer], 's5_d': s5_d[layer], 's5_w_glu': s5_w_glu[layer],
              'hg_norm_g': hg_norm_g[layer], 'p_a': p_a[layer], 'p_b': p_b[layer], 'w_out': w_out[layer],
              'moe_w_router': moe_w_router[layer], 'moe_b_router': moe_b_router[layer],
              'moe_w1': moe_w1[layer], 'moe_w3': moe_w3[layer], 'moe_w2': moe_w2[layer],
              'moe_ws1': moe_ws1[layer], 'moe_ws3': moe_ws3[layer], 'moe_ws2': moe_ws2[layer]}
        x, ctx = layer_forward(x, ctx, c, c_ctx, lp, lb_all[layer], rows, layer == DEPTH - 1)
    return rms_norm(x, final_norm_g)
```

```python
import numpy as np
from contextlib import ExitStack
import concourse.bass as bass
import concourse.mybir as mybir
from concourse.bass_utils import run_bass_kernel_spmd

F32 = mybir.dt.float32
BF16 = mybir.dt.bfloat16
I32 = mybir.dt.int32
AF = mybir.ActivationFunctionType
ALU = mybir.AluOpType
AX = mybir.AxisListType

PI = float(np.pi)
TWO_PI = float(2 * np.pi)
EPS = 1e-6
DEBUG = False
USE_CONV = False
SK = 2


class TT:
    def __init__(s, h):
        s.h = h
        s.w = None
        s.r = {}

    def __getitem__(s, k):
        return s.h[k]


class KB:
    def __init__(s, nc):
        s.nc = nc
        s.gstack = ExitStack()
        s.stack = s.gstack
        s.eng = {'pe': nc.tensor, 'dve': nc.vector, 'act': nc.scalar, 'pool': nc.gpsimd, 'sp': nc.sync}
        s.csem = {}
        s.ccnt = {}
        s.seen = {e: {} for e in s.eng}
        s.dsem = {}
        s.dpos = {}
        s.nsem = 0
        s.ninst = 0
        s.uid = 0
        s.bsem = None
        s.bcnt = 0

    def newsem(s):
        s.nsem += 1
        return s.gstack.enter_context(s.nc.semaphore("s%d" % s.nsem))

    def nm(s, name):
        s.uid += 1
        return "%s_%d" % (name, s.uid)

    def sb(s, name, shape, dt=F32):
        return TT(s.stack.enter_context(s.nc.sbuf_tensor(s.nm(name), list(shape), dt)))

    def ps(s, name, shape, dt=F32):
        return TT(s.stack.enter_context(s.nc.psum_tensor(s.nm(name), list(shape), dt)))

    def ring(s, name, n, shape, dt=F32):
        return [s.sb(name, shape, dt) for _ in range(n)]

    def views(s, t, n):
        return [TT(t.h) for _ in range(n)]

    def _wait(s, e, sem, val):
        key = id(sem)
        if s.seen[e].get(key, 0) >= val:
            return
        s.seen[e][key] = val
        s.eng[e].wait_ge(sem, val)
        s.ninst += 1

    def op(s, e, fn, reads=(), writes=(), dma=False):
        deps = []
        for t in reads:
            if t.w is not None:
                deps.append(t.w)
        for t in writes:
            if t.w is not None:
                deps.append(t.w)
            deps.extend(t.r.values())
        if dma:
            q = s.dsem.setdefault(e, [])
            if len(q) < 16:
                q.append([s.newsem(), 0])
                slot = q[-1]
            else:
                p = s.dpos.get(e, 0)
                slot = q[p]
                s.dpos[e] = (p + 1) % 16
            sem = slot[0]
            if slot[1] > 0:
                s._wait(e, sem, slot[1])
            slot[1] += 16
            val = slot[1]
            inc = 16
        else:
            if e not in s.csem or s.ccnt[e] >= 20000:
                s.csem[e] = s.newsem()
                s.ccnt[e] = 0
            sem = s.csem[e]
            s.ccnt[e] += 1
            val = s.ccnt[e]
            inc = 1
        for (dsem, dval, deng) in deps:
            if deng == e and e == 'pe' and not dma:
                continue
            s._wait(e, dsem, dval)
        ins = fn(s.eng[e])
        ins.then_inc(sem, inc)
        s.ninst += 1
        tag = (sem, val, None if dma else e)
        for t in writes:
            t.w = tag
            t.r = {}
        for t in reads:
            t.r[id(sem)] = tag
        return tag

    def outstanding(s):
        tags = []
        for e, sem in s.csem.items():
            tags.append((sem, s.ccnt[e]))
        for e, q in s.dsem.items():
            for slot in q:
                if slot[1] > 0:
                    tags.append((slot[0], slot[1]))
        return tags

    def barrier(s):
        if s.bsem is None:
            s.bsem = s.newsem()
        for (sem, val) in s.outstanding():
            s._wait('sp', sem, val)
        s.bcnt += 1
        s.eng['sp'].nop().then_inc(s.bsem, 1)
        for e in ('pe', 'dve', 'act', 'pool'):
            s.eng[e].wait_ge(s.bsem, s.bcnt)
        s.ninst += 5

    def finish(s):
        for (sem, val) in s.outstanding():
            s._wait('sp', sem, val)

    def dma(s, out, in_, R=(), W=(), q='sp', **kw):
        return s.op(q, lambda e: e.dma_start(out=out, in_=in_, **kw), reads=R, writes=W, dma=True)

    def mm(s, out, lhsT, rhs, start, stop, R, W):
        return s.op('pe', lambda e: e.matmul(out, lhsT=lhsT, rhs=rhs, start=start, stop=stop), reads=R, writes=W)

    def act(s, out, in_, func, R, W, **kw):
        return s.op('act', lambda e: e.activation(out=out, in_=in_, func=func, **kw), reads=R, writes=W)

    def tt(s, e, out, in0, in1, op, R, W):
        return s.op(e, lambda g: g.tensor_tensor(out=out, in0=in0, in1=in1, op=op), reads=R, writes=W)

    def ts(s, e, out, in0, s1, s2, op0, op1, R, W):
        if s2 is None:
            return s.op(e, lambda g: g.tensor_scalar(out=out, in0=in0, scalar1=s1, scalar2=None, op0=op0), reads=R, writes=W)
        return s.op(e, lambda g: g.tensor_scalar(out=out, in0=in0, scalar1=s1, scalar2=s2, op0=op0, op1=op1), reads=R, writes=W)

    def stt(s, e, out, in0, scalar, in1, op0, op1, R, W):
        return s.op(e, lambda g: g.scalar_tensor_tensor(out=out, in0=in0, scalar=scalar, in1=in1, op0=op0, op1=op1), reads=R, writes=W)

    def cp(s, e, out, in_, R, W):
        return s.op(e, lambda g: g.tensor_copy(out=out, in_=in_), reads=R, writes=W)

    def memset(s, e, t, ap, val):
        return s.op(e, lambda g: g.memset(ap, val), writes=[t])


class Phase:
    def __init__(s, k):
        s.k = k

    def __enter__(s):
        s.prev = s.k.stack
        s.st = ExitStack()
        s.k.stack = s.st
        return s

    def __exit__(s, *a):
        s.k.barrier()
        s.k.stack = s.prev
        s.st.close()
        return False


def bview(ap, shape):
    return ap.broadcast_to(list(shape))


def load_bf16(k, name, w_ap, kt, n, q='pool'):
    t = k.sb(name, [128, kt, n], BF16)
    k.dma(t[:, :, :], w_ap.rearrange("(kt p) n -> p kt n", p=128), W=[t], q=q)
    return t


def load_f32(k, name, ap, shape, q='sp'):
    t = k.sb(name, shape, F32)
    if len(shape) == 2:
        k.dma(t[:, :], ap, W=[t], q=q)
    else:
        k.dma(t[:, :, :], ap, W=[t], q=q)
    return t


class Producer:
    def __init__(s, k, identb, pT, nbuf=2):
        s.k = k
        s.identb = identb
        s.pT = pT
        s.xt = k.ring("xt", nbuf, [128, 1024])
        s.junk = k.sb("junk", [128, 1024], BF16)
        s.hb = k.ring("hb", 2, [128, 1024], BF16)
        s.ss = k.ring("ss", 2, [128, 1])
        s.i = 0

    def rstd(s, xt, ss):
        k = s.k
        k.act(s.junk[:, :], xt[:, :], AF.Square, R=[xt], W=[s.junk, ss], accum_out=ss[:, 0:1])
        k.ts('dve', ss[:, 0:1], ss[:, 0:1], 1.0 / 1024, EPS, ALU.mult, ALU.add, R=[ss], W=[ss])
        k.act(ss[:, 0:1], ss[:, 0:1], AF.Sqrt, R=[ss], W=[ss])
        k.op('dve', lambda e: e.reciprocal(out=ss[:, 0:1], in_=ss[:, 0:1]), reads=[ss], writes=[ss])

    def make(s, x_ap, scale, shift, hT, xdep=()):
        k = s.k
        i = s.i
        s.i += 1
        xt = s.xt[i % len(s.xt)]
        hb = s.hb[i % 2]
        ss = s.ss[i % 2]
        k.dma(xt[:, :], x_ap, R=list(xdep), W=[xt])
        s.rstd(xt, ss)
        k.stt('dve', xt[:, :], xt[:, :], ss[:, 0:1], scale[:, :], ALU.mult, ALU.mult, R=[xt, ss, scale], W=[xt])
        k.tt('pool', hb[:, :], xt[:, :], shift[:, :], ALU.add, R=[xt, shift], W=[hb])
        s.transpose(hb, hT)
        return xt

    def transpose(s, hb, hT):
        k = s.k
        for kt in range(8):
            k.mm(s.pT[:, kt * 128:(kt + 1) * 128], hb[:, kt * 128:(kt + 1) * 128], s.identb[:, :], True, True,
                 R=[hb, s.identb], W=[s.pT])
        k.act(hT[:, :, :], s.pT[:, :].rearrange("p (a b) -> p a b", a=8), AF.Copy, R=[s.pT], W=[hT])


def sincos(k, arg, shape, sin_out, cos_out, tmp, tmpi, R_extra=()):
    def full(t):
        return t[:, :] if len(shape) == 2 else t[:, :, :]
    a = full(arg)
    k.ts('dve', full(tmp), a, 1.0 / TWO_PI, None, ALU.mult, None, R=[arg], W=[tmp])
    k.cp('dve', full(tmpi), full(tmp), R=[tmp], W=[tmpi])
    k.cp('dve', full(tmp), full(tmpi), R=[tmpi], W=[tmp])
    k.stt('dve', full(tmp), full(tmp), -TWO_PI, a, ALU.mult, ALU.add, R=[tmp, arg], W=[tmp])
    k.ts('dve', full(tmp), full(tmp), PI, -PI, ALU.min, ALU.max, R=[tmp], W=[tmp])
    so, sT = sin_out
    co, cT = cos_out
    k.act(so, full(tmp), AF.Sin, R=[tmp], W=[sT])
    k.act(full(tmp), full(tmp), AF.Abs, R=[tmp], W=[tmp])
    k.act(co, full(tmp), AF.Sin, R=[tmp], W=[cT], scale=-1.0, bias=PI / 2)


def build(dbg=False):
    nc = bass.Bass("TRN2", target_bir_lowering=False)

    def din(name, shape, dt=F32):
        return nc.dram_tensor(name, list(shape), dt, kind="ExternalInput").ap()

    def dscr(name, shape, dt=F32):
        if dbg:
            return nc.dram_tensor(name, list(shape), dt, kind="ExternalOutput").ap()
        return nc.dram_tensor(name, list(shape), dt).ap()

    xf = din("xf", [8192, 1024]); ctxf = din("ctxf", [256, 1024])
    crep = din("crep", [128, 8, 128]); ccrep = din("ccrep", [128, 8, 128])
    w_ada = din("w_ada", [1024, 6144]); b_ada = din("b_ada", [1, 6144])
    n1g = din("n1g", [128, 1024]); n2g = din("n2g", [128, 1024]); fng = din("fng", [128, 1024])
    w_u = din("w_u", [1024, 256]); w_q = din("w_q", [1024, 768]); w_fF = din("w_fF", [1024, 768])
    w_fB = din("w_fB", [1024, 768]); w_i = din("w_i", [1024, 768]); w_go = din("w_go", [1024, 768])
    w_ga = din("w_ga", [1024, 1024]); w_gb = din("w_gb", [1024, 1024])
    lamA_re = din("lamA_re", [128, 32]); lamA_im = din("lamA_im", [128, 32]); logdtA = din("logdtA", [128, 32])
    lamB_re = din("lamB_re", [128, 32, 64]); lamB_im = din("lamB_im", [128, 32, 64]); logdtB = din("logdtB", [128, 32, 64])
    Bre_pad = din("Bre_pad", [128, 32, 64]); Bim_pad = din("Bim_pad", [128, 32, 64])
    CA = din("CA", [128, 16, 128]); CB = din("CB", [128, 16, 128])
    dcol_d = din("dcol", [128, 2]); sgn_d = din("sgn", [128, 1]); swapm_d = din("swapm", [128, 128])
    iota_d = din("iota256", [128, 256])
    lbl = din("lbl", [128, 2, 768]); hgg6 = din("hgg6", [128, 768])
    hgM = din("hgM", [2, 3, 128, 128])
    cind_d = din("cind", [128, 2])
    wglu = din("wglu", [256, 256]); p_a = din("p_a", [256, 1024]); p_b = din("p_b", [768, 1024])
    w_out = din("w_out", [1024, 1024])
    w_r = din("w_r", [1024, 64]); b_r = din("b_r", [128, 64])
    w1 = din("w1", [65 * 128, 2048]); w3 = din("w3", [65 * 128, 2048]); w2 = din("w2", [65 * 128, 2048])
    pcol_d = din("pcol", [128, 1])
    ident_d = din("ident", [128, 128])
    triu_d = din("triu", [128, 128]); e127_d = din("e127", [128, 128]); thr_d = din("thr", [1, 192])
    out = nc.dram_tensor("out", [4096, 1024], F32, kind="ExternalOutput").ap()

    modr = dscr("modr", [8, 128, 1024])
    yT_d = dscr("yT_d", [128, 2, 4096], BF16)
    oF_d = dscr("oF_d", [4096, 768])
    on_d = dscr("on_d", [4096, 768])
    x2_d = dscr("x2_d", [4096, 1024])
    h2T_d = dscr("h2T_d", [128, 8, 4096], BF16)
    h2tok_d = dscr("h2tok_d", [4096, 1024], BF16)
    vb_d = nc.dram_tensor("vb_d", [66, 128, 768], BF16).ap()
    NROW = 192 * 256
    xg_d = nc.dram_tensor("xg_d", [NROW, 1024], BF16).ap()
    yg_d = nc.dram_tensor("yg_d", [NROW, 1024], BF16).ap()

    k = KB(nc)
    with k.gstack:
        modrT = [TT(modr) for _ in range(8)]
        yT_dT = [TT(yT_d) for _ in range(32)]
        oF_dT = [TT(oF_d) for _ in range(64)]
        on_dT = [TT(on_d) for _ in range(64)]
        x2_dT = [TT(x2_d) for _ in range(32)]
        h2T_dT = [TT(h2T_d) for _ in range(32)]
        h2tok_dT = [TT(h2tok_d) for _ in range(32)]
        vb_dT = [TT(vb_d) for _ in range(66)]
        xgT = TT(xg_d); ygT = TT(yg_d)
        dest8A = TT(k.gstack.enter_context(nc.sbuf_tensor("dest8A", [128, 32, 8], I32)))
        w8A = TT(k.gstack.enter_context(nc.sbuf_tensor("w8A", [128, 32, 8], F32)))
        be_i = TT(k.gstack.enter_context(nc.sbuf_tensor("be_i", [128, 192], I32)))
        zt = TT(k.gstack.enter_context(nc.sbuf_tensor("zt", [128, 2048], BF16)))
        k.memset('pool', zt, zt[:, :], 0.0)
        zinit = [0]
        bcreg = k.gstack.enter_context(nc.gpsimd.register("bcreg"))
        nc.gpsimd.reg_mov(bcreg, NROW - 1)
        bcreg2 = k.gstack.enter_context(nc.gpsimd.register("bcreg2"))
        nc.gpsimd.reg_mov(bcreg2, (64 if USE_CONV else 65) * 128 - 1)
        wb_d = [nc.dram_tensor("w%db_d" % i_, [64 * 128, 2048], BF16).ap() for i_ in range(3)]
        wbT = [TT(wb_d[i_]) for i_ in range(3)]
        stg = [TT(k.gstack.enter_context(nc.sbuf_tensor("stg%d" % i_, [128, 2048], BF16))) for i_ in range(2)]
        cinit = [0]

        def conv_some(n):
            if not USE_CONV:
                return
            for _ in range(n):
                c = cinit[0]
                if c >= 192:
                    return
                cinit[0] += 1
                e_, wh = c // 3, c % 3
                st_ = stg[c % 2]
                k.dma(st_[:, :], (w1, w3, w2)[wh][e_ * 128:(e_ + 1) * 128, :], W=[st_], q='pool')
                k.dma(wb_d[wh][e_ * 128:(e_ + 1) * 128, :], st_[:, :], R=[st_], W=[wbT[wh]], q='act')

        def zero_some(n):
            for _ in range(n):
                c = zinit[0]
                if c >= NROW // 256:
                    return
                zinit[0] += 1
                k.dma(xg_d[c * 256:(c + 1) * 256, :].rearrange("(p a) c -> p (a c)", p=128), zt[:, :], R=[zt], W=[xgT], q='act')

        with Phase(k):
            sc_ = load_f32(k, "silc", crep, [128, 8, 128])
            scc = load_f32(k, "silcc", ccrep, [128, 8, 128])
            k.act(sc_[:, :, :], sc_[:, :, :], AF.Silu, R=[sc_], W=[sc_])
            k.act(scc[:, :, :], scc[:, :, :], AF.Silu, R=[scc], W=[scc])
            ba = k.sb("ba", [1, 6144]); k.dma(ba[:, :], b_ada, W=[ba])
            ones1 = k.sb("ones1", [1, 128]); k.memset('dve', ones1, ones1[:, :], 1.0)
            g1t = load_f32(k, "n1g", n1g, [128, 1024]); g2t = load_f32(k, "n2g", n2g, [128, 1024])
            wa = k.ring("wa", 2, [128, 8, 1024])
            psx = [k.ps("psx", [128, 1024]) for _ in range(2)]
            psc = [k.ps("psc", [128, 1024]) for _ in range(2)]
            res = k.ring("res", 3, [128, 1024])
            ri = 0
            for j in range(6):
                w_t = wa[j % 2]
                k.dma(w_t[:, :, :], w_ada[:, j * 1024:(j + 1) * 1024].rearrange("(kt p) n -> p kt n", p=128), W=[w_t])
                variants = [(sc_, psx[j % 2], j)]
                if j < 2:
                    variants.append((scc, psc[j % 2], 6 + j))
                for (lt, pt, dst) in variants:
                    for hf in range(2):
                        cs = slice(hf * 512, (hf + 1) * 512)
                        for kt in range(8):
                            k.mm(pt[:, cs], lt[:, kt, :], w_t[:, kt, cs], kt == 0, False, R=[lt, w_t], W=[pt])
                        k.mm(pt[:, cs], ones1[0:1, :], ba[0:1, j * 1024 + hf * 512: j * 1024 + (hf + 1) * 512], False, True,
                             R=[ones1, ba], W=[pt])
                    r_t = res[ri % 3]; ri += 1
                    if j == 1 or j == 4:
                        gt = g1t if j == 1 else g2t
                        k.stt('dve', r_t[:, :], pt[:, :], 1.0, gt[:, :], ALU.add, ALU.mult, R=[pt, gt], W=[r_t])
                    else:
                        k.cp('dve', r_t[:, :], pt[:, :], R=[pt], W=[r_t])
                    k.dma(modr[dst], r_t[:, :], R=[r_t], W=[modrT[dst]])

        with Phase(k):
            identb = k.sb("identb", [128, 128], BF16); k.dma(identb[:, :], ident_d, W=[identb], q='pool')
            uT = k.sb("uT", [128, 2, 8704], BF16)
            Bb1 = k.sb("Bb1", [128, 32, 128], BF16); Bb2 = k.sb("Bb2", [128, 32, 128], BF16)
            L1 = k.sb("L1", [128, 16, 128], BF16); L2 = k.sb("L2", [128, 16, 128], BF16)
            Rot = k.sb("Rot", [128, 32, 128])
            cosT = k.sb("cosT", [128, 32, 256], BF16); sinT = k.sb("sinT", [128, 32, 256], BF16)
            rdec = k.sb("rdec", [128, 32])
            dcol = load_f32(k, "dcol", dcol_d, [128, 2])
            with Phase(k):
                Wu = load_bf16(k, "Wu", w_u, 8, 256)
                rows = {}
                for nm_, idx in (("shift1", 0), ("scale1", 1), ("cshift1", 6), ("cscale1", 7)):
                    rows[nm_] = k.sb(nm_, [128, 1024]); k.dma(rows[nm_][:, :], modr[idx], R=[modrT[idx]], W=[rows[nm_]])
                pT = k.ps("pT", [128, 1024]); pu = [k.ps("pu", [128, 512]) for _ in range(2)]
                prod = Producer(k, identb, pT, nbuf=3)
                hTs = k.ring("hT", 2, [128, 8, 128], BF16)
                tiles = [(ctxf[t * 128:(t + 1) * 128, :], rows["cscale1"], rows["cshift1"], t * 128) for t in range(2)]
                tiles += [(xf[t * 128:(t + 1) * 128, :], rows["scale1"], rows["shift1"], 256 + t * 128) for t in range(64)]
                for ti, (xap, scl, shf, c0) in enumerate(tiles):
                    hT = hTs[ti % 2]
                    prod.make(xap, scl, shf, hT)
                    zero_some(3)
                    p_ = pu[ti % 2]
                    for hf in range(2):
                        for kt in range(8):
                            k.mm(p_[:, hf * 128:(hf + 1) * 128], Wu[:, kt, hf * 128:(hf + 1) * 128], hT[:, kt, :], kt == 0, kt == 7,
                                 R=[Wu, hT], W=[p_])
                    k.cp('dve', uT[:, :, c0:c0 + 128], p_[:, 0:256].rearrange("p (a b) -> p a b", a=2), R=[p_], W=[uT])
                k.cp('pool', uT[:, :, 8448:8704], uT[:, :, 0:256], R=[uT], W=[uT])
            with Phase(k):
                lre = load_f32(k, "lreA", lamA_re, [128, 32]); lim = load_f32(k, "limA", lamA_im, [128, 32])
                ldt = load_f32(k, "ldtA", logdtA, [128, 32])
                sgn = load_f32(k, "sgn", sgn_d, [128, 1])
                swapm = load_f32(k, "swapm", swapm_d, [128, 128]); identf = load_f32(k, "identf", ident_d, [128, 128])
                iota = load_f32(k, "iota", iota_d, [128, 256])
                k.ts('dve', lre[:, :], lre[:, :], -1e-4, None, ALU.min, None, R=[lre], W=[lre])
                k.act(ldt[:, :], ldt[:, :], AF.Exp, R=[ldt], W=[ldt])
                th = k.sb("thA", [128, 32])
                k.tt('dve', lre[:, :], lre[:, :], ldt[:, :], ALU.mult, R=[lre, ldt], W=[lre])
                k.tt('dve', th[:, :], lim[:, :], ldt[:, :], ALU.mult, R=[lim, ldt], W=[th])
                k.act(rdec[:, :], lre[:, :], AF.Exp, R=[lre], W=[rdec])
                a256 = k.sb("a256", [128, 32]); tA = k.sb("tA", [128, 32]); tAi = k.sb("tAi", [128, 32], I32)
                s256 = k.sb("s256", [128, 32]); c256 = k.sb("c256", [128, 32])
                k.ts('dve', a256[:, :], th[:, :], 256.0, None, ALU.mult, None, R=[th], W=[a256])
                sincos(k, a256, [128, 32], (s256[:, :], s256), (c256[:, :], c256), tA, tAi)
                k.ts('dve', s256[:, :], s256[:, :], sgn[:, 0:1], None, ALU.mult, None, R=[s256, sgn], W=[s256])
                for gd in range(32):
                    k.ts('dve', Rot[:, gd, :], identf[:, :], c256[:, gd:gd + 1], None, ALU.mult, None, R=[identf, c256], W=[Rot])
                    k.stt('dve', Rot[:, gd, :], swapm[:, :], s256[:, gd:gd + 1], Rot[:, gd, :], ALU.mult, ALU.add,
                          R=[swapm, s256, Rot], W=[Rot])
                argt = k.sb("argt", [128, 256]); tT = k.sb("tT", [128, 256]); tTi = k.sb("tTi", [128, 256], I32)
                for gd in range(32):
                    k.ts('dve', argt[:, :], iota[:, :], th[:, gd:gd + 1], None, ALU.mult, None, R=[iota, th], W=[argt])
                    sincos(k, argt, [128, 256], (sinT[:, gd, :], sinT), (cosT[:, gd, :], cosT), tT, tTi)
                ca = load_f32(k, "ca", CA, [128, 16, 128]); cb = load_f32(k, "cb", CB, [128, 16, 128])
                k.ts('dve', L1[:, :, :], ca[:, :, :], sgn[:, 0:1], None, ALU.mult, None, R=[ca, sgn], W=[L1])
                k.ts('dve', L2[:, :, :], cb[:, :, :], -1.0, None, ALU.mult, None, R=[cb], W=[L2])

            for G0 in (0, 16):
              with Phase(k):
                      SH = [128, 16, 64]
                      lr = load_f32(k, "lrB", lamB_re[:, G0:G0 + 16, :], SH); li = load_f32(k, "liB", lamB_im[:, G0:G0 + 16, :], SH); ld = load_f32(k, "ldB", logdtB[:, G0:G0 + 16, :], SH)
                      br = load_f32(k, "brB", Bre_pad[:, G0:G0 + 16, :], SH); bi = load_f32(k, "biB", Bim_pad[:, G0:G0 + 16, :], SH)
                      f_ = lambda t: t[:, :, :]
                      k.ts('dve', f_(lr), f_(lr), -1e-4, None, ALU.min, None, R=[lr], W=[lr])
                      k.act(f_(ld), f_(ld), AF.Exp, R=[ld], W=[ld])
                      aB = k.sb("aB", SH); thB = k.sb("thB", SH)
                      k.tt('dve', f_(aB), f_(lr), f_(ld), ALU.mult, R=[lr, ld], W=[aB])
                      k.tt('dve', f_(thB), f_(li), f_(ld), ALU.mult, R=[li, ld], W=[thB])
                      sB = k.sb("sB", SH); cB = k.sb("cB", SH); tB = k.sb("tB", SH); tBi = k.sb("tBi", SH, I32)
                      sincos(k, thB, SH, (f_(sB), sB), (f_(cB), cB), tB, tBi)
                      k.act(f_(aB), f_(aB), AF.Exp, R=[aB], W=[aB])
                      k.tt('dve', f_(cB), f_(cB), f_(aB), ALU.mult, R=[cB, aB], W=[cB])
                      k.tt('dve', f_(sB), f_(sB), f_(aB), ALU.mult, R=[sB, aB], W=[sB])
                      k.ts('dve', f_(cB), f_(cB), -1.0, None, ALU.add, None, R=[cB], W=[cB])
                      den = aB
                      k.tt('dve', f_(den), f_(lr), f_(lr), ALU.mult, R=[lr], W=[den])
                      k.tt('dve', f_(tB), f_(li), f_(li), ALU.mult, R=[li], W=[tB])
                      k.tt('dve', f_(den), f_(den), f_(tB), ALU.add, R=[den, tB], W=[den])
                      k.op('dve', lambda e: e.reciprocal(out=f_(den), in_=f_(den)), reads=[den], writes=[den])
                      cre = thB; cim = ld
                      k.tt('dve', f_(cre), f_(cB), f_(lr), ALU.mult, R=[cB, lr], W=[cre])
                      k.tt('dve', f_(tB), f_(sB), f_(li), ALU.mult, R=[sB, li], W=[tB])
                      k.tt('dve', f_(cre), f_(cre), f_(tB), ALU.add, R=[cre, tB], W=[cre])
                      k.tt('dve', f_(cre), f_(cre), f_(den), ALU.mult, R=[cre, den], W=[cre])
                      k.tt('dve', f_(cim), f_(sB), f_(lr), ALU.mult, R=[sB, lr], W=[cim])
                      k.tt('dve', f_(tB), f_(cB), f_(li), ALU.mult, R=[cB, li], W=[tB])
                      k.tt('dve', f_(cim), f_(cim), f_(tB), ALU.subtract, R=[cim, tB], W=[cim])
                      k.tt('dve', f_(cim), f_(cim), f_(den), ALU.mult, R=[cim, den], W=[cim])
                      bbre = sB; bbim = cB
                      k.tt('dve', f_(bbre), f_(cre), f_(br), ALU.mult, R=[cre, br], W=[bbre])
                      k.tt('dve', f_(tB), f_(cim), f_(bi), ALU.mult, R=[cim, bi], W=[tB])
                      k.tt('dve', f_(bbre), f_(bbre), f_(tB), ALU.subtract, R=[bbre, tB], W=[bbre])
                      k.tt('dve', f_(bbim), f_(cre), f_(bi), ALU.mult, R=[cre, bi], W=[bbim])
                      k.tt('dve', f_(tB), f_(cim), f_(br), ALU.mult, R=[cim, br], W=[tB])
                      k.tt('dve', f_(bbim), f_(bbim), f_(tB), ALU.add, R=[bbim, tB], W=[bbim])
                      k.cp('dve', Bb1[:, G0:G0 + 16, 0:64], f_(bbre), R=[bbre], W=[Bb1])
                      k.cp('dve', Bb1[:, G0:G0 + 16, 64:128], f_(bbim), R=[bbim], W=[Bb1])
                      k.cp('dve', Bb2[:, G0:G0 + 16, 0:64], f_(bbim), R=[bbim], W=[Bb2])
                      k.ts('dve', Bb2[:, G0:G0 + 16, 64:128], f_(bbre), -1.0, None, ALU.mult, None, R=[bbre], W=[Bb2])

            with Phase(k):
                yF = k.sb("yF", [128, 2, 4096])
                PP = [k.ps("PP", [128, 512]) for _ in range(4)]
                yps = [k.ps("yps", [128, 512]) for _ in range(2)]
                psm2 = [k.ps("psm", [128, 512]) for _ in range(2)]; psm = [psm2[g_ % 2] for g_ in range(16)]
                m1r = k.ring("m1", 6, [128, 256]); m2r = k.ring("m2", 6, [128, 256]); wr = k.ring("w", 6, [128, 256])
                vr = k.ring("v", 6, [128, 256])
                A1r = k.ring("A1", 6, [128, 256], BF16); A2r = k.ring("A2", 6, [128, 256], BF16)
                ybf = k.ring("ybf", 2, [128, 256], BF16); ytmp = k.ring("ytmp", 2, [128, 256])
                units = []
                vins = []
                for fd in range(2):
                    nblk = 17 if fd == 0 else 33
                    vin = [k.sb("vin", [128, 1]) for _ in range(16)]
                    vins.append(vin)
                    for g in range(16):
                        k.memset('pool', vin[g], vin[g][:, :], 0.0)
                    for m in range(nblk):
                        if fd == 0:
                            c0 = m * 256
                            near = m >= 1
                        else:
                            c0 = 8704 - 256 * m - 256
                            near = m >= 17
                        n0 = c0 - 256
                        for hf in range(2):
                            for gi in range(8):
                                units.append(dict(fd=fd, m=m, hf=hf, gi=gi, c0=c0, n0=n0, near=near, last=(m == nblk - 1)))

                def stageA(i, u):
                    fd, hf, c0 = u["fd"], u["hf"], u["c0"]
                    g = hf * 8 + u["gi"]; gd = fd * 16 + g
                    rhs = uT[:, hf, c0:c0 + 256] if fd == 0 else uT[:, hf, c0:c0 + 256][:, ::-1]
                    P = PP[i % 4]; m1 = m1r[i % 6]; m2 = m2r[i % 6]; w_ = wr[i % 6]
                    k.mm(P[:, 0:256], Bb1[:, gd, :], rhs, True, True, R=[Bb1, uT], W=[P])
                    k.mm(P[:, 256:512], Bb2[:, gd, :], rhs, True, True, R=[Bb2, uT], W=[P])
                    k.tt('dve', m1[:, :], P[:, 0:256], cosT[:, gd, :], ALU.mult, R=[P, cosT], W=[m1])
                    k.tt('dve', m2[:, :], P[:, 256:512], sinT[:, gd, :], ALU.mult, R=[P, sinT], W=[m2])
                    k.tt('dve' if u["near"] else 'pool', w_[:, :], m1[:, :], m2[:, :], ALU.add, R=[m1, m2], W=[w_])
                    if u["gi"] == 0:
                        conv_some(2)

                def stageB(i, u):
                    fd, hf, c0, n0, gi = u["fd"], u["hf"], u["c0"], u["n0"], u["gi"]
                    g = hf * 8 + gi; gd = fd * 16 + g
                    w_ = wr[i % 6]; v = vr[i % 6]; A1 = A1r[i % 6]; A2 = A2r[i % 6]
                    vi = vins[fd][g]
                    k.op('dve', lambda e: e.tensor_tensor_scan(
                        out=v[:, :], data0=bview(rdec[:, gd:gd + 1], [128, 256]), data1=w_[:, :],
                        initial=vi[:, 0:1], op0=ALU.mult, op1=ALU.add), reads=[rdec, w_, vi], writes=[v])
                    if not u["last"]:
                        k.mm(psm[g][:, g:g + 1], Rot[:, gd, :], v[:, 255:256], True, True, R=[Rot, v], W=[psm[g]])
                        k.act(vi[:, 0:1], psm[g][:, g:g + 1], AF.Copy, R=[psm[g]], W=[vi])
                    if u["near"]:
                        k.tt('pool', A1[:, :], v[:, :], cosT[:, gd, :], ALU.mult, R=[v, cosT], W=[A1])
                        k.tt('pool', A2[:, :], v[:, :], sinT[:, gd, :], ALU.mult, R=[v, sinT], W=[A2])
                        a1 = A1[:, :] if fd == 0 else A1[:, ::-1]
                        a2 = A2[:, :] if fd == 0 else A2[:, ::-1]
                        k.mm(yps[hf][:, 0:256], L1[:, g, :], a1, gi == 0, False, R=[L1, A1], W=[yps[hf]])
                        k.mm(yps[hf][:, 0:256], L2[:, g, :], a2, False, gi == 7, R=[L2, A2], W=[yps[hf]])
                        if gi == 7:
                            if fd == 0:
                                k.stt('dve', yF[:, hf, n0:n0 + 256], uT[:, hf, c0:c0 + 256], dcol[:, hf:hf + 1], yps[hf][:, 0:256],
                                      ALU.mult, ALU.add, R=[uT, dcol, yps[hf]], W=[yF])
                            else:
                                yt = ytmp[hf]; yb = ybf[hf]
                                k.tt('dve', yt[:, :], yps[hf][:, 0:256], yF[:, hf, n0:n0 + 256], ALU.add, R=[yps[hf], yF], W=[yt])
                                k.act(yb[:, :], yt[:, :], AF.Gelu, R=[yt], W=[yb])
                                k.dma(yT_d[:, hf, n0:n0 + 256], yb[:, :], R=[yb], W=[yT_dT[n0 // 128], yT_dT[n0 // 128 + 1]])

                for i in range(len(units) + SK):
                    if i < len(units):
                        stageA(i, units[i])
                    if i >= SK:
                        stageB(i - SK, units[i - SK])

        with Phase(k):
            identb = k.sb("identb", [128, 128], BF16); k.dma(identb[:, :], ident_d, W=[identb], q='pool')
            conv_some(1000)
            Wq = load_bf16(k, "Wq", w_q, 8, 768); Wi = load_bf16(k, "Wi", w_i, 8, 768)
            rows = {}
            for nm_, idx in (("shift1", 0), ("scale1", 1), ("cshift1", 6), ("cscale1", 7)):
                rows[nm_] = k.sb(nm_, [128, 1024]); k.dma(rows[nm_][:, :], modr[idx], R=[modrT[idx]], W=[rows[nm_]])
            lbt = load_f32(k, "lbl", lbl, [128, 2, 768])
            lb = k.sb("lb", [128, 768]); oml = k.sb("oml", [128, 768])
            k.tt('dve', lb[:, :], lbt[:, 0, :], lbt[:, 1, :], ALU.subtract, R=[lbt], W=[lb])
            k.act(lb[:, :], lb[:, :], AF.Sigmoid, R=[lb], W=[lb])
            k.ts('dve', oml[:, :], lb[:, :], -1.0, 1.0, ALU.mult, ALU.add, R=[lb], W=[oml])
            g6 = load_f32(k, "hgg6", hgg6, [128, 768])
            cind = load_f32(k, "cind", cind_d, [128, 2])
            X = k.ps("X", [128, 1024]); Y = k.ps("Y", [128, 1024]); Z = k.ps("Z", [128, 1024])
            Wp = k.ps("Wp", [128, 512]); Vp = k.ps("Vp", [128, 512])
            prod = Producer(k, identb, X, nbuf=3)
            hTs = k.ring("hT", 2, [128, 8, 128], BF16)
            sg = k.sb("sg", [128, 768]); logf = k.ring("logf", 2, [128, 768])
            kb = k.ring("kb", 2, [128, 768], BF16); vb = k.ring("vb", 2, [128, 768], BF16)
            e4 = k.sb("e4", [128, 768]); kdec = k.ring("kdec", 2, [128, 768], BF16)
            e1 = k.sb("e1", [128, 6, 64]); e2 = k.sb("e2", [128, 6, 64]); e3 = k.sb("e3", [128, 6, 64])
            dec = k.ring("dec", 2, [128, 6, 2])
            qi = k.ring("qi", 2, [128, 6, 64], BF16); qin = k.ring("qin", 2, [128, 6, 64], BF16)
            kiT = k.ring("kiT", 2, [128, 6, 64], BF16)
            scm = k.ring("scm", 2, [64, 6, 64], BF16)
            S32 = [k.sb("S32", [128, 128]) for _ in range(6)]
            Sb = k.sb("Sb", [128, 6, 128], BF16)
            osb = k.ring("osb", 2, [64, 768]); oFl = k.ring("oFl", 2, [64, 768])
            ssq = k.ring("ssq", 2, [64, 6]); ojunk = k.sb("ojunk", [64, 128], BF16)
            for ch in range(2):
                with Phase(k):
                    Wf = load_bf16(k, "Wf", w_fF if ch == 0 else w_fB, 8, 768)
                    Minc = load_f32(k, "Minc", hgM[ch, 0], [128, 128])
                    Msuf = load_f32(k, "Msuf", hgM[ch, 2], [128, 128])
                    Mq = k.sb("Mq", [128, 130])
                    k.dma(Mq[:, 0:64], hgM[ch, 1][:, 0:64], W=[Mq])
                    k.dma(Mq[:, 64:66], cind_d, W=[Mq])
                    k.dma(Mq[:, 66:130], hgM[ch, 0][:, 0:64], W=[Mq])
                    maskb = k.sb("maskb", [64, 64], BF16); k.dma(maskb[:, :], hgM[ch, 0][0:64, 0:64], W=[maskb], q='pool')
                    for h_ in range(6):
                        k.memset('dve', S32[h_], S32[h_][:, :], 0.0)
                    k.memset('pool', Sb, Sb[:, :, :], 0.0)
                    if ch == 0:
                        tl = [("c", t) for t in range(2)] + [("x", w) for w in range(64)]
                        corder = [0, 1]
                    else:
                        tl = [("c", t) for t in (1, 0)] + [("x", w) for w in range(63, -1, -1)]
                        corder = [1, 0]
                    xcol = xf.rearrange("(r w) d -> r w d", w=64)
                    def stA(ti, kind, idx):
                        hT = hTs[ti % 2]
                        near = kind == "x"
                        if kind == "c":
                            prod.make(ctxf[idx * 128:(idx + 1) * 128, :], rows["cscale1"], rows["cshift1"], hT)
                        else:
                            prod.make(xcol[:, idx, :], rows["scale1"], rows["shift1"], hT)
                        lf = logf[ti % 2]; kb_ = kb[ti % 2]; vb_ = vb[ti % 2]; kd = kdec[ti % 2]; dc = dec[ti % 2]
                        for (a, b) in ((0, 512), (512, 768)):
                            for kt in range(8):
                                k.mm(Y[:, a:b], hT[:, kt, :], Wf[:, kt, a:b], kt == 0, kt == 7, R=[hT, Wf], W=[Y])
                        tidx = idx if kind == "c" else 2 + idx
                        if ch == 0:
                            for (a, b) in ((0, 512), (512, 768)):
                                for kt in range(8):
                                    k.mm(X[:, a:b], hT[:, kt, :], Wi[:, kt, a:b], kt == 0, kt == 7, R=[hT, Wi], W=[X])
                        k.act(sg[:, :], Y[:, 0:768], AF.Sigmoid, R=[Y], W=[sg])
                        k.tt('dve', sg[:, :], sg[:, :], oml[:, :], ALU.mult, R=[sg, oml], W=[sg])
                        k.tt('dve', sg[:, :], sg[:, :], lb[:, :], ALU.add, R=[sg, lb], W=[sg])
                        k.act(lf[:, :], sg[:, :], AF.Ln, R=[sg], W=[lf])
                        k.ts('pool', kb_[:, :], sg[:, :], -1.0, 1.0, ALU.mult, ALU.add, R=[sg], W=[kb_])
                        if ch == 0:
                            k.act(vb_[:, :], X[:, 0:768], AF.Copy, R=[X], W=[vb_])
                            k.dma(vb_d[tidx], vb_[:, :], R=[vb_], W=[vb_dT[tidx]])
                        else:
                            k.dma(vb_[:, :], vb_d[tidx], R=[vb_dT[tidx]], W=[vb_])
                        for (a, b) in ((0, 512), (512, 768)):
                            k.mm(Y[:, a:b], Msuf[:, :], lf[:, a:b], True, True, R=[Msuf, lf], W=[Y])
                        k.act(e4[:, :], Y[:, 0:768], AF.Exp, R=[Y], W=[e4])
                        k.tt('dve', kd[:, :], kb_[:, :], e4[:, :], ALU.mult, R=[kb_, e4], W=[kd])
                        ncol = 130 if near else 2
                        for h in range(6):
                            off = (h // 3) * 512 + (h % 3) * 130
                            rq = Mq[:, 0:130] if near else Mq[:, 64:66]
                            k.mm(X[:, off:off + ncol], lf[:, h * 128:(h + 1) * 128], rq, True, True, R=[lf, Mq], W=[X])
                        Yv = X[:, :].rearrange("p (b x) -> p b x", b=2)[:, :, 0:390].rearrange("p b (h c) -> p b h c", h=3)

                        def hv(t):
                            return t[:, :, :].rearrange("p (b h) c -> p b h c", b=2)
                        if near:
                            k.act(hv(e1), Yv[:, :, :, 0:64], AF.Exp, R=[X], W=[e1])
                            k.act(hv(e2), Yv[:, :, :, 0:64], AF.Exp, R=[X], W=[e2], scale=-1.0)
                            k.act(hv(e3), Yv[:, :, :, 66:130], AF.Exp, R=[X], W=[e3])
                            k.act(hv(dc), Yv[:, :, :, 64:66], AF.Exp, R=[X], W=[dc])
                        else:
                            k.act(hv(dc), Yv[:, :, :, 0:2], AF.Exp, R=[X], W=[dc])
                        if near:
                            qi_ = qi[ti % 2]; qn_ = qin[ti % 2]; ki_ = kiT[ti % 2]; sc_m = scm[ti % 2]
                            for h in range(6):
                                for kt in range(8):
                                    k.mm(Wp[:, h * 64:(h + 1) * 64], Wq[:, kt, h * 128:(h + 1) * 128], hT[:, kt, 0:64], kt == 0, kt == 7,
                                         R=[Wq, hT], W=[Wp])
                            Wpv = Wp[:, 0:384].rearrange("p (h c) -> p h c", h=6)
                            k.tt('dve', qi_[:, :, :], Wpv, e1[:, :, :], ALU.mult, R=[Wp, e1], W=[qi_])
                            k.tt('dve', qn_[:, :, :], Wpv, e3[:, :, :], ALU.mult, R=[Wp, e3], W=[qn_])
                            for h in range(6):
                                k.mm(Vp[:, h * 64:(h + 1) * 64], kb_[0:64, h * 128:(h + 1) * 128], identb[0:64, 0:64], True, True,
                                     R=[kb_, identb], W=[Vp])
                            k.tt('dve', ki_[:, :, :], Vp[:, 0:384].rearrange("p (h c) -> p h c", h=6), e2[:, :, :], ALU.mult,
                                 R=[Vp, e2], W=[ki_])
                            for h in range(6):
                                k.mm(Wp[0:64, h * 64:(h + 1) * 64], ki_[:, h, :], qi_[:, h, :], True, True, R=[ki_, qi_], W=[Wp])
                            k.tt('dve', sc_m[:, :, :], Wp[0:64, 0:384].rearrange("p (h c) -> p h c", h=6),
                                 bview(maskb[:, None, :], [64, 6, 64]), ALU.mult, R=[Wp, maskb], W=[sc_m])
                        return dict(ti=ti, idx=idx, near=near, kd=kd, vb_=vb_, dc=dc, qn_=(qn_ if near else None), sc_m=(sc_m if near else None))

                    def stB(hd):
                        ti = hd["ti"]; idx = hd["idx"]; near = hd["near"]; kd = hd["kd"]; vb_ = hd["vb_"]; dc = hd["dc"]
                        qn_ = hd["qn_"]; sc_m = hd["sc_m"]
                        for c in corder:
                            if c == 0 and near:
                                for h in range(6):
                                    hs = slice(h * 128, (h + 1) * 128)
                                    k.mm(Z[0:64, hs], sc_m[:, h, :], vb_[0:64, hs], True, False, R=[sc_m, vb_], W=[Z])
                                    k.mm(Z[0:64, hs], qn_[:, h, :], Sb[:, h, :], False, True, R=[qn_, Sb], W=[Z])
                                w = idx
                                if ch == 0:
                                    ob = osb[ti % 2]
                                    k.act(ob[:, :], Z[0:64, 0:768], AF.Copy, R=[Z], W=[ob])
                                    k.dma(oF_d[w * 64:(w + 1) * 64, :], ob[:, :], R=[ob], W=[oF_dT[w]])
                                else:
                                    ob = osb[ti % 2]; of_ = oFl[ti % 2]; sq = ssq[ti % 2]
                                    k.dma(of_[:, :], oF_d[w * 64:(w + 1) * 64, :], R=[oF_dT[w]], W=[of_])
                                    k.tt('dve', ob[:, :], Z[0:64, 0:768], of_[:, :], ALU.add, R=[Z, of_], W=[ob])
                                    for h in range(6):
                                        k.act(ojunk[:, :], ob[:, h * 128:(h + 1) * 128], AF.Square, R=[ob], W=[ojunk, sq],
                                              accum_out=sq[:, h:h + 1])
                                    k.ts('dve', sq[:, :], sq[:, :], 1.0 / 128, EPS, ALU.mult, ALU.add, R=[sq], W=[sq])
                                    k.act(sq[:, :], sq[:, :], AF.Sqrt, R=[sq], W=[sq])
                                    k.op('dve', lambda e, sq=sq: e.reciprocal(out=sq[:, :], in_=sq[:, :]), reads=[sq], writes=[sq])
                                    obv = ob[:, :].rearrange("p (h c) -> p h c", h=6)
                                    k.tt('dve', obv, obv, bview(sq[:, :, None], [64, 6, 128]), ALU.mult, R=[ob, sq], W=[ob])
                                    k.tt('pool', ob[:, :], ob[:, :], g6[0:64, :], ALU.mult, R=[ob, g6], W=[ob])
                                    k.dma(on_d.rearrange("(r w) c -> r w c", w=64)[:, w, :], ob[:, :], R=[ob], W=[on_dT[w]])
                            cs = slice(c * 64, (c + 1) * 64)
                            for h in range(6):
                                hs = slice(h * 128, (h + 1) * 128)
                                k.mm(Z[:, hs], kd[cs, hs], vb_[cs, hs], True, True, R=[kd, vb_], W=[Z])
                            for h in range(6):
                                hs = slice(h * 128, (h + 1) * 128)
                                k.stt('dve', S32[h][:, :], S32[h][:, :], dc[:, h, c:c + 1], Z[:, hs], ALU.mult, ALU.add,
                                      R=[S32[h], dc, Z], W=[S32[h]])
                            for h in range(6):
                                k.cp('pool', Sb[:, h, :], S32[h][:, :], R=[S32[h]], W=[Sb])

                    pend_ = None
                    for ti, (kind, idx) in enumerate(tl):
                        hd_ = stA(ti, kind, idx)
                        if pend_ is not None:
                            stB(pend_)
                        pend_ = hd_
                    stB(pend_)

        with Phase(k):
            selA = k.sb("selA", [128, 32, 64]); gateA = k.sb("gateA", [128, 32, 64])
            with Phase(k):
                identb = k.sb("identb", [128, 128], BF16); k.dma(identb[:, :], ident_d, W=[identb], q='pool')
                identf = load_f32(k, "identf", ident_d, [128, 128])
                Wgo = load_bf16(k, "Wgo", w_go, 8, 768); Wga = load_bf16(k, "Wga", w_ga, 8, 1024); Wgb = load_bf16(k, "Wgb", w_gb, 8, 1024)
                Wgl = load_bf16(k, "Wgl", wglu, 2, 256); Pa = load_bf16(k, "Pa", p_a, 2, 1024); Pb = load_bf16(k, "Pb", p_b, 6, 1024)
                Wo = load_bf16(k, "Wo", w_out, 8, 1024); Wr = load_bf16(k, "Wr", w_r, 8, 64)
                brt = load_f32(k, "brt", b_r, [128, 64])
                rows = {}
                for nm_, idx in (("shift1", 0), ("scale1", 1), ("g1", 2), ("shift2", 3), ("scale2", 4)):
                    rows[nm_] = k.sb(nm_, [128, 1024]); k.dma(rows[nm_][:, :], modr[idx], R=[modrT[idx]], W=[rows[nm_]])
                X = k.ps("X", [128, 1024]); Y = k.ps("Y", [128, 1024]); Z = k.ps("Z", [128, 1024]); Wp = k.ps("Wp", [128, 1024])
                prod = Producer(k, identb, X, nbuf=2)
                hT = k.sb("hT", [128, 8, 128], BF16)
                yT = k.sb("yT", [128, 2, 128], BF16); sgl = k.sb("sgl", [128, 2, 128]); yaT = k.sb("yaT", [128, 2, 128], BF16)
                ont = k.sb("ont", [128, 768]); sgo = k.sb("sgo", [128, 768]); ybb = k.sb("ybb", [128, 768], BF16)
                ybT = k.sb("ybT", [128, 6, 128], BF16)
                sga = k.sb("sga", [128, 1024]); t1 = k.sb("t1", [128, 1024]); mb = k.sb("mb", [128, 1024], BF16)
                mT = k.sb("mT", [128, 8, 128], BF16)
                x2 = k.sb("x2", [128, 1024]); h2b = k.sb("h2b", [128, 1024], BF16); h2T = k.sb("h2T", [128, 8, 128], BF16)
                ss2 = k.sb("ss2", [128, 1])
                rs = k.sb("rs", [128, 64]); bz = k.sb("bz", [128, 64]); eq = k.sb("eq", [128, 64]); m1 = k.sb("m1g", [128, 8])
                m2 = k.sb("m2g", [128, 8]); top8 = k.sb("top8", [128, 8]); gok = k.sb("gok", [128, 8]); msk = k.sb("msk", [128, 64])
                tq = k.sb("tq", [128, 64]); den = k.sb("den", [128, 1])
                for t in range(32):
                    r0 = t * 128
                    xt = prod.make(xf[r0:r0 + 128, :], rows["scale1"], rows["shift1"], hT)
                    k.dma(yT[:, :, :], yT_d[:, :, r0:r0 + 128], R=[yT_dT[t]], W=[yT])
                    for mh in range(2):
                        for kh in range(2):
                            k.mm(Wp[:, mh * 128:(mh + 1) * 128], Wgl[:, kh, mh * 128:(mh + 1) * 128], yT[:, kh, :], kh == 0, kh == 1,
                                 R=[Wgl, yT], W=[Wp])
                    k.act(sgl[:, :, :], Wp[:, 0:256].rearrange("p (a b) -> p a b", a=2), AF.Sigmoid, R=[Wp], W=[sgl])
                    k.tt('dve', yaT[:, :, :], yT[:, :, :], sgl[:, :, :], ALU.mult, R=[yT, sgl], W=[yaT])
                    for (a, b) in ((0, 512), (512, 768)):
                        for kt in range(8):
                            k.mm(Y[:, a:b], hT[:, kt, :], Wgo[:, kt, a:b], kt == 0, kt == 7, R=[hT, Wgo], W=[Y])
                    k.act(sgo[:, :], Y[:, 0:768], AF.Silu, R=[Y], W=[sgo])
                    k.dma(ont[:, :], on_d[r0:r0 + 128, :], R=on_dT, W=[ont])
                    k.tt('dve', ybb[:, :], ont[:, :], sgo[:, :], ALU.mult, R=[ont, sgo], W=[ybb])
                    for j in range(6):
                        k.mm(X[:, j * 128:(j + 1) * 128], ybb[:, j * 128:(j + 1) * 128], identb[:, :], True, True, R=[ybb, identb], W=[X])
                    k.act(ybT[:, :, :], X[:, 0:768].rearrange("p (a b) -> p a b", a=6), AF.Copy, R=[X], W=[ybT])
                    for hf in range(2):
                        cs = slice(hf * 512, (hf + 1) * 512)
                        for kt in range(8):
                            k.mm(Y[:, cs], hT[:, kt, :], Wga[:, kt, cs], kt == 0, kt == 7, R=[hT, Wga], W=[Y])
                    k.act(sga[:, :], Y[:, :], AF.Sigmoid, R=[Y], W=[sga])
                    for hf in range(2):
                        cs = slice(hf * 512, (hf + 1) * 512)
                        for kh in range(2):
                            k.mm(Z[:, cs], yaT[:, kh, :], Pa[:, kh, cs], kh == 0, kh == 1, R=[yaT, Pa], W=[Z])
                    k.tt('dve', t1[:, :], sga[:, :], Z[:, :], ALU.mult, R=[sga, Z], W=[t1])
                    for hf in range(2):
                        cs = slice(hf * 512, (hf + 1) * 512)
                        for kt in range(8):
                            k.mm(Y[:, cs], hT[:, kt, :], Wgb[:, kt, cs], kt == 0, kt == 7, R=[hT, Wgb], W=[Y])
                    k.act(sga[:, :], Y[:, :], AF.Sigmoid, R=[Y], W=[sga])
                    for hf in range(2):
                        cs = slice(hf * 512, (hf + 1) * 512)
                        for j in range(6):
                            k.mm(Z[:, cs], ybT[:, j, :], Pb[:, j, cs], j == 0, j == 5, R=[ybT, Pb], W=[Z])
                    k.tt('dve', sga[:, :], sga[:, :], Z[:, :], ALU.mult, R=[sga, Z], W=[sga])
                    k.tt('pool', mb[:, :], t1[:, :], sga[:, :], ALU.add, R=[t1, sga], W=[mb])
                    prod.transpose(mb, mT)
                    for hf in range(2):
                        cs = slice(hf * 512, (hf + 1) * 512)
                        for kt in range(8):
                            k.mm(Wp[:, cs], mT[:, kt, :], Wo[:, kt, cs], kt == 0, kt == 7, R=[mT, Wo], W=[Wp])
                    k.dma(x2[:, :], xf[r0:r0 + 128, :], R=[xt], W=[x2])
                    k.tt('dve', t1[:, :], Wp[:, :], rows["g1"][:, :], ALU.mult, R=[Wp, rows["g1"]], W=[t1])
                    k.tt('dve', x2[:, :], x2[:, :], t1[:, :], ALU.add, R=[x2, t1], W=[x2])
                    k.dma(x2_d[r0:r0 + 128, :], x2[:, :], R=[x2], W=[x2_dT[t]])
                    prod.rstd(x2, ss2)
                    k.stt('dve', t1[:, :], x2[:, :], ss2[:, 0:1], rows["scale2"][:, :], ALU.mult, ALU.mult, R=[x2, ss2, rows["scale2"]], W=[t1])
                    k.tt('pool', h2b[:, :], t1[:, :], rows["shift2"][:, :], ALU.add, R=[t1, rows["shift2"]], W=[h2b])
                    prod.transpose(h2b, h2T)
                    k.dma(h2T_d[:, :, r0:r0 + 128], h2T[:, :, :], R=[h2T], W=[h2T_dT[t]])
                    for kt in range(8):
                        k.mm(Wp[:, 0:64], h2T[:, kt, :], Wr[:, kt, :], kt == 0, kt == 7, R=[h2T, Wr], W=[Wp])
                    k.act(rs[:, :], Wp[:, 0:64], AF.Sigmoid, R=[Wp], W=[rs])
                    k.tt('dve', bz[:, :], rs[:, :], brt[:, :], ALU.add, R=[rs, brt], W=[bz])
                    bz3 = bz[:, :].rearrange("p (g e) -> p g e", g=8)
                    k.op('dve', lambda e: e.tensor_reduce(out=m1[:, :], in_=bz3, axis=AX.X, op=ALU.max), reads=[bz], writes=[m1])
                    k.tt('dve', eq[:, :].rearrange("p (g e) -> p g e", g=8), bz3, bview(m1[:, :, None], [128, 8, 8]), ALU.is_equal,
                         R=[bz, m1], W=[eq])
                    k.stt('dve', eq[:, :], eq[:, :], -1e9, bz[:, :], ALU.mult, ALU.add, R=[eq, bz], W=[eq])
                    k.op('dve', lambda e: e.tensor_reduce(out=m2[:, :], in_=eq[:, :].rearrange("p (g e) -> p g e", g=8), axis=AX.X, op=ALU.max),
                         reads=[eq], writes=[m2])
                    k.tt('dve', m1[:, :], m1[:, :], m2[:, :], ALU.add, R=[m1, m2], W=[m1])
                    k.op('dve', lambda e: e.max(out=top8[:, :], in_=m1[:, :]), reads=[m1], writes=[top8])
                    k.ts('dve', gok[:, :], m1[:, :], top8[:, 3:4], None, ALU.is_ge, None, R=[m1, top8], W=[gok])
                    gok3 = bview(gok[:, :, None], [128, 8, 8])
                    k.tt('dve', msk[:, :].rearrange("p (g e) -> p g e", g=8), bz3, gok3, ALU.mult, R=[bz, gok], W=[msk])
                    k.ts('dve', gok[:, :], gok[:, :], 1e9, -1e9, ALU.mult, ALU.add, R=[gok], W=[gok])
                    k.tt('dve', msk[:, :].rearrange("p (g e) -> p g e", g=8), msk[:, :].rearrange("p (g e) -> p g e", g=8), gok3, ALU.add,
                         R=[msk, gok], W=[msk])
                    k.op('dve', lambda e: e.max(out=top8[:, :], in_=msk[:, :]), reads=[msk], writes=[top8])
                    k.ts('dve', selA[:, t, :], msk[:, :], top8[:, 7:8], None, ALU.is_ge, None, R=[msk, top8], W=[selA])
                    k.tt('dve', tq[:, :], selA[:, t, :], rs[:, :], ALU.mult, R=[selA, rs], W=[tq])
                    k.op('dve', lambda e: e.reduce_sum(out=den[:, :], in_=tq[:, :], axis=AX.X), reads=[tq], writes=[den])
                    k.op('dve', lambda e: e.reciprocal(out=den[:, :], in_=den[:, :]), reads=[den], writes=[den])
                    k.ts('dve', gateA[:, t, :], tq[:, :], den[:, 0:1], 2.5, ALU.mult, ALU.mult, R=[tq, den], W=[gateA])
                    k.dma(h2tok_d[r0:r0 + 128, :], h2b[:, :], R=[h2b], W=[h2tok_dT[t]])
                zero_some(1000)

            with Phase(k):
                triu = load_f32(k, "triu", triu_d, [128, 128]); e127 = load_f32(k, "e127", e127_d, [128, 128])
                CUM = k.ps("CUM", [128, 2048]); TOT = k.ps("TOT", [128, 2048])
                cumA = k.sb("cumA", [128, 32, 64]); totA = k.sb("totA", [128, 32, 64]); base = k.sb("base", [128, 32, 64])
                for i in range(32):
                    k.mm(CUM[:, i * 64:(i + 1) * 64], triu[:, :], selA[:, i, :], True, True, R=[triu, selA], W=[CUM])
                k.cp('dve', cumA[:, :, :], CUM[:, :].rearrange("p (a b) -> p a b", a=32), R=[CUM], W=[cumA])
                cflat = cumA[:, :, :].rearrange("p a b -> p (a b)")
                for c in range(4):
                    k.mm(TOT[:, c * 512:(c + 1) * 512], e127[:, :], cflat[:, c * 512:(c + 1) * 512], True, True, R=[e127, cumA], W=[TOT])
                k.act(totA[:, :, :], TOT[:, :].rearrange("p (a b) -> p a b", a=32), AF.Copy, R=[TOT], W=[totA])
                k.memset('dve', base, base[:, 0, :], 0.0)
                for i in range(1, 32):
                    k.tt('dve', base[:, i, :], base[:, i - 1, :], totA[:, i - 1, :], ALU.add, R=[base, totA], W=[base])
                cnt = k.sb("cnt", [128, 64]); nbf = k.sb("nbf", [128, 64]); nbi = k.sb("nbi", [128, 64], I32)
                ones64 = k.sb("ones64", [128, 64]); pend = k.sb("pend", [128, 64]); pstart = k.sb("pstart", [128, 64])
                k.memset('pool', ones64, ones64[:, :], 1.0)
                k.tt('dve', cnt[:, :], base[:, 31, :], totA[:, 31, :], ALU.add, R=[base, totA], W=[cnt])
                k.ts('dve', nbf[:, :], cnt[:, :], 1.0 / 256, 255.0 / 256 - 0.5 + 1.0 / 512, ALU.mult, ALU.add, R=[cnt], W=[nbf])
                k.cp('dve', nbi[:, :], nbf[:, :], R=[nbf], W=[nbi])
                k.cp('dve', nbf[:, :], nbi[:, :], R=[nbi], W=[nbf])
                k.ts('dve', nbf[:, :], nbf[:, :], 256.0, None, ALU.mult, None, R=[nbf], W=[nbf])
                k.op('dve', lambda e: e.tensor_tensor_scan(out=pend[:, :], data0=ones64[:, :], data1=nbf[:, :], initial=0.0,
                                                           op0=ALU.mult, op1=ALU.add), reads=[ones64, nbf], writes=[pend])
                k.tt('dve', pstart[:, :], pend[:, :], nbf[:, :], ALU.subtract, R=[pend, nbf], W=[pstart])
                k.tt('dve', cumA[:, :, :], cumA[:, :, :], base[:, :, :], ALU.add, R=[cumA, base], W=[cumA])
                k.tt('dve', cumA[:, :, :], cumA[:, :, :], bview(pstart[:, None, :], [128, 32, 64]), ALU.add, R=[cumA, pstart], W=[cumA])
                k.tt('dve', cumA[:, :, :], cumA[:, :, :], selA[:, :, :], ALU.mult, R=[cumA, selA], W=[cumA])
                cmp = k.sb("cmp", [128, 192, 64]); bef = k.sb("bef", [128, 192]); pcol = load_f32(k, "pcol", pcol_d, [128, 1])
                thrb = k.sb("thrb", [128, 192]); k.dma(thrb[:, :], thr_d.broadcast_to([128, 192]), W=[thrb])
                k.tt('dve', cmp[:, :, :], bview(pend[:, None, :], [128, 192, 64]), bview(thrb[:, :, None], [128, 192, 64]), ALU.is_le,
                     R=[pend, thrb], W=[cmp])
                k.op('dve', lambda e: e.reduce_sum(out=bef[:, :], in_=cmp[:, :, :], axis=AX.X), reads=[cmp], writes=[bef])
                k.ts('dve', bef[:, :], bef[:, :], 63.0, 128.0, ALU.min, ALU.mult, R=[bef], W=[bef])
                k.ts('dve', bef[:, :], bef[:, :], pcol[:, 0:1], None, ALU.add, None, R=[bef, pcol], W=[bef])
                k.cp('dve', be_i[:, :], bef[:, :], R=[bef], W=[be_i])
                t8 = k.ring("t8", 2, [128, 8]); eqg = k.ring("eqg", 2, [128, 64]); hk = k.ring("hk", 3, [128, 1024], BF16)
                for i in range(32):
                    t8_ = t8[i % 2]; hk_ = hk[i % 3]
                    k.dma(hk_[:, :], h2tok_d[i * 128:(i + 1) * 128, :], R=[h2tok_dT[i]], W=[hk_])
                    k.op('dve', lambda e, t8_=t8_, i=i: e.max(out=t8_[:, :], in_=cumA[:, i, :]), reads=[cumA], writes=[t8_])
                    k.ts('dve', dest8A[:, i, :], t8_[:, :], -1.0, None, ALU.add, None, R=[t8_], W=[dest8A])
                    for kk in range(8):
                        eq_ = eqg[kk % 2]
                        k.stt('dve', eq_[:, :], cumA[:, i, :], t8_[:, kk:kk + 1], gateA[:, i, :], ALU.is_equal, ALU.mult,
                              R=[cumA, t8_, gateA], W=[eq_])
                        k.op('dve', lambda e, eq_=eq_, i=i, kk=kk: e.reduce_sum(out=w8A[:, i, kk:kk + 1], in_=eq_[:, :], axis=AX.X),
                             reads=[eq_], writes=[w8A])
                    for kk in range(8):
                        k.op('pool', lambda e, i=i, kk=kk, hk_=hk_: e.indirect_dma_start(
                            out=xg_d[:, :], out_offset=bass.IndirectOffsetOnAxis(ap=dest8A[:, i, kk:kk + 1], axis=0),
                            in_=hk_[:, :], in_offset=None, bounds_check=bcreg, oob_is_err=False),
                            reads=[hk_, dest8A], writes=[xgT], dma=True)

        with Phase(k):
            identb = k.sb("identb", [128, 128], BF16); k.dma(identb[:, :], ident_d, W=[identb], q='pool')
            W1r = k.ring("W1", 4, [128, 8, 256], BF16); W3r = k.ring("W3", 4, [128, 8, 256], BF16); W2r = k.ring("W2", 4, [128, 2, 1024], BF16)
            xblk = k.ring("xblk", 3, [128, 2, 1024], BF16); XTr = k.ring("XT", 2, [128, 8, 256], BF16)
            sr = k.ring("sr", 2, [128, 512]); hbr = k.ring("hbr", 2, [128, 2, 256], BF16); yblk = k.ring("yblk", 2, [128, 2, 1024], BF16)
            XTp = [k.ps("XTp", [128, 1024]) for _ in range(1)]
            H13 = k.ps("H13", [128, 1024]); OP = [k.ps("OP", [128, 1024]) for _ in range(2)]
            for j in range(192):
                W1 = W1r[j % 4]; W3 = W3r[j % 4]; W2 = W2r[j % 4]; xb_ = xblk[j % 3]; XT = XTr[j % 2]
                s_ = sr[j % 2]; hb_ = hbr[j % 2]; yb_ = yblk[j % 2]
                for (Wt, wsrc, wT_) in (((W1, wb_d[0], wbT[0]), (W3, wb_d[1], wbT[1]), (W2, wb_d[2], wbT[2])) if USE_CONV else ((W1, w1, wbT[0]), (W3, w3, wbT[1]), (W2, w2, wbT[2]))):
                    k.op('pool', lambda e, Wt=Wt, wsrc=wsrc, j=j: e.indirect_dma_start(
                        out=Wt[:, :, :].rearrange("p a b -> p (a b)"), out_offset=None, in_=wsrc[:, :],
                        in_offset=bass.IndirectOffsetOnAxis(ap=be_i[:, j:j + 1], axis=0),
                        bounds_check=bcreg2, oob_is_err=False), reads=[be_i, wT_], writes=[Wt], dma=True)
                k.dma(xb_[:, :, :], xg_d[j * 256:(j + 1) * 256, :].rearrange("(a p) c -> p a c", p=128), R=[xgT], W=[xb_])
                for a in range(2):
                    P_ = XTp[0]
                    for kt in range(8):
                        k.mm(P_[:, kt * 128:(kt + 1) * 128], xb_[:, a, kt * 128:(kt + 1) * 128], identb[:, :], True, True, R=[xb_, identb], W=[P_])
                    if a == 0:
                        k.act(XT[:, :, 0:128], P_[:, :].rearrange("p (a b) -> p a b", a=8), AF.Copy, R=[P_], W=[XT])
                    else:
                        k.cp('dve', XT[:, :, 128:256], P_[:, :].rearrange("p (a b) -> p a b", a=8), R=[P_], W=[XT])
                for (Wx, off) in ((W1, 0), (W3, 512)):
                    for mt in range(2):
                        for kt in range(8):
                            k.mm(H13[:, off + mt * 256: off + (mt + 1) * 256], Wx[:, kt, mt * 128:(mt + 1) * 128], XT[:, kt, :], kt == 0, kt == 7,
                                 R=[Wx, XT], W=[H13])
                k.act(s_[:, :], H13[:, 0:512], AF.Silu, R=[H13], W=[s_])
                k.tt('dve', hb_[:, :, :], s_[:, :].rearrange("p (a b) -> p a b", a=2), H13[:, 512:1024].rearrange("p (a b) -> p a b", a=2),
                     ALU.mult, R=[s_, H13], W=[hb_])
                for a in range(2):
                    O_ = OP[a]
                    for hf in range(2):
                        cs = slice(hf * 512, (hf + 1) * 512)
                        for ft in range(2):
                            k.mm(O_[:, cs], hb_[:, ft, a * 128:(a + 1) * 128], W2[:, ft, cs], ft == 0, ft == 1, R=[hb_, W2], W=[O_])
                    if a == 0:
                        k.act(yb_[:, 0, :], O_[:, :], AF.Copy, R=[O_], W=[yb_])
                    else:
                        k.cp('dve', yb_[:, 1, :], O_[:, :], R=[O_], W=[yb_])
                k.dma(yg_d[j * 256:(j + 1) * 256, :].rearrange("(a p) c -> p a c", p=128), yb_[:, :, :], R=[yb_], W=[ygT])

        with Phase(k):
            g2r = k.sb("g2r", [128, 1024]); k.dma(g2r[:, :], modr[5], R=[modrT[5]], W=[g2r])
            fg = load_f32(k, "fng", fng, [128, 1024])
            W1s = k.sb("W1s", [128, 8, 256], BF16); W3s = k.sb("W3s", [128, 8, 256], BF16); W2s = k.sb("W2s", [128, 2, 1024], BF16)
            for (Wt, wsrc) in ((W1s, w1), (W3s, w3), (W2s, w2)):
                k.dma(Wt[:, :, :].rearrange("p a b -> p (a b)"), wsrc[64 * 128:65 * 128, :], W=[Wt], q='pool')
            H1 = k.ps("H1", [128, 1024]); H3 = k.ps("H3", [128, 1024]); OPs = [k.ps("OPs", [128, 1024]) for _ in range(2)]
            h2s = k.ring("h2s", 2, [128, 8, 512], BF16)
            sl = k.ring("sl", 2, [128, 1024]); hg = k.ring("hg", 2, [128, 2, 512], BF16)
            acc = k.ring("acc", 2, [128, 1024]); Yk = k.ring("Yk", 6, [128, 1024], BF16)
            x3 = k.ring("x3", 2, [128, 1024]); junk = k.sb("junk4", [128, 1024], BF16); ssf = k.ring("ssf", 2, [128, 1])
            yi = 0
            for g4 in range(8):
                h2_ = h2s[g4 % 2]; s_ = sl[g4 % 2]; hg_ = hg[g4 % 2]
                k.dma(h2_[:, :, :], h2T_d[:, :, g4 * 512:(g4 + 1) * 512], R=h2T_dT, W=[h2_])
                for (Wx, Hx) in ((W1s, H1), (W3s, H3)):
                    for mt in range(2):
                        for kt in range(8):
                            k.mm(Hx[:, mt * 512:(mt + 1) * 512], Wx[:, kt, mt * 128:(mt + 1) * 128], h2_[:, kt, :], kt == 0, kt == 7,
                                 R=[Wx, h2_], W=[Hx])
                k.act(s_[:, :], H1[:, :], AF.Silu, R=[H1], W=[s_])
                k.tt('dve', hg_[:, :, :], s_[:, :].rearrange("p (a b) -> p a b", a=2), H3[:, :].rearrange("p (a b) -> p a b", a=2), ALU.mult,
                     R=[s_, H3], W=[hg_])
                for tt_ in range(4):
                    t = g4 * 4 + tt_
                    r0 = t * 128
                    O_ = OPs[t % 2]; ac = acc[t % 2]; x_ = x3[t % 2]; s_f = ssf[t % 2]
                    for hf in range(2):
                        cs = slice(hf * 512, (hf + 1) * 512)
                        for ft in range(2):
                            k.mm(O_[:, cs], hg_[:, ft, tt_ * 128:(tt_ + 1) * 128], W2s[:, ft, cs], ft == 0, ft == 1, R=[hg_, W2s], W=[O_])
                    k.act(ac[:, :], O_[:, :], AF.Copy, R=[O_], W=[ac])
                    for kk in range(8):
                        y_ = Yk[yi % 6]; yi += 1
                        k.op('pool', lambda e, y_=y_, t=t, kk=kk: e.indirect_dma_start(
                            out=y_[:, :], out_offset=None, in_=yg_d[:, :],
                            in_offset=bass.IndirectOffsetOnAxis(ap=dest8A[:, t, kk:kk + 1], axis=0),
                            bounds_check=bcreg, oob_is_err=False), reads=[ygT, dest8A], writes=[y_], dma=True)
                        k.stt('dve', ac[:, :], y_[:, :], w8A[:, t, kk:kk + 1], ac[:, :], ALU.mult, ALU.add, R=[y_, w8A, ac], W=[ac])
                    k.dma(x_[:, :], x2_d[r0:r0 + 128, :], R=[x2_dT[t]], W=[x_])
                    k.tt('dve', ac[:, :], ac[:, :], g2r[:, :], ALU.mult, R=[ac, g2r], W=[ac])
                    k.tt('pool', x_[:, :], x_[:, :], ac[:, :], ALU.add, R=[x_, ac], W=[x_])
                    k.act(junk[:, :], x_[:, :], AF.Square, R=[x_], W=[junk, s_f], accum_out=s_f[:, 0:1])
                    k.ts('dve', s_f[:, 0:1], s_f[:, 0:1], 1.0 / 1024, EPS, ALU.mult, ALU.add, R=[s_f], W=[s_f])
                    k.act(s_f[:, 0:1], s_f[:, 0:1], AF.Sqrt, R=[s_f], W=[s_f])
                    k.op('dve', lambda e, s_f=s_f: e.reciprocal(out=s_f[:, 0:1], in_=s_f[:, 0:1]), reads=[s_f], writes=[s_f])
                    k.stt('dve', x_[:, :], x_[:, :], s_f[:, 0:1], fg[:, :], ALU.mult, ALU.mult, R=[x_, s_f, fg], W=[x_])
                    k.dma(out[r0:r0 + 128, :], x_[:, :], R=[x_], W=[])
        k.finish()
        print("kernel build: ninst=%d nsem=%d" % (k.ninst, k.nsem))
    return nc


def _hg_mats():
    M = np.zeros((2, 3, 128, 128), np.float32)
    s = np.arange(128)[:, None]; t = np.arange(128)[None, :]
    same = (s // 64) == (t // 64)
    for d in range(2):
        if d == 0:
            inc = same & (s <= t); suf = same & (s > t); ref = (t // 64) * 64 + 32
        else:
            inc = same & (s >= t); suf = same & (s < t); ref = (t // 64) * 64 + 31
        inc = inc.astype(np.float32)
        M[d, 0] = inc
        M[d, 1] = inc - inc[np.arange(128)[:, None], ref]
        M[d, 2] = suf.astype(np.float32)
    return M


def _prep_core(inp, b, kf):
    f = np.float32
    rv = (lambda a: a[::-1]) if kf else (lambda a: a)
    d = {}
    d["xf"] = np.ascontiguousarray(rv(inp["x"][b]), f)
    d["ctxf"] = np.ascontiguousarray(rv(inp["ctx"][b]), f)
    rep = lambda v: np.ascontiguousarray(np.broadcast_to(v.reshape(8, 128).T[:, :, None], (128, 8, 128)), f)
    d["crep"] = rep(inp["c"][b]); d["ccrep"] = rep(inp["c_ctx"])
    d["w_ada"] = np.ascontiguousarray(inp["w_ada"][0], f); d["b_ada"] = np.ascontiguousarray(inp["b_ada"][0][None, :], f)
    rows = lambda v, n=128: np.ascontiguousarray(np.broadcast_to(v[None, :], (n, v.shape[0])), f)
    d["n1g"] = rows(inp["norm1_g"][0]); d["n2g"] = rows(inp["norm2_g"][0]); d["fng"] = rows(inp["final_norm_g"])
    w = inp["w_in"][0]
    sl = {"u": (0, 256), "q": (256, 1024), "ff": (1024, 1792), "fb": (1792, 2560), "i": (2560, 3328), "go": (3328, 4096),
          "ga": (4096, 5120), "gb": (5120, 6144)}
    cut = lambda n: np.ascontiguousarray(w[:, sl[n][0]:sl[n][1]], f)
    d["w_u"] = cut("u"); d["w_q"] = cut("q"); d["w_i"] = cut("i"); d["w_go"] = cut("go"); d["w_ga"] = cut("ga"); d["w_gb"] = cut("gb")
    d["w_fF"] = cut("fb" if kf else "ff"); d["w_fB"] = cut("ff" if kf else "fb")
    dirs = [1, 0] if kf else [0, 1]
    lam_re = inp["s5_lam_re"][0][dirs]; lam_im = inp["s5_lam_im"][0][dirs]; logdt = inp["s5_log_dt"][0][dirs]
    bre = inp["s5_b_re"][0][dirs]; bim = inp["s5_b_im"][0][dirs]
    A = lambda v: np.ascontiguousarray(np.concatenate([v.reshape(32, 64).T] * 2, axis=0), f)
    d["lamA_re"] = A(lam_re); d["lamA_im"] = A(lam_im)
    d["logdtA"] = np.ascontiguousarray(np.broadcast_to(logdt.reshape(1, 32), (128, 32)), f)
    Bb = lambda v: np.ascontiguousarray(np.broadcast_to(v.reshape(1, 32, 64), (128, 32, 64)), f)
    d["lamB_re"] = Bb(lam_re); d["lamB_im"] = Bb(lam_im)
    d["logdtB"] = np.ascontiguousarray(np.broadcast_to(logdt.reshape(1, 32, 1), (128, 32, 64)), f)
    brp = np.zeros((128, 32, 64), f); bip = np.zeros((128, 32, 64), f)
    for fd in range(2):
        for g in range(16):
            r0 = (g % 8) * 16
            brp[r0:r0 + 16, fd * 16 + g, :] = bre[fd, g].T
            bip[r0:r0 + 16, fd * 16 + g, :] = bim[fd, g].T
    d["Bre_pad"] = brp; d["Bim_pad"] = bip
    cre = inp["s5_c_re"][0]; cim = inp["s5_c_im"][0]
    ca = np.zeros((128, 16, 128), f); cb = np.zeros((128, 16, 128), f)
    for g in range(16):
        c0 = (g % 8) * 16
        ca[0:64, g, c0:c0 + 16] = cre[g].T; ca[64:128, g, c0:c0 + 16] = cim[g].T
        cb[0:64, g, c0:c0 + 16] = cim[g].T; cb[64:128, g, c0:c0 + 16] = cre[g].T
    d["CA"] = ca; d["CB"] = cb
    d["dcol"] = np.ascontiguousarray(inp["s5_d"][0].reshape(2, 128).T, f)
    sg = np.ones((128, 1), f); sg[64:] = -1; d["sgn"] = sg
    sw = np.zeros((128, 128), f); sw[np.arange(128), (np.arange(128) + 64) % 128] = 1; d["swapm"] = sw
    d["iota256"] = np.ascontiguousarray(np.broadcast_to(np.arange(1, 257, dtype=f)[None, :], (128, 256)), f)
    d["lbl"] = np.ascontiguousarray(np.broadcast_to(inp["hg_lb_logits"][None, :, :], (128, 2, 768)), f)
    d["hgg6"] = rows(np.tile(inp["hg_norm_g"][0], 6))
    d["hgM"] = _hg_mats()
    ci = np.zeros((128, 2), f); ci[:64, 0] = 1; ci[64:, 1] = 1; d["cind"] = ci
    d["wglu"] = np.ascontiguousarray(inp["s5_w_glu"][0], f); d["p_a"] = np.ascontiguousarray(inp["p_a"][0], f)
    d["p_b"] = np.ascontiguousarray(inp["p_b"][0], f); d["w_out"] = np.ascontiguousarray(inp["w_out"][0], f)
    d["w_r"] = np.ascontiguousarray(inp["moe_w_router"][0], f); d["b_r"] = rows(inp["moe_b_router"][0])
    d["ident"] = np.eye(128, dtype=f)
    d["triu"] = np.triu(np.ones((128, 128), f))
    e127 = np.zeros((128, 128), f); e127[127, :] = 1; d["e127"] = e127
    d["thr"] = (np.arange(192, dtype=f) * 256)[None, :]
    d["pcol"] = np.arange(128, dtype=f)[:, None]
    return d


_SHARED = {}


def kernel(**inp):
    inp = {k_: np.asarray(v) for k_, v in inp.items()}
    f = np.float32
    def relay(w, ws):
        a = np.concatenate([w, ws[None]], 0)
        e_, r_, n_ = a.shape
        return np.ascontiguousarray(a.reshape(e_, r_ // 128, 128, n_).transpose(0, 2, 1, 3).reshape(e_ * 128, (r_ // 128) * n_), f)
    w1 = relay(inp["moe_w1"][0], inp["moe_ws1"][0])
    w3 = relay(inp["moe_w3"][0], inp["moe_ws3"][0])
    w2 = relay(inp["moe_w2"][0], inp["moe_ws2"][0])
    in_maps = []
    for core in range(8):
        b, kf = core // 2, core % 2
        d = _prep_core(inp, b, kf)
        d["w1"] = w1; d["w3"] = w3; d["w2"] = w2
        in_maps.append(d)
    nc = build(DEBUG)
    res = run_bass_kernel_spmd(nc, in_maps, core_ids=list(range(8)))
    outp = np.zeros((4, 8192, 1024), f)
    for core in range(8):
        b, kf = core // 2, core % 2
        o = res.results[core]["out"]
        if kf == 0:
            outp[b, :4096] = o
        else:
            outp[b, 4096:] = o[::-1]
    if DEBUG:
        _SHARED["res"] = res
    return outp
```

```python
import numpy as np
from contextlib import ExitStack
import concourse.bass as bass
import concourse.mybir as mybir
from concourse.bass_utils import run_bass_kernel_spmd

F32 = mybir.dt.float32
BF16 = mybir.dt.bfloat16
I32 = mybir.dt.int32
AF = mybir.ActivationFunctionType
ALU = mybir.AluOpType
AX = mybir.AxisListType

PI = float(np.pi)
TWO_PI = float(2 * np.pi)
EPS = 1e-6
DEBUG = False
USE_CONV = False
SK = 4


class TT:
    def __init__(s, h):
        s.h = h
        s.w = None
        s.r = {}

    def __getitem__(s, k):
        return s.h[k]


class KB:
    def __init__(s, nc):
        s.nc = nc
        s.gstack = ExitStack()
        s.stack = s.gstack
        s.eng = {'pe': nc.tensor, 'dve': nc.vector, 'act': nc.scalar, 'pool': nc.gpsimd, 'sp': nc.sync}
        s.csem = {}
        s.ccnt = {}
        s.seen = {e: {} for e in s.eng}
        s.dsem = {}
        s.dpos = {}
        s.nsem = 0
        s.ninst = 0
        s.uid = 0
        s.bsem = None
        s.bcnt = 0

    def newsem(s):
        s.nsem += 1
        return s.gstack.enter_context(s.nc.semaphore("s%d" % s.nsem))

    def nm(s, name):
        s.uid += 1
        return "%s_%d" % (name, s.uid)

    def sb(s, name, shape, dt=F32):
        return TT(s.stack.enter_context(s.nc.sbuf_tensor(s.nm(name), list(shape), dt)))

    def ps(s, name, shape, dt=F32):
        return TT(s.stack.enter_context(s.nc.psum_tensor(s.nm(name), list(shape), dt)))

    def ring(s, name, n, shape, dt=F32):
        return [s.sb(name, shape, dt) for _ in range(n)]

    def views(s, t, n):
        return [TT(t.h) for _ in range(n)]

    def _wait(s, e, sem, val):
        key = id(sem)
        if s.seen[e].get(key, 0) >= val:
            return
        s.seen[e][key] = val
        s.eng[e].wait_ge(sem, val)
        s.ninst += 1

    def op(s, e, fn, reads=(), writes=(), dma=False):
        deps = []
        for t in reads:
            if t.w is not None:
                deps.append(t.w)
        for t in writes:
            if t.w is not None:
                deps.append(t.w)
            deps.extend(t.r.values())
        if dma:
            q = s.dsem.setdefault(e, [])
            if len(q) < 16:
                q.append([s.newsem(), 0])
                slot = q[-1]
            else:
                p = s.dpos.get(e, 0)
                slot = q[p]
                s.dpos[e] = (p + 1) % 16
            sem = slot[0]
            if slot[1] > 0:
                s._wait(e, sem, slot[1])
            slot[1] += 16
            val = slot[1]
            inc = 16
        else:
            if e not in s.csem or s.ccnt[e] >= 20000:
                s.csem[e] = s.newsem()
                s.ccnt[e] = 0
            sem = s.csem[e]
            s.ccnt[e] += 1
            val = s.ccnt[e]
            inc = 1
        for (dsem, dval, deng) in deps:
            if deng == e and e == 'pe' and not dma:
                continue
            s._wait(e, dsem, dval)
        ins = fn(s.eng[e])
        ins.then_inc(sem, inc)
        s.ninst += 1
        tag = (sem, val, None if dma else e)
        for t in writes:
            t.w = tag
            t.r = {}
        for t in reads:
            t.r[id(sem)] = tag
        return tag

    def outstanding(s):
        tags = []
        for e, sem in s.csem.items():
            tags.append((sem, s.ccnt[e]))
        for e, q in s.dsem.items():
            for slot in q:
                if slot[1] > 0:
                    tags.append((slot[0], slot[1]))
        return tags

    def barrier(s):
        if s.bsem is None:
            s.bsem = s.newsem()
        for (sem, val) in s.outstanding():
            s._wait('sp', sem, val)
        s.bcnt += 1
        s.eng['sp'].nop().then_inc(s.bsem, 1)
        for e in ('pe', 'dve', 'act', 'pool'):
            s.eng[e].wait_ge(s.bsem, s.bcnt)
        s.ninst += 5

    def finish(s):
        for (sem, val) in s.outstanding():
            s._wait('sp', sem, val)

    def dma(s, out, in_, R=(), W=(), q='sp', **kw):
        return s.op(q, lambda e: e.dma_start(out=out, in_=in_, **kw), reads=R, writes=W, dma=True)

    def mm(s, out, lhsT, rhs, start, stop, R, W):
        return s.op('pe', lambda e: e.matmul(out, lhsT=lhsT, rhs=rhs, start=start, stop=stop), reads=R, writes=W)

    def act(s, out, in_, func, R, W, **kw):
        return s.op('act', lambda e: e.activation(out=out, in_=in_, func=func, **kw), reads=R, writes=W)

    def tt(s, e, out, in0, in1, op, R, W):
        return s.op(e, lambda g: g.tensor_tensor(out=out, in0=in0, in1=in1, op=op), reads=R, writes=W)

    def ts(s, e, out, in0, s1, s2, op0, op1, R, W):
        if s2 is None:
            return s.op(e, lambda g: g.tensor_scalar(out=out, in0=in0, scalar1=s1, scalar2=None, op0=op0), reads=R, writes=W)
        return s.op(e, lambda g: g.tensor_scalar(out=out, in0=in0, scalar1=s1, scalar2=s2, op0=op0, op1=op1), reads=R, writes=W)

    def stt(s, e, out, in0, scalar, in1, op0, op1, R, W):
        return s.op(e, lambda g: g.scalar_tensor_tensor(out=out, in0=in0, scalar=scalar, in1=in1, op0=op0, op1=op1), reads=R, writes=W)

    def cp(s, e, out, in_, R, W):
        return s.op(e, lambda g: g.tensor_copy(out=out, in_=in_), reads=R, writes=W)

    def memset(s, e, t, ap, val):
        return s.op(e, lambda g: g.memset(ap, val), writes=[t])


class Phase:
    def __init__(s, k):
        s.k = k

    def __enter__(s):
        s.prev = s.k.stack
        s.st = ExitStack()
        s.k.stack = s.st
        return s

    def __exit__(s, *a):
        s.k.barrier()
        s.k.stack = s.prev
        s.st.close()
        return False


def bview(ap, shape):
    return ap.broadcast_to(list(shape))


def load_bf16(k, name, w_ap, kt, n, q='pool'):
    t = k.sb(name, [128, kt, n], BF16)
    k.dma(t[:, :, :], w_ap.rearrange("(kt p) n -> p kt n", p=128), W=[t], q=q)
    return t


def load_f32(k, name, ap, shape, q='sp'):
    t = k.sb(name, shape, F32)
    if len(shape) == 2:
        k.dma(t[:, :], ap, W=[t], q=q)
    else:
        k.dma(t[:, :, :], ap, W=[t], q=q)
    return t


class Producer:
    def __init__(s, k, identb, pT, nbuf=2):
        s.k = k
        s.identb = identb
        s.pT = pT
        s.xt = k.ring("xt", nbuf, [128, 1024])
        s.junk = k.sb("junk", [128, 1024], BF16)
        s.hb = k.ring("hb", 2, [128, 1024], BF16)
        s.ss = k.ring("ss", 2, [128, 1])
        s.i = 0

    def rstd(s, xt, ss):
        k = s.k
        k.act(s.junk[:, :], xt[:, :], AF.Square, R=[xt], W=[s.junk, ss], accum_out=ss[:, 0:1])
        k.ts('dve', ss[:, 0:1], ss[:, 0:1], 1.0 / 1024, EPS, ALU.mult, ALU.add, R=[ss], W=[ss])
        k.act(ss[:, 0:1], ss[:, 0:1], AF.Sqrt, R=[ss], W=[ss])
        k.op('dve', lambda e: e.reciprocal(out=ss[:, 0:1], in_=ss[:, 0:1]), reads=[ss], writes=[ss])

    def make(s, x_ap, scale, shift, hT, xdep=()):
        k = s.k
        i = s.i
        s.i += 1
        xt = s.xt[i % len(s.xt)]
        hb = s.hb[i % 2]
        ss = s.ss[i % 2]
        k.dma(xt[:, :], x_ap, R=list(xdep), W=[xt])
        s.rstd(xt, ss)
        k.stt('dve', xt[:, :], xt[:, :], ss[:, 0:1], scale[:, :], ALU.mult, ALU.mult, R=[xt, ss, scale], W=[xt])
        k.tt('pool', hb[:, :], xt[:, :], shift[:, :], ALU.add, R=[xt, shift], W=[hb])
        s.transpose(hb, hT)
        return xt

    def transpose(s, hb, hT):
        k = s.k
        for kt in range(8):
            k.mm(s.pT[:, kt * 128:(kt + 1) * 128], hb[:, kt * 128:(kt + 1) * 128], s.identb[:, :], True, True,
                 R=[hb, s.identb], W=[s.pT])
        k.act(hT[:, :, :], s.pT[:, :].rearrange("p (a b) -> p a b", a=8), AF.Copy, R=[s.pT], W=[hT])


def sincos(k, arg, shape, sin_out, cos_out, tmp, tmpi, R_extra=()):
    def full(t):
        return t[:, :] if len(shape) == 2 else t[:, :, :]
    a = full(arg)
    k.ts('dve', full(tmp), a, 1.0 / TWO_PI, None, ALU.mult, None, R=[arg], W=[tmp])
    k.cp('dve', full(tmpi), full(tmp), R=[tmp], W=[tmpi])
    k.cp('dve', full(tmp), full(tmpi), R=[tmpi], W=[tmp])
    k.stt('dve', full(tmp), full(tmp), -TWO_PI, a, ALU.mult, ALU.add, R=[tmp, arg], W=[tmp])
    k.ts('dve', full(tmp), full(tmp), PI, -PI, ALU.min, ALU.max, R=[tmp], W=[tmp])
    so, sT = sin_out
    co, cT = cos_out
    k.act(so, full(tmp), AF.Sin, R=[tmp], W=[sT])
    k.act(full(tmp), full(tmp), AF.Abs, R=[tmp], W=[tmp])
    k.act(co, full(tmp), AF.Sin, R=[tmp], W=[cT], scale=-1.0, bias=PI / 2)


def build(dbg=False):
    nc = bass.Bass("TRN2", target_bir_lowering=False)

    def din(name, shape, dt=F32):
        return nc.dram_tensor(name, list(shape), dt, kind="ExternalInput").ap()

    def dscr(name, shape, dt=F32):
        if dbg:
            return nc.dram_tensor(name, list(shape), dt, kind="ExternalOutput").ap()
        return nc.dram_tensor(name, list(shape), dt).ap()

    xf = din("xf", [8192, 1024]); ctxf = din("ctxf", [256, 1024])
    crep = din("crep", [128, 8, 128]); ccrep = din("ccrep", [128, 8, 128])
    w_ada = din("w_ada", [1024, 6144]); b_ada = din("b_ada", [1, 6144])
    n1g = din("n1g", [128, 1024]); n2g = din("n2g", [128, 1024]); fng = din("fng", [128, 1024])
    w_u = din("w_u", [1024, 256]); w_q = din("w_q", [1024, 768]); w_fF = din("w_fF", [1024, 768])
    w_fB = din("w_fB", [1024, 768]); w_i = din("w_i", [1024, 768]); w_go = din("w_go", [1024, 768])
    w_ga = din("w_ga", [1024, 1024]); w_gb = din("w_gb", [1024, 1024])
    lamA_re = din("lamA_re", [128, 32]); lamA_im = din("lamA_im", [128, 32]); logdtA = din("logdtA", [128, 32])
    lamB_re = din("lamB_re", [128, 32, 64]); lamB_im = din("lamB_im", [128, 32, 64]); logdtB = din("logdtB", [128, 32, 64])
    Bre_pad = din("Bre_pad", [128, 32, 64]); Bim_pad = din("Bim_pad", [128, 32, 64])
    CA = din("CA", [128, 16, 128]); CB = din("CB", [128, 16, 128])
    dcol_d = din("dcol", [128, 2]); sgn_d = din("sgn", [128, 1]); swapm_d = din("swapm", [128, 128])
    iota_d = din("iota256", [128, 256])
    lbl = din("lbl", [128, 2, 768]); hgg6 = din("hgg6", [128, 768])
    hgM = din("hgM", [2, 3, 128, 128])
    cind_d = din("cind", [128, 2])
    wglu = din("wglu", [256, 256]); p_a = din("p_a", [256, 1024]); p_b = din("p_b", [768, 1024])
    w_out = din("w_out", [1024, 1024])
    w_r = din("w_r", [1024, 64]); b_r = din("b_r", [128, 64])
    w1 = din("w1", [65 * 128, 2048]); w3 = din("w3", [65 * 128, 2048]); w2 = din("w2", [65 * 128, 2048])
    pcol_d = din("pcol", [128, 1])
    ident_d = din("ident", [128, 128])
    triu_d = din("triu", [128, 128]); e127_d = din("e127", [128, 128]); thr_d = din("thr", [1, 192])
    out = nc.dram_tensor("out", [4096, 1024], F32, kind="ExternalOutput").ap()

    modr = dscr("modr", [8, 128, 1024])
    yT_d = dscr("yT_d", [128, 2, 4096], BF16)
    oF_d = dscr("oF_d", [4096, 768])
    on_d = dscr("on_d", [4096, 768])
    x2_d = dscr("x2_d", [4096, 1024])
    h2T_d = dscr("h2T_d", [128, 8, 4096], BF16)
    h2tok_d = dscr("h2tok_d", [4096, 1024], BF16)
    NROW = 192 * 256
    xg_d = nc.dram_tensor("xg_d", [NROW, 1024], BF16).ap()
    yg_d = nc.dram_tensor("yg_d", [NROW, 1024], BF16).ap()

    k = KB(nc)
    with k.gstack:
        modrT = [TT(modr) for _ in range(8)]
        yT_dT = [TT(yT_d) for _ in range(32)]
        oF_dT = [TT(oF_d) for _ in range(64)]
        on_dT = [TT(on_d) for _ in range(64)]
        x2_dT = [TT(x2_d) for _ in range(32)]
        h2T_dT = [TT(h2T_d) for _ in range(32)]
        h2tok_dT = [TT(h2tok_d) for _ in range(32)]
        xgT = TT(xg_d); ygT = TT(yg_d)
        dest8A = TT(k.gstack.enter_context(nc.sbuf_tensor("dest8A", [128, 32, 8], I32)))
        w8A = TT(k.gstack.enter_context(nc.sbuf_tensor("w8A", [128, 32, 8], F32)))
        be_i = TT(k.gstack.enter_context(nc.sbuf_tensor("be_i", [128, 192], I32)))
        zt = TT(k.gstack.enter_context(nc.sbuf_tensor("zt", [128, 2048], BF16)))
        k.memset('pool', zt, zt[:, :], 0.0)
        zinit = [0]
        bcreg = k.gstack.enter_context(nc.gpsimd.register("bcreg"))
        nc.gpsimd.reg_mov(bcreg, NROW - 1)
        bcreg2 = k.gstack.enter_context(nc.gpsimd.register("bcreg2"))
        nc.gpsimd.reg_mov(bcreg2, (64 if USE_CONV else 65) * 128 - 1)
        wb_d = [nc.dram_tensor("w%db_d" % i_, [64 * 128, 2048], BF16).ap() for i_ in range(3)]
        wbT = [TT(wb_d[i_]) for i_ in range(3)]
        stg = [TT(k.gstack.enter_context(nc.sbuf_tensor("stg%d" % i_, [128, 2048], BF16))) for i_ in range(2)]
        cinit = [0]

        def conv_some(n):
            if not USE_CONV:
                return
            for _ in range(n):
                c = cinit[0]
                if c >= 192:
                    return
                cinit[0] += 1
                e_, wh = c // 3, c % 3
                st_ = stg[c % 2]
                k.dma(st_[:, :], (w1, w3, w2)[wh][e_ * 128:(e_ + 1) * 128, :], W=[st_], q='pool')
                k.dma(wb_d[wh][e_ * 128:(e_ + 1) * 128, :], st_[:, :], R=[st_], W=[wbT[wh]], q='act')

        def zero_some(n):
            for _ in range(n):
                c = zinit[0]
                if c >= NROW // 256:
                    return
                zinit[0] += 1
                k.dma(xg_d[c * 256:(c + 1) * 256, :].rearrange("(p a) c -> p (a c)", p=128), zt[:, :], R=[zt], W=[xgT], q='act')

        with Phase(k):
            sc_ = load_f32(k, "silc", crep, [128, 8, 128])
            scc = load_f32(k, "silcc", ccrep, [128, 8, 128])
            k.act(sc_[:, :, :], sc_[:, :, :], AF.Silu, R=[sc_], W=[sc_])
            k.act(scc[:, :, :], scc[:, :, :], AF.Silu, R=[scc], W=[scc])
            ba = k.sb("ba", [1, 6144]); k.dma(ba[:, :], b_ada, W=[ba])
            ones1 = k.sb("ones1", [1, 128]); k.memset('dve', ones1, ones1[:, :], 1.0)
            g1t = load_f32(k, "n1g", n1g, [128, 1024]); g2t = load_f32(k, "n2g", n2g, [128, 1024])
            wa = k.ring("wa", 2, [128, 8, 1024])
            psx = [k.ps("psx", [128, 1024]) for _ in range(2)]
            psc = [k.ps("psc", [128, 1024]) for _ in range(2)]
            res = k.ring("res", 3, [128, 1024])
            ri = 0
            for j in range(6):
                w_t = wa[j % 2]
                k.dma(w_t[:, :, :], w_ada[:, j * 1024:(j + 1) * 1024].rearrange("(kt p) n -> p kt n", p=128), W=[w_t])
                variants = [(sc_, psx[j % 2], j)]
                if j < 2:
                    variants.append((scc, psc[j % 2], 6 + j))
                for (lt, pt, dst) in variants:
                    for hf in range(2):
                        cs = slice(hf * 512, (hf + 1) * 512)
                        for kt in range(8):
                            k.mm(pt[:, cs], lt[:, kt, :], w_t[:, kt, cs], kt == 0, False, R=[lt, w_t], W=[pt])
                        k.mm(pt[:, cs], ones1[0:1, :], ba[0:1, j * 1024 + hf * 512: j * 1024 + (hf + 1) * 512], False, True,
                             R=[ones1, ba], W=[pt])
                    r_t = res[ri % 3]; ri += 1
                    if j == 1 or j == 4:
                        gt = g1t if j == 1 else g2t
                        k.stt('dve', r_t[:, :], pt[:, :], 1.0, gt[:, :], ALU.add, ALU.mult, R=[pt, gt], W=[r_t])
                    else:
                        k.cp('dve', r_t[:, :], pt[:, :], R=[pt], W=[r_t])
                    k.dma(modr[dst], r_t[:, :], R=[r_t], W=[modrT[dst]])

        with Phase(k):
            identb = k.sb("identb", [128, 128], BF16); k.dma(identb[:, :], ident_d, W=[identb], q='pool')
            uT = k.sb("uT", [128, 2, 8704], BF16)
            Bb1 = k.sb("Bb1", [128, 32, 128], BF16); Bb2 = k.sb("Bb2", [128, 32, 128], BF16)
            L1 = k.sb("L1", [128, 16, 128], BF16); L2 = k.sb("L2", [128, 16, 128], BF16)
            Rot = k.sb("Rot", [128, 32, 128])
            cosT = k.sb("cosT", [128, 32, 256], BF16); sinT = k.sb("sinT", [128, 32, 256], BF16)
            rdec = k.sb("rdec", [128, 32])
            dcol = load_f32(k, "dcol", dcol_d, [128, 2])
            with Phase(k):
                Wu = load_bf16(k, "Wu", w_u, 8, 256)
                rows = {}
                for nm_, idx in (("shift1", 0), ("scale1", 1), ("cshift1", 6), ("cscale1", 7)):
                    rows[nm_] = k.sb(nm_, [128, 1024]); k.dma(rows[nm_][:, :], modr[idx], R=[modrT[idx]], W=[rows[nm_]])
                pT = k.ps("pT", [128, 1024]); pu = [k.ps("pu", [128, 512]) for _ in range(2)]
                prod = Producer(k, identb, pT, nbuf=3)
                hTs = k.ring("hT", 2, [128, 8, 128], BF16)
                tiles = [(ctxf[t * 128:(t + 1) * 128, :], rows["cscale1"], rows["cshift1"], t * 128) for t in range(2)]
                tiles += [(xf[t * 128:(t + 1) * 128, :], rows["scale1"], rows["shift1"], 256 + t * 128) for t in range(64)]
                for ti, (xap, scl, shf, c0) in enumerate(tiles):
                    hT = hTs[ti % 2]
                    prod.make(xap, scl, shf, hT)
                    zero_some(3)
                    p_ = pu[ti % 2]
                    for hf in range(2):
                        for kt in range(8):
                            k.mm(p_[:, hf * 128:(hf + 1) * 128], Wu[:, kt, hf * 128:(hf + 1) * 128], hT[:, kt, :], kt == 0, kt == 7,
                                 R=[Wu, hT], W=[p_])
                    k.cp('dve', uT[:, :, c0:c0 + 128], p_[:, 0:256].rearrange("p (a b) -> p a b", a=2), R=[p_], W=[uT])
                k.cp('pool', uT[:, :, 8448:8704], uT[:, :, 0:256], R=[uT], W=[uT])
            with Phase(k):
                lre = load_f32(k, "lreA", lamA_re, [128, 32]); lim = load_f32(k, "limA", lamA_im, [128, 32])
                ldt = load_f32(k, "ldtA", logdtA, [128, 32])
                sgn = load_f32(k, "sgn", sgn_d, [128, 1])
                swapm = load_f32(k, "swapm", swapm_d, [128, 128]); identf = load_f32(k, "identf", ident_d, [128, 128])
                iota = load_f32(k, "iota", iota_d, [128, 256])
                k.ts('dve', lre[:, :], lre[:, :], -1e-4, None, ALU.min, None, R=[lre], W=[lre])
                k.act(ldt[:, :], ldt[:, :], AF.Exp, R=[ldt], W=[ldt])
                th = k.sb("thA", [128, 32])
                k.tt('dve', lre[:, :], lre[:, :], ldt[:, :], ALU.mult, R=[lre, ldt], W=[lre])
                k.tt('dve', th[:, :], lim[:, :], ldt[:, :], ALU.mult, R=[lim, ldt], W=[th])
                k.act(rdec[:, :], lre[:, :], AF.Exp, R=[lre], W=[rdec])
                a256 = k.sb("a256", [128, 32]); tA = k.sb("tA", [128, 32]); tAi = k.sb("tAi", [128, 32], I32)
                s256 = k.sb("s256", [128, 32]); c256 = k.sb("c256", [128, 32])
                k.ts('dve', a256[:, :], th[:, :], 256.0, None, ALU.mult, None, R=[th], W=[a256])
                sincos(k, a256, [128, 32], (s256[:, :], s256), (c256[:, :], c256), tA, tAi)
                k.ts('dve', s256[:, :], s256[:, :], sgn[:, 0:1], None, ALU.mult, None, R=[s256, sgn], W=[s256])
                for gd in range(32):
                    k.ts('dve', Rot[:, gd, :], identf[:, :], c256[:, gd:gd + 1], None, ALU.mult, None, R=[identf, c256], W=[Rot])
                    k.stt('dve', Rot[:, gd, :], swapm[:, :], s256[:, gd:gd + 1], Rot[:, gd, :], ALU.mult, ALU.add,
                          R=[swapm, s256, Rot], W=[Rot])
                argt = k.sb("argt", [128, 256]); tT = k.sb("tT", [128, 256]); tTi = k.sb("tTi", [128, 256], I32)
                for gd in range(32):
                    k.ts('dve', argt[:, :], iota[:, :], th[:, gd:gd + 1], None, ALU.mult, None, R=[iota, th], W=[argt])
                    sincos(k, argt, [128, 256], (sinT[:, gd, :], sinT), (cosT[:, gd, :], cosT), tT, tTi)
                ca = load_f32(k, "ca", CA, [128, 16, 128]); cb = load_f32(k, "cb", CB, [128, 16, 128])
                k.ts('dve', L1[:, :, :], ca[:, :, :], sgn[:, 0:1], None, ALU.mult, None, R=[ca, sgn], W=[L1])
                k.ts('dve', L2[:, :, :], cb[:, :, :], -1.0, None, ALU.mult, None, R=[cb], W=[L2])

            for G0 in (0, 16):
              with Phase(k):
                      SH = [128, 16, 64]
                      lr = load_f32(k, "lrB", lamB_re[:, G0:G0 + 16, :], SH); li = load_f32(k, "liB", lamB_im[:, G0:G0 + 16, :], SH); ld = load_f32(k, "ldB", logdtB[:, G0:G0 + 16, :], SH)
                      br = load_f32(k, "brB", Bre_pad[:, G0:G0 + 16, :], SH); bi = load_f32(k, "biB", Bim_pad[:, G0:G0 + 16, :], SH)
                      f_ = lambda t: t[:, :, :]
                      k.ts('dve', f_(lr), f_(lr), -1e-4, None, ALU.min, None, R=[lr], W=[lr])
                      k.act(f_(ld), f_(ld), AF.Exp, R=[ld], W=[ld])
                      aB = k.sb("aB", SH); thB = k.sb("thB", SH)
                      k.tt('dve', f_(aB), f_(lr), f_(ld), ALU.mult, R=[lr, ld], W=[aB])
                      k.tt('dve', f_(thB), f_(li), f_(ld), ALU.mult, R=[li, ld], W=[thB])
                      sB = k.sb("sB", SH); cB = k.sb("cB", SH); tB = k.sb("tB", SH); tBi = k.sb("tBi", SH, I32)
                      sincos(k, thB, SH, (f_(sB), sB), (f_(cB), cB), tB, tBi)
                      k.act(f_(aB), f_(aB), AF.Exp, R=[aB], W=[aB])
                      k.tt('dve', f_(cB), f_(cB), f_(aB), ALU.mult, R=[cB, aB], W=[cB])
                      k.tt('dve', f_(sB), f_(sB), f_(aB), ALU.mult, R=[sB, aB], W=[sB])
                      k.ts('dve', f_(cB), f_(cB), -1.0, None, ALU.add, None, R=[cB], W=[cB])
                      den = aB
                      k.tt('dve', f_(den), f_(lr), f_(lr), ALU.mult, R=[lr], W=[den])
                      k.tt('dve', f_(tB), f_(li), f_(li), ALU.mult, R=[li], W=[tB])
                      k.tt('dve', f_(den), f_(den), f_(tB), ALU.add, R=[den, tB], W=[den])
                      k.op('dve', lambda e: e.reciprocal(out=f_(den), in_=f_(den)), reads=[den], writes=[den])
                      cre = thB; cim = ld
                      k.tt('dve', f_(cre), f_(cB), f_(lr), ALU.mult, R=[cB, lr], W=[cre])
                      k.tt('dve', f_(tB), f_(sB), f_(li), ALU.mult, R=[sB, li], W=[tB])
                      k.tt('dve', f_(cre), f_(cre), f_(tB), ALU.add, R=[cre, tB], W=[cre])
                      k.tt('dve', f_(cre), f_(cre), f_(den), ALU.mult, R=[cre, den], W=[cre])
                      k.tt('dve', f_(cim), f_(sB), f_(lr), ALU.mult, R=[sB, lr], W=[cim])
                      k.tt('dve', f_(tB), f_(cB), f_(li), ALU.mult, R=[cB, li], W=[tB])
                      k.tt('dve', f_(cim), f_(cim), f_(tB), ALU.subtract, R=[cim, tB], W=[cim])
                      k.tt('dve', f_(cim), f_(cim), f_(den), ALU.mult, R=[cim, den], W=[cim])
                      bbre = sB; bbim = cB
                      k.tt('dve', f_(bbre), f_(cre), f_(br), ALU.mult, R=[cre, br], W=[bbre])
                      k.tt('dve', f_(tB), f_(cim), f_(bi), ALU.mult, R=[cim, bi], W=[tB])
                      k.tt('dve', f_(bbre), f_(bbre), f_(tB), ALU.subtract, R=[bbre, tB], W=[bbre])
                      k.tt('dve', f_(bbim), f_(cre), f_(bi), ALU.mult, R=[cre, bi], W=[bbim])
                      k.tt('dve', f_(tB), f_(cim), f_(br), ALU.mult, R=[cim, br], W=[tB])
                      k.tt('dve', f_(bbim), f_(bbim), f_(tB), ALU.add, R=[bbim, tB], W=[bbim])
                      k.cp('dve', Bb1[:, G0:G0 + 16, 0:64], f_(bbre), R=[bbre], W=[Bb1])
                      k.cp('dve', Bb1[:, G0:G0 + 16, 64:128], f_(bbim), R=[bbim], W=[Bb1])
                      k.cp('dve', Bb2[:, G0:G0 + 16, 0:64], f_(bbim), R=[bbim], W=[Bb2])
                      k.ts('dve', Bb2[:, G0:G0 + 16, 64:128], f_(bbre), -1.0, None, ALU.mult, None, R=[bbre], W=[Bb2])

            with Phase(k):
                yF = k.sb("yF", [128, 2, 4096])
                PP = [k.ps("PP", [128, 512]) for _ in range(5)]
                yps = [k.ps("yps", [128, 512]) for _ in range(2)]
                psm_t = k.ps("psm", [128, 512]); psm = k.views(psm_t, 16)
                m1r = k.ring("m1", 6, [128, 256]); m2r = k.ring("m2", 6, [128, 256]); wr = k.ring("w", 6, [128, 256])
                vr = k.ring("v", 6, [128, 256])
                A1r = k.ring("A1", 6, [128, 256], BF16); A2r = k.ring("A2", 6, [128, 256], BF16)
                ybf = k.ring("ybf", 2, [128, 256], BF16); ytmp = k.ring("ytmp", 2, [128, 256])
                units = []
                vins = []
                for fd in range(2):
                    nblk = 17 if fd == 0 else 33
                    vin = [k.sb("vin", [128, 1]) for _ in range(16)]
                    vins.append(vin)
                    for g in range(16):
                        k.memset('pool', vin[g], vin[g][:, :], 0.0)
                    for m in range(nblk):
                        if fd == 0:
                            c0 = m * 256
                            near = m >= 1
                        else:
                            c0 = 8704 - 256 * m - 256
                            near = m >= 17
                        n0 = c0 - 256
                        for hf in range(2):
                            for gi in range(8):
                                units.append(dict(fd=fd, m=m, hf=hf, gi=gi, c0=c0, n0=n0, near=near, last=(m == nblk - 1)))

                def stageA(i, u):
                    fd, hf, c0 = u["fd"], u["hf"], u["c0"]
                    g = hf * 8 + u["gi"]; gd = fd * 16 + g
                    rhs = uT[:, hf, c0:c0 + 256] if fd == 0 else uT[:, hf, c0:c0 + 256][:, ::-1]
                    P = PP[i % 5]; m1 = m1r[i % 6]; m2 = m2r[i % 6]; w_ = wr[i % 6]
                    k.mm(P[:, 0:256], Bb1[:, gd, :], rhs, True, True, R=[Bb1, uT], W=[P])
                    k.mm(P[:, 256:512], Bb2[:, gd, :], rhs, True, True, R=[Bb2, uT], W=[P])
                    k.tt('dve', m1[:, :], P[:, 0:256], cosT[:, gd, :], ALU.mult, R=[P, cosT], W=[m1])
                    k.tt('dve', m2[:, :], P[:, 256:512], sinT[:, gd, :], ALU.mult, R=[P, sinT], W=[m2])
                    k.tt('pool', w_[:, :], m1[:, :], m2[:, :], ALU.add, R=[m1, m2], W=[w_])
                    if u["gi"] == 0:
                        conv_some(2)

                def stageB(i, u):
                    fd, hf, c0, n0, gi = u["fd"], u["hf"], u["c0"], u["n0"], u["gi"]
                    g = hf * 8 + gi; gd = fd * 16 + g
                    w_ = wr[i % 6]; v = vr[i % 6]; A1 = A1r[i % 6]; A2 = A2r[i % 6]
                    vi = vins[fd][g]
                    k.op('dve', lambda e: e.tensor_tensor_scan(
                        out=v[:, :], data0=bview(rdec[:, gd:gd + 1], [128, 256]), data1=w_[:, :],
                        initial=vi[:, 0:1], op0=ALU.mult, op1=ALU.add), reads=[rdec, w_, vi], writes=[v])
                    if not u["last"]:
                        k.mm(psm[g][:, g:g + 1], Rot[:, gd, :], v[:, 255:256], True, True, R=[Rot, v], W=[psm[g]])
                        k.act(vi[:, 0:1], psm[g][:, g:g + 1], AF.Copy, R=[psm[g]], W=[vi])
                    if u["near"]:
                        k.tt('pool', A1[:, :], v[:, :], cosT[:, gd, :], ALU.mult, R=[v, cosT], W=[A1])
                        k.tt('pool', A2[:, :], v[:, :], sinT[:, gd, :], ALU.mult, R=[v, sinT], W=[A2])
                        a1 = A1[:, :] if fd == 0 else A1[:, ::-1]
                        a2 = A2[:, :] if fd == 0 else A2[:, ::-1]
                        k.mm(yps[hf][:, 0:256], L1[:, g, :], a1, gi == 0, False, R=[L1, A1], W=[yps[hf]])
                        k.mm(yps[hf][:, 0:256], L2[:, g, :], a2, False, gi == 7, R=[L2, A2], W=[yps[hf]])
                        if gi == 7:
                            if fd == 0:
                                k.stt('dve', yF[:, hf, n0:n0 + 256], uT[:, hf, c0:c0 + 256], dcol[:, hf:hf + 1], yps[hf][:, 0:256],
                                      ALU.mult, ALU.add, R=[uT, dcol, yps[hf]], W=[yF])
                            else:
                                yt = ytmp[hf]; yb = ybf[hf]
                                k.tt('dve', yt[:, :], yps[hf][:, 0:256], yF[:, hf, n0:n0 + 256], ALU.add, R=[yps[hf], yF], W=[yt])
                                k.act(yb[:, :], yt[:, :], AF.Gelu, R=[yt], W=[yb])
                                k.dma(yT_d[:, hf, n0:n0 + 256], yb[:, :], R=[yb], W=[yT_dT[n0 // 128], yT_dT[n0 // 128 + 1]])

                for i in range(len(units) + SK):
                    if i < len(units):
                        stageA(i, units[i])
                    if i >= SK:
                        stageB(i - SK, units[i - SK])

        with Phase(k):
            identb = k.sb("identb", [128, 128], BF16); k.dma(identb[:, :], ident_d, W=[identb], q='pool')
            conv_some(1000)
            Wq = load_bf16(k, "Wq", w_q, 8, 768); Wi = load_bf16(k, "Wi", w_i, 8, 768)
            rows = {}
            for nm_, idx in (("shift1", 0), ("scale1", 1), ("cshift1", 6), ("cscale1", 7)):
                rows[nm_] = k.sb(nm_, [128, 1024]); k.dma(rows[nm_][:, :], modr[idx], R=[modrT[idx]], W=[rows[nm_]])
            lbt = load_f32(k, "lbl", lbl, [128, 2, 768])
            lb = k.sb("lb", [128, 768]); oml = k.sb("oml", [128, 768])
            k.tt('dve', lb[:, :], lbt[:, 0, :], lbt[:, 1, :], ALU.subtract, R=[lbt], W=[lb])
            k.act(lb[:, :], lb[:, :], AF.Sigmoid, R=[lb], W=[lb])
            k.ts('dve', oml[:, :], lb[:, :], -1.0, 1.0, ALU.mult, ALU.add, R=[lb], W=[oml])
            g6 = load_f32(k, "hgg6", hgg6, [128, 768])
            cind = load_f32(k, "cind", cind_d, [128, 2])
            X = k.ps("X", [128, 1024]); Y = k.ps("Y", [128, 1024]); Z = k.ps("Z", [128, 1024])
            Wp = k.ps("Wp", [128, 512]); Vp = k.ps("Vp", [128, 512])
            prod = Producer(k, identb, X, nbuf=3)
            hTs = k.ring("hT", 2, [128, 8, 128], BF16)
            sg = k.sb("sg", [128, 768]); logf = k.ring("logf", 2, [128, 768])
            kb = k.ring("kb", 2, [128, 768], BF16); vb = k.ring("vb", 2, [128, 768], BF16)
            e4 = k.sb("e4", [128, 768]); kdec = k.ring("kdec", 2, [128, 768], BF16)
            e1 = k.sb("e1", [128, 6, 64]); e2 = k.sb("e2", [128, 6, 64]); e3 = k.sb("e3", [128, 6, 64])
            dec = k.ring("dec", 2, [128, 6, 2])
            qi = k.ring("qi", 2, [128, 6, 64], BF16); qin = k.ring("qin", 2, [128, 6, 64], BF16)
            kiT = k.ring("kiT", 2, [128, 6, 64], BF16)
            scm = k.ring("scm", 2, [64, 6, 64], BF16)
            S32_t = k.sb("S32", [128, 6, 128]); S32 = k.views(S32_t, 6)
            Sb = k.sb("Sb", [128, 6, 128], BF16)
            osb = k.ring("osb", 2, [64, 768]); oFl = k.ring("oFl", 2, [64, 768])
            ssq = k.ring("ssq", 2, [64, 6]); ojunk = k.sb("ojunk", [64, 128], BF16)
            for ch in range(2):
                with Phase(k):
                    Wf = load_bf16(k, "Wf", w_fF if ch == 0 else w_fB, 8, 768)
                    Minc = load_f32(k, "Minc", hgM[ch, 0], [128, 128])
                    Msuf = load_f32(k, "Msuf", hgM[ch, 2], [128, 128])
                    Mq = k.sb("Mq", [128, 130])
                    k.dma(Mq[:, 0:64], hgM[ch, 1][:, 0:64], W=[Mq])
                    k.dma(Mq[:, 64:66], cind_d, W=[Mq])
                    k.dma(Mq[:, 66:130], hgM[ch, 0][:, 0:64], W=[Mq])
                    maskb = k.sb("maskb", [64, 64], BF16); k.dma(maskb[:, :], hgM[ch, 0][0:64, 0:64], W=[maskb], q='pool')
                    k.memset('dve', S32_t, S32_t[:, :, :], 0.0)
                    k.memset('pool', Sb, Sb[:, :, :], 0.0)
                    if ch == 0:
                        tl = [("c", t) for t in range(2)] + [("x", w) for w in range(64)]
                        corder = [0, 1]
                    else:
                        tl = [("c", t) for t in (1, 0)] + [("x", w) for w in range(63, -1, -1)]
                        corder = [1, 0]
                    xcol = xf.rearrange("(r w) d -> r w d", w=64)
                    for ti, (kind, idx) in enumerate(tl):
                        hT = hTs[ti % 2]
                        near = kind == "x"
                        if kind == "c":
                            prod.make(ctxf[idx * 128:(idx + 1) * 128, :], rows["cscale1"], rows["cshift1"], hT)
                        else:
                            prod.make(xcol[:, idx, :], rows["scale1"], rows["shift1"], hT)
                        lf = logf[ti % 2]; kb_ = kb[ti % 2]; vb_ = vb[ti % 2]; kd = kdec[ti % 2]; dc = dec[ti % 2]
                        for (a, b) in ((0, 512), (512, 768)):
                            for kt in range(8):
                                k.mm(Y[:, a:b], hT[:, kt, :], Wf[:, kt, a:b], kt == 0, kt == 7, R=[hT, Wf], W=[Y])
                        for (a, b) in ((0, 512), (512, 768)):
                            for kt in range(8):
                                k.mm(Z[:, a:b], hT[:, kt, :], Wi[:, kt, a:b], kt == 0, kt == 7, R=[hT, Wi], W=[Z])
                        k.act(sg[:, :], Y[:, 0:768], AF.Sigmoid, R=[Y], W=[sg])
                        k.tt('dve', sg[:, :], sg[:, :], oml[:, :], ALU.mult, R=[sg, oml], W=[sg])
                        k.tt('dve', sg[:, :], sg[:, :], lb[:, :], ALU.add, R=[sg, lb], W=[sg])
                        k.act(lf[:, :], sg[:, :], AF.Ln, R=[sg], W=[lf])
                        k.ts('pool', kb_[:, :], sg[:, :], -1.0, 1.0, ALU.mult, ALU.add, R=[sg], W=[kb_])
                        k.act(vb_[:, :], Z[:, 0:768], AF.Copy, R=[Z], W=[vb_])
                        for (a, b) in ((0, 512), (512, 768)):
                            k.mm(X[:, a:b], Msuf[:, :], lf[:, a:b], True, True, R=[Msuf, lf], W=[X])
                        k.act(e4[:, :], X[:, 0:768], AF.Exp, R=[X], W=[e4])
                        k.tt('dve', kd[:, :], kb_[:, :], e4[:, :], ALU.mult, R=[kb_, e4], W=[kd])
                        ncol = 130 if near else 2
                        for h in range(6):
                            off = (h // 3) * 512 + (h % 3) * 130
                            rq = Mq[:, 0:130] if near else Mq[:, 64:66]
                            k.mm(Y[:, off:off + ncol], lf[:, h * 128:(h + 1) * 128], rq, True, True, R=[lf, Mq], W=[Y])
                        Yv = Y[:, :].rearrange("p (b x) -> p b x", b=2)[:, :, 0:390].rearrange("p b (h c) -> p b h c", h=3)

                        def hv(t):
                            return t[:, :, :].rearrange("p (b h) c -> p b h c", b=2)
                        if near:
                            k.act(hv(e1), Yv[:, :, :, 0:64], AF.Exp, R=[Y], W=[e1])
                            k.act(hv(e2), Yv[:, :, :, 0:64], AF.Exp, R=[Y], W=[e2], scale=-1.0)
                            k.act(hv(e3), Yv[:, :, :, 66:130], AF.Exp, R=[Y], W=[e3])
                            k.act(hv(dc), Yv[:, :, :, 64:66], AF.Exp, R=[Y], W=[dc])
                        else:
                            k.act(hv(dc), Yv[:, :, :, 0:2], AF.Exp, R=[Y], W=[dc])
                        if near:
                            qi_ = qi[ti % 2]; qn_ = qin[ti % 2]; ki_ = kiT[ti % 2]; sc_m = scm[ti % 2]
                            for h in range(6):
                                for kt in range(8):
                                    k.mm(Wp[:, h * 64:(h + 1) * 64], Wq[:, kt, h * 128:(h + 1) * 128], hT[:, kt, 0:64], kt == 0, kt == 7,
                                         R=[Wq, hT], W=[Wp])
                            Wpv = Wp[:, 0:384].rearrange("p (h c) -> p h c", h=6)
                            k.tt('dve', qi_[:, :, :], Wpv, e1[:, :, :], ALU.mult, R=[Wp, e1], W=[qi_])
                            k.tt('dve', qn_[:, :, :], Wpv, e3[:, :, :], ALU.mult, R=[Wp, e3], W=[qn_])
                            for h in range(6):
                                k.mm(Vp[:, h * 64:(h + 1) * 64], kb_[0:64, h * 128:(h + 1) * 128], identb[0:64, 0:64], True, True,
                                     R=[kb_, identb], W=[Vp])
                            k.tt('dve', ki_[:, :, :], Vp[:, 0:384].rearrange("p (h c) -> p h c", h=6), e2[:, :, :], ALU.mult,
                                 R=[Vp, e2], W=[ki_])
                            for h in range(6):
                                k.mm(Wp[0:64, h * 64:(h + 1) * 64], ki_[:, h, :], qi_[:, h, :], True, True, R=[ki_, qi_], W=[Wp])
                            k.tt('dve', sc_m[:, :, :], Wp[0:64, 0:384].rearrange("p (h c) -> p h c", h=6),
                                 bview(maskb[:, None, :], [64, 6, 64]), ALU.mult, R=[Wp, maskb], W=[sc_m])
                        for c in corder:
                            if c == 0 and near:
                                for h in range(6):
                                    hs = slice(h * 128, (h + 1) * 128)
                                    k.mm(Z[0:64, hs], sc_m[:, h, :], vb_[0:64, hs], True, False, R=[sc_m, vb_], W=[Z])
                                    k.mm(Z[0:64, hs], qn_[:, h, :], Sb[:, h, :], False, True, R=[qn_, Sb], W=[Z])
                                w = idx
                                if ch == 0:
                                    ob = osb[ti % 2]
                                    k.act(ob[:, :], Z[0:64, 0:768], AF.Copy, R=[Z], W=[ob])
                                    k.dma(oF_d[w * 64:(w + 1) * 64, :], ob[:, :], R=[ob], W=[oF_dT[w]])
                                else:
                                    ob = osb[ti % 2]; of_ = oFl[ti % 2]; sq = ssq[ti % 2]
                                    k.dma(of_[:, :], oF_d[w * 64:(w + 1) * 64, :], R=[oF_dT[w]], W=[of_])
                                    k.tt('dve', ob[:, :], Z[0:64, 0:768], of_[:, :], ALU.add, R=[Z, of_], W=[ob])
                                    for h in range(6):
                                        k.act(ojunk[:, :], ob[:, h * 128:(h + 1) * 128], AF.Square, R=[ob], W=[ojunk, sq],
                                              accum_out=sq[:, h:h + 1])
                                    k.ts('dve', sq[:, :], sq[:, :], 1.0 / 128, EPS, ALU.mult, ALU.add, R=[sq], W=[sq])
                                    k.act(sq[:, :], sq[:, :], AF.Sqrt, R=[sq], W=[sq])
                                    k.op('dve', lambda e, sq=sq: e.reciprocal(out=sq[:, :], in_=sq[:, :]), reads=[sq], writes=[sq])
                                    obv = ob[:, :].rearrange("p (h c) -> p h c", h=6)
                                    k.tt('dve', obv, obv, bview(sq[:, :, None], [64, 6, 128]), ALU.mult, R=[ob, sq], W=[ob])
                                    k.tt('pool', ob[:, :], ob[:, :], g6[0:64, :], ALU.mult, R=[ob, g6], W=[ob])
                                    k.dma(on_d.rearrange("(r w) c -> r w c", w=64)[:, w, :], ob[:, :], R=[ob], W=[on_dT[w]])
                            cs = slice(c * 64, (c + 1) * 64)
                            for h in range(6):
                                hs = slice(h * 128, (h + 1) * 128)
                                k.mm(X[:, hs], kd[cs, hs], vb_[cs, hs], True, True, R=[kd, vb_], W=[X])
                            for h in range(6):
                                hs = slice(h * 128, (h + 1) * 128)
                                k.stt('dve', S32_t[:, h, :], S32_t[:, h, :], dc[:, h, c:c + 1], X[:, hs], ALU.mult, ALU.add,
                                      R=[S32[h], dc, X], W=[S32[h]])
                            k.cp('pool', Sb[:, :, :], S32_t[:, :, :], R=S32, W=[Sb])

        with Phase(k):
            selA = k.sb("selA", [128, 32, 64]); gateA = k.sb("gateA", [128, 32, 64])
            with Phase(k):
                identb = k.sb("identb", [128, 128], BF16); k.dma(identb[:, :], ident_d, W=[identb], q='pool')
                identf = load_f32(k, "identf", ident_d, [128, 128])
                Wgo = load_bf16(k, "Wgo", w_go, 8, 768); Wga = load_bf16(k, "Wga", w_ga, 8, 1024); Wgb = load_bf16(k, "Wgb", w_gb, 8, 1024)
                Wgl = load_bf16(k, "Wgl", wglu, 2, 256); Pa = load_bf16(k, "Pa", p_a, 2, 1024); Pb = load_bf16(k, "Pb", p_b, 6, 1024)
                Wo = load_bf16(k, "Wo", w_out, 8, 1024); Wr = load_bf16(k, "Wr", w_r, 8, 64)
                brt = load_f32(k, "brt", b_r, [128, 64])
                rows = {}
                for nm_, idx in (("shift1", 0), ("scale1", 1), ("g1", 2), ("shift2", 3), ("scale2", 4)):
                    rows[nm_] = k.sb(nm_, [128, 1024]); k.dma(rows[nm_][:, :], modr[idx], R=[modrT[idx]], W=[rows[nm_]])
                X = k.ps("X", [128, 1024]); Y = k.ps("Y", [128, 1024]); Z = k.ps("Z", [128, 1024]); Wp = k.ps("Wp", [128, 1024])
                prod = Producer(k, identb, X, nbuf=2)
                hT = k.sb("hT", [128, 8, 128], BF16)
                yT = k.sb("yT", [128, 2, 128], BF16); sgl = k.sb("sgl", [128, 2, 128]); yaT = k.sb("yaT", [128, 2, 128], BF16)
                ont = k.sb("ont", [128, 768]); sgo = k.sb("sgo", [128, 768]); ybb = k.sb("ybb", [128, 768], BF16)
                ybT = k.sb("ybT", [128, 6, 128], BF16)
                sga = k.sb("sga", [128, 1024]); t1 = k.sb("t1", [128, 1024]); mb = k.sb("mb", [128, 1024], BF16)
                mT = k.sb("mT", [128, 8, 128], BF16)
                x2 = k.sb("x2", [128, 1024]); h2b = k.sb("h2b", [128, 1024], BF16); h2T = k.sb("h2T", [128, 8, 128], BF16)
                ss2 = k.sb("ss2", [128, 1])
                rs = k.sb("rs", [128, 64]); bz = k.sb("bz", [128, 64]); eq = k.sb("eq", [128, 64]); m1 = k.sb("m1g", [128, 8])
                m2 = k.sb("m2g", [128, 8]); top8 = k.sb("top8", [128, 8]); gok = k.sb("gok", [128, 8]); msk = k.sb("msk", [128, 64])
                tq = k.sb("tq", [128, 64]); den = k.sb("den", [128, 1])
                for t in range(32):
                    r0 = t * 128
                    xt = prod.make(xf[r0:r0 + 128, :], rows["scale1"], rows["shift1"], hT)
                    k.dma(yT[:, :, :], yT_d[:, :, r0:r0 + 128], R=[yT_dT[t]], W=[yT])
                    for mh in range(2):
                        for kh in range(2):
                            k.mm(Wp[:, mh * 128:(mh + 1) * 128], Wgl[:, kh, mh * 128:(mh + 1) * 128], yT[:, kh, :], kh == 0, kh == 1,
                                 R=[Wgl, yT], W=[Wp])
                    k.act(sgl[:, :, :], Wp[:, 0:256].rearrange("p (a b) -> p a b", a=2), AF.Sigmoid, R=[Wp], W=[sgl])
                    k.tt('dve', yaT[:, :, :], yT[:, :, :], sgl[:, :, :], ALU.mult, R=[yT, sgl], W=[yaT])
                    for (a, b) in ((0, 512), (512, 768)):
                        for kt in range(8):
                            k.mm(Y[:, a:b], hT[:, kt, :], Wgo[:, kt, a:b], kt == 0, kt == 7, R=[hT, Wgo], W=[Y])
                    k.act(sgo[:, :], Y[:, 0:768], AF.Silu, R=[Y], W=[sgo])
                    k.dma(ont[:, :], on_d[r0:r0 + 128, :], R=on_dT, W=[ont])
                    k.tt('dve', ybb[:, :], ont[:, :], sgo[:, :], ALU.mult, R=[ont, sgo], W=[ybb])
                    for j in range(6):
                        k.mm(X[:, j * 128:(j + 1) * 128], ybb[:, j * 128:(j + 1) * 128], identb[:, :], True, True, R=[ybb, identb], W=[X])
                    k.act(ybT[:, :, :], X[:, 0:768].rearrange("p (a b) -> p a b", a=6), AF.Copy, R=[X], W=[ybT])
                    for hf in range(2):
                        cs = slice(hf * 512, (hf + 1) * 512)
                        for kt in range(8):
                            k.mm(Y[:, cs], hT[:, kt, :], Wga[:, kt, cs], kt == 0, kt == 7, R=[hT, Wga], W=[Y])
                    k.act(sga[:, :], Y[:, :], AF.Sigmoid, R=[Y], W=[sga])
                    for hf in range(2):
                        cs = slice(hf * 512, (hf + 1) * 512)
                        for kh in range(2):
                            k.mm(Z[:, cs], yaT[:, kh, :], Pa[:, kh, cs], kh == 0, kh == 1, R=[yaT, Pa], W=[Z])
                    k.tt('dve', t1[:, :], sga[:, :], Z[:, :], ALU.mult, R=[sga, Z], W=[t1])
                    for hf in range(2):
                        cs = slice(hf * 512, (hf + 1) * 512)
                        for kt in range(8):
                            k.mm(Y[:, cs], hT[:, kt, :], Wgb[:, kt, cs], kt == 0, kt == 7, R=[hT, Wgb], W=[Y])
                    k.act(sga[:, :], Y[:, :], AF.Sigmoid, R=[Y], W=[sga])
                    for hf in range(2):
                        cs = slice(hf * 512, (hf + 1) * 512)
                        for j in range(6):
                            k.mm(Z[:, cs], ybT[:, j, :], Pb[:, j, cs], j == 0, j == 5, R=[ybT, Pb], W=[Z])
                    k.tt('dve', sga[:, :], sga[:, :], Z[:, :], ALU.mult, R=[sga, Z], W=[sga])
                    k.tt('pool', mb[:, :], t1[:, :], sga[:, :], ALU.add, R=[t1, sga], W=[mb])
                    prod.transpose(mb, mT)
                    for hf in range(2):
                        cs = slice(hf * 512, (hf + 1) * 512)
                        for kt in range(8):
                            k.mm(Wp[:, cs], mT[:, kt, :], Wo[:, kt, cs], kt == 0, kt == 7, R=[mT, Wo], W=[Wp])
                    k.dma(x2[:, :], xf[r0:r0 + 128, :], R=[xt], W=[x2])
                    k.tt('dve', t1[:, :], Wp[:, :], rows["g1"][:, :], ALU.mult, R=[Wp, rows["g1"]], W=[t1])
                    k.tt('dve', x2[:, :], x2[:, :], t1[:, :], ALU.add, R=[x2, t1], W=[x2])
                    k.dma(x2_d[r0:r0 + 128, :], x2[:, :], R=[x2], W=[x2_dT[t]])
                    prod.rstd(x2, ss2)
                    k.stt('dve', t1[:, :], x2[:, :], ss2[:, 0:1], rows["scale2"][:, :], ALU.mult, ALU.mult, R=[x2, ss2, rows["scale2"]], W=[t1])
                    k.tt('pool', h2b[:, :], t1[:, :], rows["shift2"][:, :], ALU.add, R=[t1, rows["shift2"]], W=[h2b])
                    prod.transpose(h2b, h2T)
                    k.dma(h2T_d[:, :, r0:r0 + 128], h2T[:, :, :], R=[h2T], W=[h2T_dT[t]])
                    for kt in range(8):
                        k.mm(Wp[:, 0:64], h2T[:, kt, :], Wr[:, kt, :], kt == 0, kt == 7, R=[h2T, Wr], W=[Wp])
                    k.act(rs[:, :], Wp[:, 0:64], AF.Sigmoid, R=[Wp], W=[rs])
                    k.tt('dve', bz[:, :], rs[:, :], brt[:, :], ALU.add, R=[rs, brt], W=[bz])
                    bz3 = bz[:, :].rearrange("p (g e) -> p g e", g=8)
                    k.op('dve', lambda e: e.tensor_reduce(out=m1[:, :], in_=bz3, axis=AX.X, op=ALU.max), reads=[bz], writes=[m1])
                    k.tt('dve', eq[:, :].rearrange("p (g e) -> p g e", g=8), bz3, bview(m1[:, :, None], [128, 8, 8]), ALU.is_equal,
                         R=[bz, m1], W=[eq])
                    k.stt('dve', eq[:, :], eq[:, :], -1e9, bz[:, :], ALU.mult, ALU.add, R=[eq, bz], W=[eq])
                    k.op('dve', lambda e: e.tensor_reduce(out=m2[:, :], in_=eq[:, :].rearrange("p (g e) -> p g e", g=8), axis=AX.X, op=ALU.max),
                         reads=[eq], writes=[m2])
                    k.tt('dve', m1[:, :], m1[:, :], m2[:, :], ALU.add, R=[m1, m2], W=[m1])
                    k.op('dve', lambda e: e.max(out=top8[:, :], in_=m1[:, :]), reads=[m1], writes=[top8])
                    k.ts('dve', gok[:, :], m1[:, :], top8[:, 3:4], None, ALU.is_ge, None, R=[m1, top8], W=[gok])
                    gok3 = bview(gok[:, :, None], [128, 8, 8])
                    k.tt('dve', msk[:, :].rearrange("p (g e) -> p g e", g=8), bz3, gok3, ALU.mult, R=[bz, gok], W=[msk])
                    k.ts('dve', gok[:, :], gok[:, :], 1e9, -1e9, ALU.mult, ALU.add, R=[gok], W=[gok])
                    k.tt('dve', msk[:, :].rearrange("p (g e) -> p g e", g=8), msk[:, :].rearrange("p (g e) -> p g e", g=8), gok3, ALU.add,
                         R=[msk, gok], W=[msk])
                    k.op('dve', lambda e: e.max(out=top8[:, :], in_=msk[:, :]), reads=[msk], writes=[top8])
                    k.ts('dve', selA[:, t, :], msk[:, :], top8[:, 7:8], None, ALU.is_ge, None, R=[msk, top8], W=[selA])
                    k.tt('dve', tq[:, :], selA[:, t, :], rs[:, :], ALU.mult, R=[selA, rs], W=[tq])
                    k.op('dve', lambda e: e.reduce_sum(out=den[:, :], in_=tq[:, :], axis=AX.X), reads=[tq], writes=[den])
                    k.op('dve', lambda e: e.reciprocal(out=den[:, :], in_=den[:, :]), reads=[den], writes=[den])
                    k.ts('dve', gateA[:, t, :], tq[:, :], den[:, 0:1], 2.5, ALU.mult, ALU.mult, R=[tq, den], W=[gateA])
                    k.dma(h2tok_d[r0:r0 + 128, :], h2b[:, :], R=[h2b], W=[h2tok_dT[t]])
                zero_some(1000)

            with Phase(k):
                triu = load_f32(k, "triu", triu_d, [128, 128]); e127 = load_f32(k, "e127", e127_d, [128, 128])
                CUM = k.ps("CUM", [128, 2048]); TOT = k.ps("TOT", [128, 2048])
                cumA = k.sb("cumA", [128, 32, 64]); totA = k.sb("totA", [128, 32, 64]); base = k.sb("base", [128, 32, 64])
                for i in range(32):
                    k.mm(CUM[:, i * 64:(i + 1) * 64], triu[:, :], selA[:, i, :], True, True, R=[triu, selA], W=[CUM])
                k.cp('dve', cumA[:, :, :], CUM[:, :].rearrange("p (a b) -> p a b", a=32), R=[CUM], W=[cumA])
                cflat = cumA[:, :, :].rearrange("p a b -> p (a b)")
                for c in range(4):
                    k.mm(TOT[:, c * 512:(c + 1) * 512], e127[:, :], cflat[:, c * 512:(c + 1) * 512], True, True, R=[e127, cumA], W=[TOT])
                k.act(totA[:, :, :], TOT[:, :].rearrange("p (a b) -> p a b", a=32), AF.Copy, R=[TOT], W=[totA])
                k.memset('dve', base, base[:, 0, :], 0.0)
                for i in range(1, 32):
                    k.tt('dve', base[:, i, :], base[:, i - 1, :], totA[:, i - 1, :], ALU.add, R=[base, totA], W=[base])
                cnt = k.sb("cnt", [128, 64]); nbf = k.sb("nbf", [128, 64]); nbi = k.sb("nbi", [128, 64], I32)
                ones64 = k.sb("ones64", [128, 64]); pend = k.sb("pend", [128, 64]); pstart = k.sb("pstart", [128, 64])
                k.memset('pool', ones64, ones64[:, :], 1.0)
                k.tt('dve', cnt[:, :], base[:, 31, :], totA[:, 31, :], ALU.add, R=[base, totA], W=[cnt])
                k.ts('dve', nbf[:, :], cnt[:, :], 1.0 / 256, 255.0 / 256 - 0.5 + 1.0 / 512, ALU.mult, ALU.add, R=[cnt], W=[nbf])
                k.cp('dve', nbi[:, :], nbf[:, :], R=[nbf], W=[nbi])
                k.cp('dve', nbf[:, :], nbi[:, :], R=[nbi], W=[nbf])
                k.ts('dve', nbf[:, :], nbf[:, :], 256.0, None, ALU.mult, None, R=[nbf], W=[nbf])
                k.op('dve', lambda e: e.tensor_tensor_scan(out=pend[:, :], data0=ones64[:, :], data1=nbf[:, :], initial=0.0,
                                                           op0=ALU.mult, op1=ALU.add), reads=[ones64, nbf], writes=[pend])
                k.tt('dve', pstart[:, :], pend[:, :], nbf[:, :], ALU.subtract, R=[pend, nbf], W=[pstart])
                k.tt('dve', cumA[:, :, :], cumA[:, :, :], base[:, :, :], ALU.add, R=[cumA, base], W=[cumA])
                k.tt('dve', cumA[:, :, :], cumA[:, :, :], bview(pstart[:, None, :], [128, 32, 64]), ALU.add, R=[cumA, pstart], W=[cumA])
                k.tt('dve', cumA[:, :, :], cumA[:, :, :], selA[:, :, :], ALU.mult, R=[cumA, selA], W=[cumA])
                cmp = k.sb("cmp", [128, 192, 64]); bef = k.sb("bef", [128, 192]); pcol = load_f32(k, "pcol", pcol_d, [128, 1])
                thrb = k.sb("thrb", [128, 192]); k.dma(thrb[:, :], thr_d.broadcast_to([128, 192]), W=[thrb])
                k.tt('dve', cmp[:, :, :], bview(pend[:, None, :], [128, 192, 64]), bview(thrb[:, :, None], [128, 192, 64]), ALU.is_le,
                     R=[pend, thrb], W=[cmp])
                k.op('dve', lambda e: e.reduce_sum(out=bef[:, :], in_=cmp[:, :, :], axis=AX.X), reads=[cmp], writes=[bef])
                k.ts('dve', bef[:, :], bef[:, :], 63.0, 128.0, ALU.min, ALU.mult, R=[bef], W=[bef])
                k.ts('dve', bef[:, :], bef[:, :], pcol[:, 0:1], None, ALU.add, None, R=[bef, pcol], W=[bef])
                k.cp('dve', be_i[:, :], bef[:, :], R=[bef], W=[be_i])
                t8 = k.ring("t8", 2, [128, 8]); eqg = k.ring("eqg", 2, [128, 64]); hk = k.ring("hk", 3, [128, 1024], BF16)
                for i in range(32):
                    t8_ = t8[i % 2]; hk_ = hk[i % 3]
                    k.dma(hk_[:, :], h2tok_d[i * 128:(i + 1) * 128, :], R=[h2tok_dT[i]], W=[hk_])
                    k.op('dve', lambda e, t8_=t8_, i=i: e.max(out=t8_[:, :], in_=cumA[:, i, :]), reads=[cumA], writes=[t8_])
                    k.ts('dve', dest8A[:, i, :], t8_[:, :], -1.0, None, ALU.add, None, R=[t8_], W=[dest8A])
                    for kk in range(8):
                        eq_ = eqg[kk % 2]
                        k.stt('dve', eq_[:, :], cumA[:, i, :], t8_[:, kk:kk + 1], gateA[:, i, :], ALU.is_equal, ALU.mult,
                              R=[cumA, t8_, gateA], W=[eq_])
                        k.op('dve', lambda e, eq_=eq_, i=i, kk=kk: e.reduce_sum(out=w8A[:, i, kk:kk + 1], in_=eq_[:, :], axis=AX.X),
                             reads=[eq_], writes=[w8A])
                    for kk in range(8):
                        k.op('pool', lambda e, i=i, kk=kk, hk_=hk_: e.indirect_dma_start(
                            out=xg_d[:, :], out_offset=bass.IndirectOffsetOnAxis(ap=dest8A[:, i, kk:kk + 1], axis=0),
                            in_=hk_[:, :], in_offset=None, bounds_check=bcreg, oob_is_err=False),
                            reads=[hk_, dest8A], writes=[xgT], dma=True)

        with Phase(k):
            identb = k.sb("identb", [128, 128], BF16); k.dma(identb[:, :], ident_d, W=[identb], q='pool')
            W1r = k.ring("W1", 4, [128, 8, 256], BF16); W3r = k.ring("W3", 4, [128, 8, 256], BF16); W2r = k.ring("W2", 4, [128, 2, 1024], BF16)
            xblk = k.ring("xblk", 3, [128, 2, 1024], BF16); XTr = k.ring("XT", 2, [128, 8, 256], BF16)
            sr = k.ring("sr", 2, [128, 512]); hbr = k.ring("hbr", 2, [128, 2, 256], BF16); yblk = k.ring("yblk", 2, [128, 2, 1024], BF16)
            XTp = [k.ps("XTp", [128, 1024]) for _ in range(1)]
            H13 = k.ps("H13", [128, 1024]); OP = [k.ps("OP", [128, 1024]) for _ in range(2)]
            for j in range(192):
                W1 = W1r[j % 4]; W3 = W3r[j % 4]; W2 = W2r[j % 4]; xb_ = xblk[j % 3]; XT = XTr[j % 2]
                s_ = sr[j % 2]; hb_ = hbr[j % 2]; yb_ = yblk[j % 2]
                for (Wt, wsrc, wT_) in (((W1, wb_d[0], wbT[0]), (W3, wb_d[1], wbT[1]), (W2, wb_d[2], wbT[2])) if USE_CONV else ((W1, w1, wbT[0]), (W3, w3, wbT[1]), (W2, w2, wbT[2]))):
                    k.op('pool', lambda e, Wt=Wt, wsrc=wsrc, j=j: e.indirect_dma_start(
                        out=Wt[:, :, :].rearrange("p a b -> p (a b)"), out_offset=None, in_=wsrc[:, :],
                        in_offset=bass.IndirectOffsetOnAxis(ap=be_i[:, j:j + 1], axis=0),
                        bounds_check=bcreg2, oob_is_err=False), reads=[be_i, wT_], writes=[Wt], dma=True)
                k.dma(xb_[:, :, :], xg_d[j * 256:(j + 1) * 256, :].rearrange("(a p) c -> p a c", p=128), R=[xgT], W=[xb_])
                for a in range(2):
                    P_ = XTp[0]
                    for kt in range(8):
                        k.mm(P_[:, kt * 128:(kt + 1) * 128], xb_[:, a, kt * 128:(kt + 1) * 128], identb[:, :], True, True, R=[xb_, identb], W=[P_])
                    if a == 0:
                        k.act(XT[:, :, 0:128], P_[:, :].rearrange("p (a b) -> p a b", a=8), AF.Copy, R=[P_], W=[XT])
                    else:
                        k.cp('dve', XT[:, :, 128:256], P_[:, :].rearrange("p (a b) -> p a b", a=8), R=[P_], W=[XT])
                for (Wx, off) in ((W1, 0), (W3, 512)):
                    for mt in range(2):
                        for kt in range(8):
                            k.mm(H13[:, off + mt * 256: off + (mt + 1) * 256], Wx[:, kt, mt * 128:(mt + 1) * 128], XT[:, kt, :], kt == 0, kt == 7,
                                 R=[Wx, XT], W=[H13])
                k.act(s_[:, :], H13[:, 0:512], AF.Silu, R=[H13], W=[s_])
                k.tt('dve', hb_[:, :, :], s_[:, :].rearrange("p (a b) -> p a b", a=2), H13[:, 512:1024].rearrange("p (a b) -> p a b", a=2),
                     ALU.mult, R=[s_, H13], W=[hb_])
                for a in range(2):
                    O_ = OP[a]
                    for hf in range(2):
                        cs = slice(hf * 512, (hf + 1) * 512)
                        for ft in range(2):
                            k.mm(O_[:, cs], hb_[:, ft, a * 128:(a + 1) * 128], W2[:, ft, cs], ft == 0, ft == 1, R=[hb_, W2], W=[O_])
                    if a == 0:
                        k.act(yb_[:, 0, :], O_[:, :], AF.Copy, R=[O_], W=[yb_])
                    else:
                        k.cp('dve', yb_[:, 1, :], O_[:, :], R=[O_], W=[yb_])
                k.dma(yg_d[j * 256:(j + 1) * 256, :].rearrange("(a p) c -> p a c", p=128), yb_[:, :, :], R=[yb_], W=[ygT])

        with Phase(k):
            g2r = k.sb("g2r", [128, 1024]); k.dma(g2r[:, :], modr[5], R=[modrT[5]], W=[g2r])
            fg = load_f32(k, "fng", fng, [128, 1024])
            W1s = k.sb("W1s", [128, 8, 256], BF16); W3s = k.sb("W3s", [128, 8, 256], BF16); W2s = k.sb("W2s", [128, 2, 1024], BF16)
            for (Wt, wsrc) in ((W1s, w1), (W3s, w3), (W2s, w2)):
                k.dma(Wt[:, :, :].rearrange("p a b -> p (a b)"), wsrc[64 * 128:65 * 128, :], W=[Wt], q='pool')
            H1 = k.ps("H1", [128, 1024]); H3 = k.ps("H3", [128, 1024]); OPs = [k.ps("OPs", [128, 1024]) for _ in range(2)]
            h2s = k.ring("h2s", 2, [128, 8, 512], BF16)
            sl = k.ring("sl", 2, [128, 1024]); hg = k.ring("hg", 2, [128, 2, 512], BF16)
            acc = k.ring("acc", 2, [128, 1024]); Yk = k.ring("Yk", 6, [128, 1024], BF16)
            x3 = k.ring("x3", 2, [128, 1024]); junk = k.sb("junk4", [128, 1024], BF16); ssf = k.ring("ssf", 2, [128, 1])
            yi = 0
            for g4 in range(8):
                h2_ = h2s[g4 % 2]; s_ = sl[g4 % 2]; hg_ = hg[g4 % 2]
                k.dma(h2_[:, :, :], h2T_d[:, :, g4 * 512:(g4 + 1) * 512], R=h2T_dT, W=[h2_])
                for (Wx, Hx) in ((W1s, H1), (W3s, H3)):
                    for mt in range(2):
                        for kt in range(8):
                            k.mm(Hx[:, mt * 512:(mt + 1) * 512], Wx[:, kt, mt * 128:(mt + 1) * 128], h2_[:, kt, :], kt == 0, kt == 7,
                                 R=[Wx, h2_], W=[Hx])
                k.act(s_[:, :], H1[:, :], AF.Silu, R=[H1], W=[s_])
                k.tt('dve', hg_[:, :, :], s_[:, :].rearrange("p (a b) -> p a b", a=2), H3[:, :].rearrange("p (a b) -> p a b", a=2), ALU.mult,
                     R=[s_, H3], W=[hg_])
                for tt_ in range(4):
                    t = g4 * 4 + tt_
                    r0 = t * 128
                    O_ = OPs[t % 2]; ac = acc[t % 2]; x_ = x3[t % 2]; s_f = ssf[t % 2]
                    for hf in range(2):
                        cs = slice(hf * 512, (hf + 1) * 512)
                        for ft in range(2):
                            k.mm(O_[:, cs], hg_[:, ft, tt_ * 128:(tt_ + 1) * 128], W2s[:, ft, cs], ft == 0, ft == 1, R=[hg_, W2s], W=[O_])
                    k.act(ac[:, :], O_[:, :], AF.Copy, R=[O_], W=[ac])
                    for kk in range(8):
                        y_ = Yk[yi % 6]; yi += 1
                        k.op('pool', lambda e, y_=y_, t=t, kk=kk: e.indirect_dma_start(
                            out=y_[:, :], out_offset=None, in_=yg_d[:, :],
                            in_offset=bass.IndirectOffsetOnAxis(ap=dest8A[:, t, kk:kk + 1], axis=0),
                            bounds_check=bcreg, oob_is_err=False), reads=[ygT, dest8A], writes=[y_], dma=True)
                        k.stt('dve', ac[:, :], y_[:, :], w8A[:, t, kk:kk + 1], ac[:, :], ALU.mult, ALU.add, R=[y_, w8A, ac], W=[ac])
                    k.dma(x_[:, :], x2_d[r0:r0 + 128, :], R=[x2_dT[t]], W=[x_])
                    k.tt('dve', ac[:, :], ac[:, :], g2r[:, :], ALU.mult, R=[ac, g2r], W=[ac])
                    k.tt('pool', x_[:, :], x_[:, :], ac[:, :], ALU.add, R=[x_, ac], W=[x_])
                    k.act(junk[:, :], x_[:, :], AF.Square, R=[x_], W=[junk, s_f], accum_out=s_f[:, 0:1])
                    k.ts('dve', s_f[:, 0:1], s_f[:, 0:1], 1.0 / 1024, EPS, ALU.mult, ALU.add, R=[s_f], W=[s_f])
                    k.act(s_f[:, 0:1], s_f[:, 0:1], AF.Sqrt, R=[s_f], W=[s_f])
                    k.op('dve', lambda e, s_f=s_f: e.reciprocal(out=s_f[:, 0:1], in_=s_f[:, 0:1]), reads=[s_f], writes=[s_f])
                    k.stt('dve', x_[:, :], x_[:, :], s_f[:, 0:1], fg[:, :], ALU.mult, ALU.mult, R=[x_, s_f, fg], W=[x_])
                    k.dma(out[r0:r0 + 128, :], x_[:, :], R=[x_], W=[])
        k.finish()
        print("kernel build: ninst=%d nsem=%d" % (k.ninst, k.nsem))
    return nc


def _hg_mats():
    M = np.zeros((2, 3, 128, 128), np.float32)
    s = np.arange(128)[:, None]; t = np.arange(128)[None, :]
    same = (s // 64) == (t // 64)
    for d in range(2):
        if d == 0:
            inc = same & (s <= t); suf = same & (s > t); ref = (t // 64) * 64 + 32
        else:
            inc = same & (s >= t); suf = same & (s < t); ref = (t // 64) * 64 + 31
        inc = inc.astype(np.float32)
        M[d, 0] = inc
        M[d, 1] = inc - inc[np.arange(128)[:, None], ref]
        M[d, 2] = suf.astype(np.float32)
    return M


def _prep_core(inp, b, kf):
    f = np.float32
    rv = (lambda a: a[::-1]) if kf else (lambda a: a)
    d = {}
    d["xf"] = np.ascontiguousarray(rv(inp["x"][b]), f)
    d["ctxf"] = np.ascontiguousarray(rv(inp["ctx"][b]), f)
    rep = lambda v: np.ascontiguousarray(np.broadcast_to(v.reshape(8, 128).T[:, :, None], (128, 8, 128)), f)
    d["crep"] = rep(inp["c"][b]); d["ccrep"] = rep(inp["c_ctx"])
    d["w_ada"] = np.ascontiguousarray(inp["w_ada"][0], f); d["b_ada"] = np.ascontiguousarray(inp["b_ada"][0][None, :], f)
    rows = lambda v, n=128: np.ascontiguousarray(np.broadcast_to(v[None, :], (n, v.shape[0])), f)
    d["n1g"] = rows(inp["norm1_g"][0]); d["n2g"] = rows(inp["norm2_g"][0]); d["fng"] = rows(inp["final_norm_g"])
    w = inp["w_in"][0]
    sl = {"u": (0, 256), "q": (256, 1024), "ff": (1024, 1792), "fb": (1792, 2560), "i": (2560, 3328), "go": (3328, 4096),
          "ga": (4096, 5120), "gb": (5120, 6144)}
    cut = lambda n: np.ascontiguousarray(w[:, sl[n][0]:sl[n][1]], f)
    d["w_u"] = cut("u"); d["w_q"] = cut("q"); d["w_i"] = cut("i"); d["w_go"] = cut("go"); d["w_ga"] = cut("ga"); d["w_gb"] = cut("gb")
    d["w_fF"] = cut("fb" if kf else "ff"); d["w_fB"] = cut("ff" if kf else "fb")
    dirs = [1, 0] if kf else [0, 1]
    lam_re = inp["s5_lam_re"][0][dirs]; lam_im = inp["s5_lam_im"][0][dirs]; logdt = inp["s5_log_dt"][0][dirs]
    bre = inp["s5_b_re"][0][dirs]; bim = inp["s5_b_im"][0][dirs]
    A = lambda v: np.ascontiguousarray(np.concatenate([v.reshape(32, 64).T] * 2, axis=0), f)
    d["lamA_re"] = A(lam_re); d["lamA_im"] = A(lam_im)
    d["logdtA"] = np.ascontiguousarray(np.broadcast_to(logdt.reshape(1, 32), (128, 32)), f)
    Bb = lambda v: np.ascontiguousarray(np.broadcast_to(v.reshape(1, 32, 64), (128, 32, 64)), f)
    d["lamB_re"] = Bb(lam_re); d["lamB_im"] = Bb(lam_im)
    d["logdtB"] = np.ascontiguousarray(np.broadcast_to(logdt.reshape(1, 32, 1), (128, 32, 64)), f)
    brp = np.zeros((128, 32, 64), f); bip = np.zeros((128, 32, 64), f)
    for fd in range(2):
        for g in range(16):
            r0 = (g % 8) * 16
            brp[r0:r0 + 16, fd * 16 + g, :] = bre[fd, g].T
            bip[r0:r0 + 16, fd * 16 + g, :] = bim[fd, g].T
    d["Bre_pad"] = brp; d["Bim_pad"] = bip
    cre = inp["s5_c_re"][0]; cim = inp["s5_c_im"][0]
    ca = np.zeros((128, 16, 128), f); cb = np.zeros((128, 16, 128), f)
    for g in range(16):
        c0 = (g % 8) * 16
        ca[0:64, g, c0:c0 + 16] = cre[g].T; ca[64:128, g, c0:c0 + 16] = cim[g].T
        cb[0:64, g, c0:c0 + 16] = cim[g].T; cb[64:128, g, c0:c0 + 16] = cre[g].T
    d["CA"] = ca; d["CB"] = cb
    d["dcol"] = np.ascontiguousarray(inp["s5_d"][0].reshape(2, 128).T, f)
    sg = np.ones((128, 1), f); sg[64:] = -1; d["sgn"] = sg
    sw = np.zeros((128, 128), f); sw[np.arange(128), (np.arange(128) + 64) % 128] = 1; d["swapm"] = sw
    d["iota256"] = np.ascontiguousarray(np.broadcast_to(np.arange(1, 257, dtype=f)[None, :], (128, 256)), f)
    d["lbl"] = np.ascontiguousarray(np.broadcast_to(inp["hg_lb_logits"][None, :, :], (128, 2, 768)), f)
    d["hgg6"] = rows(np.tile(inp["hg_norm_g"][0], 6))
    d["hgM"] = _hg_mats()
    ci = np.zeros((128, 2), f); ci[:64, 0] = 1; ci[64:, 1] = 1; d["cind"] = ci
    d["wglu"] = np.ascontiguousarray(inp["s5_w_glu"][0], f); d["p_a"] = np.ascontiguousarray(inp["p_a"][0], f)
    d["p_b"] = np.ascontiguousarray(inp["p_b"][0], f); d["w_out"] = np.ascontiguousarray(inp["w_out"][0], f)
    d["w_r"] = np.ascontiguousarray(inp["moe_w_router"][0], f); d["b_r"] = rows(inp["moe_b_router"][0])
    d["ident"] = np.eye(128, dtype=f)
    d["triu"] = np.triu(np.ones((128, 128), f))
    e127 = np.zeros((128, 128), f); e127[127, :] = 1; d["e127"] = e127
    d["thr"] = (np.arange(192, dtype=f) * 256)[None, :]
    d["pcol"] = np.arange(128, dtype=f)[:, None]
    return d


_SHARED = {}


def kernel(**inp):
    inp = {k_: np.asarray(v) for k_, v in inp.items()}
    f = np.float32
    def relay(w, ws):
        a = np.concatenate([w, ws[None]], 0)
        e_, r_, n_ = a.shape
        return np.ascontiguousarray(a.reshape(e_, r_ // 128, 128, n_).transpose(0, 2, 1, 3).reshape(e_ * 128, (r_ // 128) * n_), f)
    w1 = relay(inp["moe_w1"][0], inp["moe_ws1"][0])
    w3 = relay(inp["moe_w3"][0], inp["moe_ws3"][0])
    w2 = relay(inp["moe_w2"][0], inp["moe_ws2"][0])
    in_maps = []
    for core in range(8):
        b, kf = core // 2, core % 2
        d = _prep_core(inp, b, kf)
        d["w1"] = w1; d["w3"] = w3; d["w2"] = w2
        in_maps.append(d)
    nc = build(DEBUG)
    res = run_bass_kernel_spmd(nc, in_maps, core_ids=list(range(8)))
    outp = np.zeros((4, 8192, 1024), f)
    for core in range(8):
        b, kf = core // 2, core % 2
        o = res.results[core]["out"]
        if kf == 0:
            outp[b, :4096] = o
        else:
            outp[b, 4096:] = o[::-1]
    if DEBUG:
        _SHARED["res"] = res
    return outp
```

```python
import numpy as np
from contextlib import ExitStack
import concourse.bass as bass
import concourse.mybir as mybir
from concourse.bass_utils import run_bass_kernel_spmd

F32 = mybir.dt.float32
BF16 = mybir.dt.bfloat16
I32 = mybir.dt.int32
AF = mybir.ActivationFunctionType
ALU = mybir.AluOpType
AX = mybir.AxisListType

PI = float(np.pi)
TWO_PI = float(2 * np.pi)
EPS = 1e-6
DEBUG = False
USE_CONV = False
SK = 2


class TT:
    def __init__(s, h):
        s.h = h
        s.w = None
        s.r = {}

    def __getitem__(s, k):
        return s.h[k]


class KB:
    def __init__(s, nc):
        s.nc = nc
        s.gstack = ExitStack()
        s.stack = s.gstack
        s.eng = {'pe': nc.tensor, 'dve': nc.vector, 'act': nc.scalar, 'pool': nc.gpsimd, 'sp': nc.sync}
        s.csem = {}
        s.ccnt = {}
        s.seen = {e: {} for e in s.eng}
        s.dsem = {}
        s.dpos = {}
        s.nsem = 0
        s.ninst = 0
        s.uid = 0
        s.bsem = None
        s.bcnt = 0

    def newsem(s):
        s.nsem += 1
        return s.gstack.enter_context(s.nc.semaphore("s%d" % s.nsem))

    def nm(s, name):
        s.uid += 1
        return "%s_%d" % (name, s.uid)

    def sb(s, name, shape, dt=F32):
        return TT(s.stack.enter_context(s.nc.sbuf_tensor(s.nm(name), list(shape), dt)))

    def ps(s, name, shape, dt=F32):
        return TT(s.stack.enter_context(s.nc.psum_tensor(s.nm(name), list(shape), dt)))

    def ring(s, name, n, shape, dt=F32):
        return [s.sb(name, shape, dt) for _ in range(n)]

    def views(s, t, n):
        return [TT(t.h) for _ in range(n)]

    def _wait(s, e, sem, val):
        key = id(sem)
        if s.seen[e].get(key, 0) >= val:
            return
        s.seen[e][key] = val
        s.eng[e].wait_ge(sem, val)
        s.ninst += 1

    def op(s, e, fn, reads=(), writes=(), dma=False):
        deps = []
        for t in reads:
            if t.w is not None:
                deps.append(t.w)
        for t in writes:
            if t.w is not None:
                deps.append(t.w)
            deps.extend(t.r.values())
        if dma:
            q = s.dsem.setdefault(e, [])
            if len(q) < 16:
                q.append([s.newsem(), 0])
                slot = q[-1]
            else:
                p = s.dpos.get(e, 0)
                slot = q[p]
                s.dpos[e] = (p + 1) % 16
            sem = slot[0]
            if slot[1] > 0:
                s._wait(e, sem, slot[1])
            slot[1] += 16
            val = slot[1]
            inc = 16
        else:
            if e not in s.csem or s.ccnt[e] >= 20000:
                s.csem[e] = s.newsem()
                s.ccnt[e] = 0
            sem = s.csem[e]
            s.ccnt[e] += 1
            val = s.ccnt[e]
            inc = 1
        for (dsem, dval, deng) in deps:
            if deng == e and e == 'pe' and not dma:
                continue
            s._wait(e, dsem, dval)
        ins = fn(s.eng[e])
        ins.then_inc(sem, inc)
        s.ninst += 1
        tag = (sem, val, None if dma else e)
        for t in writes:
            t.w = tag
            t.r = {}
        for t in reads:
            t.r[id(sem)] = tag
        return tag

    def outstanding(s):
        tags = []
        for e, sem in s.csem.items():
            tags.append((sem, s.ccnt[e]))
        for e, q in s.dsem.items():
            for slot in q:
                if slot[1] > 0:
                    tags.append((slot[0], slot[1]))
        return tags

    def barrier(s):
        if s.bsem is None:
            s.bsem = s.newsem()
        for (sem, val) in s.outstanding():
            s._wait('sp', sem, val)
        s.bcnt += 1
        s.eng['sp'].nop().then_inc(s.bsem, 1)
        for e in ('pe', 'dve', 'act', 'pool'):
            s.eng[e].wait_ge(s.bsem, s.bcnt)
        s.ninst += 5

    def finish(s):
        for (sem, val) in s.outstanding():
            s._wait('sp', sem, val)

    def dma(s, out, in_, R=(), W=(), q='sp', **kw):
        return s.op(q, lambda e: e.dma_start(out=out, in_=in_, **kw), reads=R, writes=W, dma=True)

    def mm(s, out, lhsT, rhs, start, stop, R, W):
        return s.op('pe', lambda e: e.matmul(out, lhsT=lhsT, rhs=rhs, start=start, stop=stop), reads=R, writes=W)

    def act(s, out, in_, func, R, W, **kw):
        return s.op('act', lambda e: e.activation(out=out, in_=in_, func=func, **kw), reads=R, writes=W)

    def tt(s, e, out, in0, in1, op, R, W):
        return s.op(e, lambda g: g.tensor_tensor(out=out, in0=in0, in1=in1, op=op), reads=R, writes=W)

    def ts(s, e, out, in0, s1, s2, op0, op1, R, W):
        if s2 is None:
            return s.op(e, lambda g: g.tensor_scalar(out=out, in0=in0, scalar1=s1, scalar2=None, op0=op0), reads=R, writes=W)
        return s.op(e, lambda g: g.tensor_scalar(out=out, in0=in0, scalar1=s1, scalar2=s2, op0=op0, op1=op1), reads=R, writes=W)

    def stt(s, e, out, in0, scalar, in1, op0, op1, R, W):
        return s.op(e, lambda g: g.scalar_tensor_tensor(out=out, in0=in0, scalar=scalar, in1=in1, op0=op0, op1=op1), reads=R, writes=W)

    def cp(s, e, out, in_, R, W):
        return s.op(e, lambda g: g.tensor_copy(out=out, in_=in_), reads=R, writes=W)

    def memset(s, e, t, ap, val):
        return s.op(e, lambda g: g.memset(ap, val), writes=[t])


class Phase:
    def __init__(s, k):
        s.k = k

    def __enter__(s):
        s.prev = s.k.stack
        s.st = ExitStack()
        s.k.stack = s.st
        return s

    def __exit__(s, *a):
        s.k.barrier()
        s.k.stack = s.prev
        s.st.close()
        return False


def bview(ap, shape):
    return ap.broadcast_to(list(shape))


def load_bf16(k, name, w_ap, kt, n, q='pool'):
    t = k.sb(name, [128, kt, n], BF16)
    k.dma(t[:, :, :], w_ap.rearrange("(kt p) n -> p kt n", p=128), W=[t], q=q)
    return t


def load_f32(k, name, ap, shape, q='sp'):
    t = k.sb(name, shape, F32)
    if len(shape) == 2:
        k.dma(t[:, :], ap, W=[t], q=q)
    else:
        k.dma(t[:, :, :], ap, W=[t], q=q)
    return t


class Producer:
    def __init__(s, k, identb, pT, nbuf=2):
        s.k = k
        s.identb = identb
        s.pT = pT
        s.xt = k.ring("xt", nbuf, [128, 1024])
        s.junk = k.sb("junk", [128, 1024], BF16)
        s.hb = k.ring("hb", 2, [128, 1024], BF16)
        s.ss = k.ring("ss", 2, [128, 1])
        s.i = 0

    def rstd(s, xt, ss):
        k = s.k
        k.act(s.junk[:, :], xt[:, :], AF.Square, R=[xt], W=[s.junk, ss], accum_out=ss[:, 0:1])
        k.ts('dve', ss[:, 0:1], ss[:, 0:1], 1.0 / 1024, EPS, ALU.mult, ALU.add, R=[ss], W=[ss])
        k.act(ss[:, 0:1], ss[:, 0:1], AF.Sqrt, R=[ss], W=[ss])
        k.op('dve', lambda e: e.reciprocal(out=ss[:, 0:1], in_=ss[:, 0:1]), reads=[ss], writes=[ss])

    def make(s, x_ap, scale, shift, hT, xdep=()):
        k = s.k
        i = s.i
        s.i += 1
        xt = s.xt[i % len(s.xt)]
        hb = s.hb[i % 2]
        ss = s.ss[i % 2]
        k.dma(xt[:, :], x_ap, R=list(xdep), W=[xt])
        s.rstd(xt, ss)
        k.stt('dve', xt[:, :], xt[:, :], ss[:, 0:1], scale[:, :], ALU.mult, ALU.mult, R=[xt, ss, scale], W=[xt])
        k.tt('pool', hb[:, :], xt[:, :], shift[:, :], ALU.add, R=[xt, shift], W=[hb])
        s.transpose(hb, hT)
        return xt

    def transpose(s, hb, hT):
        k = s.k
        for kt in range(8):
            k.mm(s.pT[:, kt * 128:(kt + 1) * 128], hb[:, kt * 128:(kt + 1) * 128], s.identb[:, :], True, True,
                 R=[hb, s.identb], W=[s.pT])
        k.act(hT[:, :, :], s.pT[:, :].rearrange("p (a b) -> p a b", a=8), AF.Copy, R=[s.pT], W=[hT])


def sincos(k, arg, shape, sin_out, cos_out, tmp, tmpi, R_extra=()):
    def full(t):
        return t[:, :] if len(shape) == 2 else t[:, :, :]
    a = full(arg)
    k.ts('dve', full(tmp), a, 1.0 / TWO_PI, None, ALU.mult, None, R=[arg], W=[tmp])
    k.cp('dve', full(tmpi), full(tmp), R=[tmp], W=[tmpi])
    k.cp('dve', full(tmp), full(tmpi), R=[tmpi], W=[tmp])
    k.stt('dve', full(tmp), full(tmp), -TWO_PI, a, ALU.mult, ALU.add, R=[tmp, arg], W=[tmp])
    k.ts('dve', full(tmp), full(tmp), PI, -PI, ALU.min, ALU.max, R=[tmp], W=[tmp])
    so, sT = sin_out
    co, cT = cos_out
    k.act(so, full(tmp), AF.Sin, R=[tmp], W=[sT])
    k.act(full(tmp), full(tmp), AF.Abs, R=[tmp], W=[tmp])
    k.act(co, full(tmp), AF.Sin, R=[tmp], W=[cT], scale=-1.0, bias=PI / 2)


def build(dbg=False):
    nc = bass.Bass("TRN2", target_bir_lowering=False)

    def din(name, shape, dt=F32):
        return nc.dram_tensor(name, list(shape), dt, kind="ExternalInput").ap()

    def dscr(name, shape, dt=F32):
        if dbg:
            return nc.dram_tensor(name, list(shape), dt, kind="ExternalOutput").ap()
        return nc.dram_tensor(name, list(shape), dt).ap()

    xf = din("xf", [8192, 1024]); ctxf = din("ctxf", [256, 1024])
    crep = din("crep", [128, 8, 128]); ccrep = din("ccrep", [128, 8, 128])
    w_ada = din("w_ada", [1024, 6144]); b_ada = din("b_ada", [1, 6144])
    n1g = din("n1g", [128, 1024]); n2g = din("n2g", [128, 1024]); fng = din("fng", [128, 1024])
    w_u = din("w_u", [1024, 256]); w_q = din("w_q", [1024, 768]); w_fF = din("w_fF", [1024, 768])
    w_fB = din("w_fB", [1024, 768]); w_i = din("w_i", [1024, 768]); w_go = din("w_go", [1024, 768])
    w_ga = din("w_ga", [1024, 1024]); w_gb = din("w_gb", [1024, 1024])
    lamA_re = din("lamA_re", [128, 32]); lamA_im = din("lamA_im", [128, 32]); logdtA = din("logdtA", [128, 32])
    lamB_re = din("lamB_re", [128, 32, 64]); lamB_im = din("lamB_im", [128, 32, 64]); logdtB = din("logdtB", [128, 32, 64])
    Bre_pad = din("Bre_pad", [128, 32, 64]); Bim_pad = din("Bim_pad", [128, 32, 64])
    CA = din("CA", [128, 16, 128]); CB = din("CB", [128, 16, 128])
    dcol_d = din("dcol", [128, 2]); sgn_d = din("sgn", [128, 1]); swapm_d = din("swapm", [128, 128])
    iota_d = din("iota256", [128, 256])
    lbl = din("lbl", [128, 2, 768]); hgg6 = din("hgg6", [128, 768])
    hgM = din("hgM", [2, 3, 128, 128])
    cind_d = din("cind", [128, 2])
    wglu = din("wglu", [256, 256]); p_a = din("p_a", [256, 1024]); p_b = din("p_b", [768, 1024])
    w_out = din("w_out", [1024, 1024])
    w_r = din("w_r", [1024, 64]); b_r = din("b_r", [128, 64])
    w1 = din("w1", [65 * 128, 2048]); w3 = din("w3", [65 * 128, 2048]); w2 = din("w2", [65 * 128, 2048])
    pcol_d = din("pcol", [128, 1])
    ident_d = din("ident", [128, 128])
    triu_d = din("triu", [128, 128]); e127_d = din("e127", [128, 128]); thr_d = din("thr", [1, 192])
    out = nc.dram_tensor("out", [4096, 1024], F32, kind="ExternalOutput").ap()

    modr = dscr("modr", [8, 128, 1024])
    yT_d = dscr("yT_d", [128, 2, 4096], BF16)
    oF_d = dscr("oF_d", [4096, 768])
    on_d = dscr("on_d", [4096, 768])
    x2_d = dscr("x2_d", [4096, 1024])
    h2T_d = dscr("h2T_d", [128, 8, 4096], BF16)
    h2tok_d = dscr("h2tok_d", [4096, 1024], BF16)
    NROW = 192 * 256
    xg_d = nc.dram_tensor("xg_d", [NROW, 1024], BF16).ap()
    yg_d = nc.dram_tensor("yg_d", [NROW, 1024], BF16).ap()

    k = KB(nc)
    with k.gstack:
        modrT = [TT(modr) for _ in range(8)]
        yT_dT = [TT(yT_d) for _ in range(32)]
        oF_dT = [TT(oF_d) for _ in range(64)]
        on_dT = [TT(on_d) for _ in range(64)]
        x2_dT = [TT(x2_d) for _ in range(32)]
        h2T_dT = [TT(h2T_d) for _ in range(32)]
        h2tok_dT = [TT(h2tok_d) for _ in range(32)]
        xgT = TT(xg_d); ygT = TT(yg_d)
        dest8A = TT(k.gstack.enter_context(nc.sbuf_tensor("dest8A", [128, 32, 8], I32)))
        w8A = TT(k.gstack.enter_context(nc.sbuf_tensor("w8A", [128, 32, 8], F32)))
        be_i = TT(k.gstack.enter_context(nc.sbuf_tensor("be_i", [128, 192], I32)))
        zt = TT(k.gstack.enter_context(nc.sbuf_tensor("zt", [128, 2048], BF16)))
        k.memset('pool', zt, zt[:, :], 0.0)
        zinit = [0]
        bcreg = k.gstack.enter_context(nc.gpsimd.register("bcreg"))
        nc.gpsimd.reg_mov(bcreg, NROW - 1)
        bcreg2 = k.gstack.enter_context(nc.gpsimd.register("bcreg2"))
        nc.gpsimd.reg_mov(bcreg2, (64 if USE_CONV else 65) * 128 - 1)
        wb_d = [nc.dram_tensor("w%db_d" % i_, [64 * 128, 2048], BF16).ap() for i_ in range(3)]
        wbT = [TT(wb_d[i_]) for i_ in range(3)]
        stg = [TT(k.gstack.enter_context(nc.sbuf_tensor("stg%d" % i_, [128, 2048], BF16))) for i_ in range(2)]
        cinit = [0]

        def conv_some(n):
            if not USE_CONV:
                return
            for _ in range(n):
                c = cinit[0]
                if c >= 192:
                    return
                cinit[0] += 1
                e_, wh = c // 3, c % 3
                st_ = stg[c % 2]
                k.dma(st_[:, :], (w1, w3, w2)[wh][e_ * 128:(e_ + 1) * 128, :], W=[st_], q='pool')
                k.dma(wb_d[wh][e_ * 128:(e_ + 1) * 128, :], st_[:, :], R=[st_], W=[wbT[wh]], q='act')

        def zero_some(n):
            for _ in range(n):
                c = zinit[0]
                if c >= NROW // 256:
                    return
                zinit[0] += 1
                k.dma(xg_d[c * 256:(c + 1) * 256, :].rearrange("(p a) c -> p (a c)", p=128), zt[:, :], R=[zt], W=[xgT], q='act')

        with Phase(k):
            sc_ = load_f32(k, "silc", crep, [128, 8, 128])
            scc = load_f32(k, "silcc", ccrep, [128, 8, 128])
            k.act(sc_[:, :, :], sc_[:, :, :], AF.Silu, R=[sc_], W=[sc_])
            k.act(scc[:, :, :], scc[:, :, :], AF.Silu, R=[scc], W=[scc])
            ba = k.sb("ba", [1, 6144]); k.dma(ba[:, :], b_ada, W=[ba])
            ones1 = k.sb("ones1", [1, 128]); k.memset('dve', ones1, ones1[:, :], 1.0)
            g1t = load_f32(k, "n1g", n1g, [128, 1024]); g2t = load_f32(k, "n2g", n2g, [128, 1024])
            wa = k.ring("wa", 2, [128, 8, 1024])
            psx = [k.ps("psx", [128, 1024]) for _ in range(2)]
            psc = [k.ps("psc", [128, 1024]) for _ in range(2)]
            res = k.ring("res", 3, [128, 1024])
            ri = 0
            for j in range(6):
                w_t = wa[j % 2]
                k.dma(w_t[:, :, :], w_ada[:, j * 1024:(j + 1) * 1024].rearrange("(kt p) n -> p kt n", p=128), W=[w_t])
                variants = [(sc_, psx[j % 2], j)]
                if j < 2:
                    variants.append((scc, psc[j % 2], 6 + j))
                for (lt, pt, dst) in variants:
                    for hf in range(2):
                        cs = slice(hf * 512, (hf + 1) * 512)
                        for kt in range(8):
                            k.mm(pt[:, cs], lt[:, kt, :], w_t[:, kt, cs], kt == 0, False, R=[lt, w_t], W=[pt])
                        k.mm(pt[:, cs], ones1[0:1, :], ba[0:1, j * 1024 + hf * 512: j * 1024 + (hf + 1) * 512], False, True,
                             R=[ones1, ba], W=[pt])
                    r_t = res[ri % 3]; ri += 1
                    if j == 1 or j == 4:
                        gt = g1t if j == 1 else g2t
                        k.stt('dve', r_t[:, :], pt[:, :], 1.0, gt[:, :], ALU.add, ALU.mult, R=[pt, gt], W=[r_t])
                    else:
                        k.cp('dve', r_t[:, :], pt[:, :], R=[pt], W=[r_t])
                    k.dma(modr[dst], r_t[:, :], R=[r_t], W=[modrT[dst]])

        with Phase(k):
            identb = k.sb("identb", [128, 128], BF16); k.dma(identb[:, :], ident_d, W=[identb], q='pool')
            uT = k.sb("uT", [128, 2, 8704], BF16)
            Bb1 = k.sb("Bb1", [128, 32, 128], BF16); Bb2 = k.sb("Bb2", [128, 32, 128], BF16)
            L1 = k.sb("L1", [128, 16, 128], BF16); L2 = k.sb("L2", [128, 16, 128], BF16)
            Rot = k.sb("Rot", [128, 32, 128])
            cosT = k.sb("cosT", [128, 32, 256], BF16); sinT = k.sb("sinT", [128, 32, 256], BF16)
            rdec = k.sb("rdec", [128, 32])
            dcol = load_f32(k, "dcol", dcol_d, [128, 2])
            with Phase(k):
                Wu = load_bf16(k, "Wu", w_u, 8, 256)
                rows = {}
                for nm_, idx in (("shift1", 0), ("scale1", 1), ("cshift1", 6), ("cscale1", 7)):
                    rows[nm_] = k.sb(nm_, [128, 1024]); k.dma(rows[nm_][:, :], modr[idx], R=[modrT[idx]], W=[rows[nm_]])
                pT = k.ps("pT", [128, 1024]); pu = [k.ps("pu", [128, 512]) for _ in range(2)]
                prod = Producer(k, identb, pT, nbuf=3)
                hTs = k.ring("hT", 2, [128, 8, 128], BF16)
                tiles = [(ctxf[t * 128:(t + 1) * 128, :], rows["cscale1"], rows["cshift1"], t * 128) for t in range(2)]
                tiles += [(xf[t * 128:(t + 1) * 128, :], rows["scale1"], rows["shift1"], 256 + t * 128) for t in range(64)]
                for ti, (xap, scl, shf, c0) in enumerate(tiles):
                    hT = hTs[ti % 2]
                    prod.make(xap, scl, shf, hT)
                    zero_some(3)
                    p_ = pu[ti % 2]
                    for hf in range(2):
                        for kt in range(8):
                            k.mm(p_[:, hf * 128:(hf + 1) * 128], Wu[:, kt, hf * 128:(hf + 1) * 128], hT[:, kt, :], kt == 0, kt == 7,
                                 R=[Wu, hT], W=[p_])
                    k.cp('dve', uT[:, :, c0:c0 + 128], p_[:, 0:256].rearrange("p (a b) -> p a b", a=2), R=[p_], W=[uT])
                k.cp('pool', uT[:, :, 8448:8704], uT[:, :, 0:256], R=[uT], W=[uT])
            with Phase(k):
                lre = load_f32(k, "lreA", lamA_re, [128, 32]); lim = load_f32(k, "limA", lamA_im, [128, 32])
                ldt = load_f32(k, "ldtA", logdtA, [128, 32])
                sgn = load_f32(k, "sgn", sgn_d, [128, 1])
                swapm = load_f32(k, "swapm", swapm_d, [128, 128]); identf = load_f32(k, "identf", ident_d, [128, 128])
                iota = load_f32(k, "iota", iota_d, [128, 256])
                k.ts('dve', lre[:, :], lre[:, :], -1e-4, None, ALU.min, None, R=[lre], W=[lre])
                k.act(ldt[:, :], ldt[:, :], AF.Exp, R=[ldt], W=[ldt])
                th = k.sb("thA", [128, 32])
                k.tt('dve', lre[:, :], lre[:, :], ldt[:, :], ALU.mult, R=[lre, ldt], W=[lre])
                k.tt('dve', th[:, :], lim[:, :], ldt[:, :], ALU.mult, R=[lim, ldt], W=[th])
                k.act(rdec[:, :], lre[:, :], AF.Exp, R=[lre], W=[rdec])
                a256 = k.sb("a256", [128, 32]); tA = k.sb("tA", [128, 32]); tAi = k.sb("tAi", [128, 32], I32)
                s256 = k.sb("s256", [128, 32]); c256 = k.sb("c256", [128, 32])
                k.ts('dve', a256[:, :], th[:, :], 256.0, None, ALU.mult, None, R=[th], W=[a256])
                sincos(k, a256, [128, 32], (s256[:, :], s256), (c256[:, :], c256), tA, tAi)
                k.ts('dve', s256[:, :], s256[:, :], sgn[:, 0:1], None, ALU.mult, None, R=[s256, sgn], W=[s256])
                for gd in range(32):
                    k.ts('dve', Rot[:, gd, :], identf[:, :], c256[:, gd:gd + 1], None, ALU.mult, None, R=[identf, c256], W=[Rot])
                    k.stt('dve', Rot[:, gd, :], swapm[:, :], s256[:, gd:gd + 1], Rot[:, gd, :], ALU.mult, ALU.add,
                          R=[swapm, s256, Rot], W=[Rot])
                argt = k.sb("argt", [128, 256]); tT = k.sb("tT", [128, 256]); tTi = k.sb("tTi", [128, 256], I32)
                for gd in range(32):
                    k.ts('dve', argt[:, :], iota[:, :], th[:, gd:gd + 1], None, ALU.mult, None, R=[iota, th], W=[argt])
                    sincos(k, argt, [128, 256], (sinT[:, gd, :], sinT), (cosT[:, gd, :], cosT), tT, tTi)
                ca = load_f32(k, "ca", CA, [128, 16, 128]); cb = load_f32(k, "cb", CB, [128, 16, 128])
                k.ts('dve', L1[:, :, :], ca[:, :, :], sgn[:, 0:1], None, ALU.mult, None, R=[ca, sgn], W=[L1])
                k.ts('dve', L2[:, :, :], cb[:, :, :], -1.0, None, ALU.mult, None, R=[cb], W=[L2])

            for G0 in (0, 16):
              with Phase(k):
                      SH = [128, 16, 64]
                      lr = load_f32(k, "lrB", lamB_re[:, G0:G0 + 16, :], SH); li = load_f32(k, "liB", lamB_im[:, G0:G0 + 16, :], SH); ld = load_f32(k, "ldB", logdtB[:, G0:G0 + 16, :], SH)
                      br = load_f32(k, "brB", Bre_pad[:, G0:G0 + 16, :], SH); bi = load_f32(k, "biB", Bim_pad[:, G0:G0 + 16, :], SH)
                      f_ = lambda t: t[:, :, :]
                      k.ts('dve', f_(lr), f_(lr), -1e-4, None, ALU.min, None, R=[lr], W=[lr])
                      k.act(f_(ld), f_(ld), AF.Exp, R=[ld], W=[ld])
                      aB = k.sb("aB", SH); thB = k.sb("thB", SH)
                      k.tt('dve', f_(aB), f_(lr), f_(ld), ALU.mult, R=[lr, ld], W=[aB])
                      k.tt('dve', f_(thB), f_(li), f_(ld), ALU.mult, R=[li, ld], W=[thB])
                      sB = k.sb("sB", SH); cB = k.sb("cB", SH); tB = k.sb("tB", SH); tBi = k.sb("tBi", SH, I32)
                      sincos(k, thB, SH, (f_(sB), sB), (f_(cB), cB), tB, tBi)
                      k.act(f_(aB), f_(aB), AF.Exp, R=[aB], W=[aB])
                      k.tt('dve', f_(cB), f_(cB), f_(aB), ALU.mult, R=[cB, aB], W=[cB])
                      k.tt('dve', f_(sB), f_(sB), f_(aB), ALU.mult, R=[sB, aB], W=[sB])
                      k.ts('dve', f_(cB), f_(cB), -1.0, None, ALU.add, None, R=[cB], W=[cB])
                      den = aB
                      k.tt('dve', f_(den), f_(lr), f_(lr), ALU.mult, R=[lr], W=[den])
                      k.tt('dve', f_(tB), f_(li), f_(li), ALU.mult, R=[li], W=[tB])
                      k.tt('dve', f_(den), f_(den), f_(tB), ALU.add, R=[den, tB], W=[den])
                      k.op('dve', lambda e: e.reciprocal(out=f_(den), in_=f_(den)), reads=[den], writes=[den])
                      cre = thB; cim = ld
                      k.tt('dve', f_(cre), f_(cB), f_(lr), ALU.mult, R=[cB, lr], W=[cre])
                      k.tt('dve', f_(tB), f_(sB), f_(li), ALU.mult, R=[sB, li], W=[tB])
                      k.tt('dve', f_(cre), f_(cre), f_(tB), ALU.add, R=[cre, tB], W=[cre])
                      k.tt('dve', f_(cre), f_(cre), f_(den), ALU.mult, R=[cre, den], W=[cre])
                      k.tt('dve', f_(cim), f_(sB), f_(lr), ALU.mult, R=[sB, lr], W=[cim])
                      k.tt('dve', f_(tB), f_(cB), f_(li), ALU.mult, R=[cB, li], W=[tB])
                      k.tt('dve', f_(cim), f_(cim), f_(tB), ALU.subtract, R=[cim, tB], W=[cim])
                      k.tt('dve', f_(cim), f_(cim), f_(den), ALU.mult, R=[cim, den], W=[cim])
                      bbre = sB; bbim = cB
                      k.tt('dve', f_(bbre), f_(cre), f_(br), ALU.mult, R=[cre, br], W=[bbre])
                      k.tt('dve', f_(tB), f_(cim), f_(bi), ALU.mult, R=[cim, bi], W=[tB])
                      k.tt('dve', f_(bbre), f_(bbre), f_(tB), ALU.subtract, R=[bbre, tB], W=[bbre])
                      k.tt('dve', f_(bbim), f_(cre), f_(bi), ALU.mult, R=[cre, bi], W=[bbim])
                      k.tt('dve', f_(tB), f_(cim), f_(br), ALU.mult, R=[cim, br], W=[tB])
                      k.tt('dve', f_(bbim), f_(bbim), f_(tB), ALU.add, R=[bbim, tB], W=[bbim])
                      k.cp('dve', Bb1[:, G0:G0 + 16, 0:64], f_(bbre), R=[bbre], W=[Bb1])
                      k.cp('dve', Bb1[:, G0:G0 + 16, 64:128], f_(bbim), R=[bbim], W=[Bb1])
                      k.cp('dve', Bb2[:, G0:G0 + 16, 0:64], f_(bbim), R=[bbim], W=[Bb2])
                      k.ts('dve', Bb2[:, G0:G0 + 16, 64:128], f_(bbre), -1.0, None, ALU.mult, None, R=[bbre], W=[Bb2])

            with Phase(k):
                yF = k.sb("yF", [128, 2, 4096])
                PP = [k.ps("PP", [128, 512]) for _ in range(5)]
                yps = [k.ps("yps", [128, 512]) for _ in range(2)]
                psm_t = k.ps("psm", [128, 512]); psm = k.views(psm_t, 16)
                m1r = k.ring("m1", 6, [128, 256]); m2r = k.ring("m2", 6, [128, 256]); wr = k.ring("w", 6, [128, 256])
                vr = k.ring("v", 6, [128, 256])
                A1r = k.ring("A1", 6, [128, 256], BF16); A2r = k.ring("A2", 6, [128, 256], BF16)
                ybf = k.ring("ybf", 2, [128, 256], BF16); ytmp = k.ring("ytmp", 2, [128, 256])
                units = []
                vins = []
                for fd in range(2):
                    nblk = 17 if fd == 0 else 33
                    vin = [k.sb("vin", [128, 1]) for _ in range(16)]
                    vins.append(vin)
                    for g in range(16):
                        k.memset('pool', vin[g], vin[g][:, :], 0.0)
                    for m in range(nblk):
                        if fd == 0:
                            c0 = m * 256
                            near = m >= 1
                        else:
                            c0 = 8704 - 256 * m - 256
                            near = m >= 17
                        n0 = c0 - 256
                        for hf in range(2):
                            for gi in range(8):
                                units.append(dict(fd=fd, m=m, hf=hf, gi=gi, c0=c0, n0=n0, near=near, last=(m == nblk - 1)))

                def stageA(i, u):
                    fd, hf, c0 = u["fd"], u["hf"], u["c0"]
                    g = hf * 8 + u["gi"]; gd = fd * 16 + g
                    rhs = uT[:, hf, c0:c0 + 256] if fd == 0 else uT[:, hf, c0:c0 + 256][:, ::-1]
                    P = PP[i % 5]; m1 = m1r[i % 6]; m2 = m2r[i % 6]; w_ = wr[i % 6]
                    k.mm(P[:, 0:256], Bb1[:, gd, :], rhs, True, True, R=[Bb1, uT], W=[P])
                    k.mm(P[:, 256:512], Bb2[:, gd, :], rhs, True, True, R=[Bb2, uT], W=[P])
                    k.tt('dve', m1[:, :], P[:, 0:256], cosT[:, gd, :], ALU.mult, R=[P, cosT], W=[m1])
                    k.tt('dve', m2[:, :], P[:, 256:512], sinT[:, gd, :], ALU.mult, R=[P, sinT], W=[m2])
                    k.tt('pool', w_[:, :], m1[:, :], m2[:, :], ALU.add, R=[m1, m2], W=[w_])
                    if u["gi"] == 0:
                        conv_some(2)

                def stageB(i, u):
                    fd, hf, c0, n0, gi = u["fd"], u["hf"], u["c0"], u["n0"], u["gi"]
                    g = hf * 8 + gi; gd = fd * 16 + g
                    w_ = wr[i % 6]; v = vr[i % 6]; A1 = A1r[i % 6]; A2 = A2r[i % 6]
                    vi = vins[fd][g]
                    k.op('dve', lambda e: e.tensor_tensor_scan(
                        out=v[:, :], data0=bview(rdec[:, gd:gd + 1], [128, 256]), data1=w_[:, :],
                        initial=vi[:, 0:1], op0=ALU.mult, op1=ALU.add), reads=[rdec, w_, vi], writes=[v])
                    if not u["last"]:
                        k.mm(psm[g][:, g:g + 1], Rot[:, gd, :], v[:, 255:256], True, True, R=[Rot, v], W=[psm[g]])
                        k.act(vi[:, 0:1], psm[g][:, g:g + 1], AF.Copy, R=[psm[g]], W=[vi])
                    if u["near"]:
                        k.tt('pool', A1[:, :], v[:, :], cosT[:, gd, :], ALU.mult, R=[v, cosT], W=[A1])
                        k.tt('pool', A2[:, :], v[:, :], sinT[:, gd, :], ALU.mult, R=[v, sinT], W=[A2])
                        a1 = A1[:, :] if fd == 0 else A1[:, ::-1]
                        a2 = A2[:, :] if fd == 0 else A2[:, ::-1]
                        k.mm(yps[hf][:, 0:256], L1[:, g, :], a1, gi == 0, False, R=[L1, A1], W=[yps[hf]])
                        k.mm(yps[hf][:, 0:256], L2[:, g, :], a2, False, gi == 7, R=[L2, A2], W=[yps[hf]])
                        if gi == 7:
                            if fd == 0:
                                k.stt('dve', yF[:, hf, n0:n0 + 256], uT[:, hf, c0:c0 + 256], dcol[:, hf:hf + 1], yps[hf][:, 0:256],
                                      ALU.mult, ALU.add, R=[uT, dcol, yps[hf]], W=[yF])
                            else:
                                yt = ytmp[hf]; yb = ybf[hf]
                                k.tt('dve', yt[:, :], yps[hf][:, 0:256], yF[:, hf, n0:n0 + 256], ALU.add, R=[yps[hf], yF], W=[yt])
                                k.act(yb[:, :], yt[:, :], AF.Gelu, R=[yt], W=[yb])
                                k.dma(yT_d[:, hf, n0:n0 + 256], yb[:, :], R=[yb], W=[yT_dT[n0 // 128], yT_dT[n0 // 128 + 1]])

                for i in range(len(units) + SK):
                    if i < len(units):
                        stageA(i, units[i])
                    if i >= SK:
                        stageB(i - SK, units[i - SK])

        with Phase(k):
            identb = k.sb("identb", [128, 128], BF16); k.dma(identb[:, :], ident_d, W=[identb], q='pool')
            conv_some(1000)
            Wq = load_bf16(k, "Wq", w_q, 8, 768); Wi = load_bf16(k, "Wi", w_i, 8, 768)
            rows = {}
            for nm_, idx in (("shift1", 0), ("scale1", 1), ("cshift1", 6), ("cscale1", 7)):
                rows[nm_] = k.sb(nm_, [128, 1024]); k.dma(rows[nm_][:, :], modr[idx], R=[modrT[idx]], W=[rows[nm_]])
            lbt = load_f32(k, "lbl", lbl, [128, 2, 768])
            lb = k.sb("lb", [128, 768]); oml = k.sb("oml", [128, 768])
            k.tt('dve', lb[:, :], lbt[:, 0, :], lbt[:, 1, :], ALU.subtract, R=[lbt], W=[lb])
            k.act(lb[:, :], lb[:, :], AF.Sigmoid, R=[lb], W=[lb])
            k.ts('dve', oml[:, :], lb[:, :], -1.0, 1.0, ALU.mult, ALU.add, R=[lb], W=[oml])
            g6 = load_f32(k, "hgg6", hgg6, [128, 768])
            cind = load_f32(k, "cind", cind_d, [128, 2])
            X = k.ps("X", [128, 1024]); Y = k.ps("Y", [128, 1024]); Z = k.ps("Z", [128, 1024])
            Wp = k.ps("Wp", [128, 512]); Vp = k.ps("Vp", [128, 512])
            prod = Producer(k, identb, X, nbuf=3)
            hTs = k.ring("hT", 2, [128, 8, 128], BF16)
            sg = k.sb("sg", [128, 768]); logf = k.ring("logf", 2, [128, 768])
            kb = k.ring("kb", 2, [128, 768], BF16); vb = k.ring("vb", 2, [128, 768], BF16)
            e4 = k.sb("e4", [128, 768]); kdec = k.ring("kdec", 2, [128, 768], BF16)
            e1 = k.sb("e1", [128, 6, 64]); e2 = k.sb("e2", [128, 6, 64]); e3 = k.sb("e3", [128, 6, 64])
            dec = k.ring("dec", 2, [128, 6, 2])
            qi = k.ring("qi", 2, [128, 6, 64], BF16); qin = k.ring("qin", 2, [128, 6, 64], BF16)
            kiT = k.ring("kiT", 2, [128, 6, 64], BF16)
            scm = k.ring("scm", 2, [64, 6, 64], BF16)
            S32_t = k.sb("S32", [128, 6, 128]); S32 = k.views(S32_t, 6)
            Sb = k.sb("Sb", [128, 6, 128], BF16)
            osb = k.ring("osb", 2, [64, 768]); oFl = k.ring("oFl", 2, [64, 768])
            ssq = k.ring("ssq", 2, [64, 6]); ojunk = k.sb("ojunk", [64, 128], BF16)
            for ch in range(2):
                with Phase(k):
                    Wf = load_bf16(k, "Wf", w_fF if ch == 0 else w_fB, 8, 768)
                    Minc = load_f32(k, "Minc", hgM[ch, 0], [128, 128])
                    Msuf = load_f32(k, "Msuf", hgM[ch, 2], [128, 128])
                    Mq = k.sb("Mq", [128, 130])
                    k.dma(Mq[:, 0:64], hgM[ch, 1][:, 0:64], W=[Mq])
                    k.dma(Mq[:, 64:66], cind_d, W=[Mq])
                    k.dma(Mq[:, 66:130], hgM[ch, 0][:, 0:64], W=[Mq])
                    maskb = k.sb("maskb", [64, 64], BF16); k.dma(maskb[:, :], hgM[ch, 0][0:64, 0:64], W=[maskb], q='pool')
                    k.memset('dve', S32_t, S32_t[:, :, :], 0.0)
                    k.memset('pool', Sb, Sb[:, :, :], 0.0)
                    if ch == 0:
                        tl = [("c", t) for t in range(2)] + [("x", w) for w in range(64)]
                        corder = [0, 1]
                    else:
                        tl = [("c", t) for t in (1, 0)] + [("x", w) for w in range(63, -1, -1)]
                        corder = [1, 0]
                    xcol = xf.rearrange("(r w) d -> r w d", w=64)
                    for ti, (kind, idx) in enumerate(tl):
                        hT = hTs[ti % 2]
                        near = kind == "x"
                        if kind == "c":
                            prod.make(ctxf[idx * 128:(idx + 1) * 128, :], rows["cscale1"], rows["cshift1"], hT)
                        else:
                            prod.make(xcol[:, idx, :], rows["scale1"], rows["shift1"], hT)
                        lf = logf[ti % 2]; kb_ = kb[ti % 2]; vb_ = vb[ti % 2]; kd = kdec[ti % 2]; dc = dec[ti % 2]
                        for (a, b) in ((0, 512), (512, 768)):
                            for kt in range(8):
                                k.mm(Y[:, a:b], hT[:, kt, :], Wf[:, kt, a:b], kt == 0, kt == 7, R=[hT, Wf], W=[Y])
                        for (a, b) in ((0, 512), (512, 768)):
                            for kt in range(8):
                                k.mm(Z[:, a:b], hT[:, kt, :], Wi[:, kt, a:b], kt == 0, kt == 7, R=[hT, Wi], W=[Z])
                        k.act(sg[:, :], Y[:, 0:768], AF.Sigmoid, R=[Y], W=[sg])
                        k.tt('dve', sg[:, :], sg[:, :], oml[:, :], ALU.mult, R=[sg, oml], W=[sg])
                        k.tt('dve', sg[:, :], sg[:, :], lb[:, :], ALU.add, R=[sg, lb], W=[sg])
                        k.act(lf[:, :], sg[:, :], AF.Ln, R=[sg], W=[lf])
                        k.ts('pool', kb_[:, :], sg[:, :], -1.0, 1.0, ALU.mult, ALU.add, R=[sg], W=[kb_])
                        k.act(vb_[:, :], Z[:, 0:768], AF.Copy, R=[Z], W=[vb_])
                        for (a, b) in ((0, 512), (512, 768)):
                            k.mm(X[:, a:b], Msuf[:, :], lf[:, a:b], True, True, R=[Msuf, lf], W=[X])
                        k.act(e4[:, :], X[:, 0:768], AF.Exp, R=[X], W=[e4])
                        k.tt('dve', kd[:, :], kb_[:, :], e4[:, :], ALU.mult, R=[kb_, e4], W=[kd])
                        ncol = 130 if near else 2
                        for h in range(6):
                            off = (h // 3) * 512 + (h % 3) * 130
                            rq = Mq[:, 0:130] if near else Mq[:, 64:66]
                            k.mm(Y[:, off:off + ncol], lf[:, h * 128:(h + 1) * 128], rq, True, True, R=[lf, Mq], W=[Y])
                        Yv = Y[:, :].rearrange("p (b x) -> p b x", b=2)[:, :, 0:390].rearrange("p b (h c) -> p b h c", h=3)

                        def hv(t):
                            return t[:, :, :].rearrange("p (b h) c -> p b h c", b=2)
                        if near:
                            k.act(hv(e1), Yv[:, :, :, 0:64], AF.Exp, R=[Y], W=[e1])
                            k.act(hv(e2), Yv[:, :, :, 0:64], AF.Exp, R=[Y], W=[e2], scale=-1.0)
                            k.act(hv(e3), Yv[:, :, :, 66:130], AF.Exp, R=[Y], W=[e3])
                            k.act(hv(dc), Yv[:, :, :, 64:66], AF.Exp, R=[Y], W=[dc])
                        else:
                            k.act(hv(dc), Yv[:, :, :, 0:2], AF.Exp, R=[Y], W=[dc])
                        if near:
                            qi_ = qi[ti % 2]; qn_ = qin[ti % 2]; ki_ = kiT[ti % 2]; sc_m = scm[ti % 2]
                            for h in range(6):
                                for kt in range(8):
                                    k.mm(Wp[:, h * 64:(h + 1) * 64], Wq[:, kt, h * 128:(h + 1) * 128], hT[:, kt, 0:64], kt == 0, kt == 7,
                                         R=[Wq, hT], W=[Wp])
                            Wpv = Wp[:, 0:384].rearrange("p (h c) -> p h c", h=6)
                            k.tt('dve', qi_[:, :, :], Wpv, e1[:, :, :], ALU.mult, R=[Wp, e1], W=[qi_])
                            k.tt('dve', qn_[:, :, :], Wpv, e3[:, :, :], ALU.mult, R=[Wp, e3], W=[qn_])
                            for h in range(6):
                                k.mm(Vp[:, h * 64:(h + 1) * 64], kb_[0:64, h * 128:(h + 1) * 128], identb[0:64, 0:64], True, True,
                                     R=[kb_, identb], W=[Vp])
                            k.tt('dve', ki_[:, :, :], Vp[:, 0:384].rearrange("p (h c) -> p h c", h=6), e2[:, :, :], ALU.mult,
                                 R=[Vp, e2], W=[ki_])
                            for h in range(6):
                                k.mm(Wp[0:64, h * 64:(h + 1) * 64], ki_[:, h, :], qi_[:, h, :], True, True, R=[ki_, qi_], W=[Wp])
                            k.tt('dve', sc_m[:, :, :], Wp[0:64, 0:384].rearrange("p (h c) -> p h c", h=6),
                                 bview(maskb[:, None, :], [64, 6, 64]), ALU.mult, R=[Wp, maskb], W=[sc_m])
                        for c in corder:
                            if c == 0 and near:
                                for h in range(6):
                                    hs = slice(h * 128, (h + 1) * 128)
                                    k.mm(Z[0:64, hs], sc_m[:, h, :], vb_[0:64, hs], True, False, R=[sc_m, vb_], W=[Z])
                                    k.mm(Z[0:64, hs], qn_[:, h, :], Sb[:, h, :], False, True, R=[qn_, Sb], W=[Z])
                                w = idx
                                if ch == 0:
                                    ob = osb[ti % 2]
                                    k.act(ob[:, :], Z[0:64, 0:768], AF.Copy, R=[Z], W=[ob])
                                    k.dma(oF_d[w * 64:(w + 1) * 64, :], ob[:, :], R=[ob], W=[oF_dT[w]], q='act')
                                else:
                                    ob = osb[ti % 2]; of_ = oFl[ti % 2]; sq = ssq[ti % 2]
                                    k.dma(of_[:, :], oF_d[w * 64:(w + 1) * 64, :], R=[oF_dT[w]], W=[of_])
                                    k.tt('dve', ob[:, :], Z[0:64, 0:768], of_[:, :], ALU.add, R=[Z, of_], W=[ob])
                                    for h in range(6):
                                        k.act(ojunk[:, :], ob[:, h * 128:(h + 1) * 128], AF.Square, R=[ob], W=[ojunk, sq],
                                              accum_out=sq[:, h:h + 1])
                                    k.ts('dve', sq[:, :], sq[:, :], 1.0 / 128, EPS, ALU.mult, ALU.add, R=[sq], W=[sq])
                                    k.act(sq[:, :], sq[:, :], AF.Sqrt, R=[sq], W=[sq])
                                    k.op('dve', lambda e, sq=sq: e.reciprocal(out=sq[:, :], in_=sq[:, :]), reads=[sq], writes=[sq])
                                    obv = ob[:, :].rearrange("p (h c) -> p h c", h=6)
                                    k.tt('dve', obv, obv, bview(sq[:, :, None], [64, 6, 128]), ALU.mult, R=[ob, sq], W=[ob])
                                    k.tt('pool', ob[:, :], ob[:, :], g6[0:64, :], ALU.mult, R=[ob, g6], W=[ob])
                                    k.dma(on_d.rearrange("(r w) c -> r w c", w=64)[:, w, :], ob[:, :], R=[ob], W=[on_dT[w]], q='act')
                            cs = slice(c * 64, (c + 1) * 64)
                            for h in range(6):
                                hs = slice(h * 128, (h + 1) * 128)
                                k.mm(X[:, hs], kd[cs, hs], vb_[cs, hs], True, True, R=[kd, vb_], W=[X])
                            for h in range(6):
                                hs = slice(h * 128, (h + 1) * 128)
                                k.stt('dve', S32_t[:, h, :], S32_t[:, h, :], dc[:, h, c:c + 1], X[:, hs], ALU.mult, ALU.add,
                                      R=[S32[h], dc, X], W=[S32[h]])
                            k.cp('pool', Sb[:, :, :], S32_t[:, :, :], R=S32, W=[Sb])

        with Phase(k):
            selA = k.sb("selA", [128, 32, 64]); gateA = k.sb("gateA", [128, 32, 64])
            with Phase(k):
                identb = k.sb("identb", [128, 128], BF16); k.dma(identb[:, :], ident_d, W=[identb], q='pool')
                identf = load_f32(k, "identf", ident_d, [128, 128])
                Wgo = load_bf16(k, "Wgo", w_go, 8, 768); Wga = load_bf16(k, "Wga", w_ga, 8, 1024); Wgb = load_bf16(k, "Wgb", w_gb, 8, 1024)
                Wgl = load_bf16(k, "Wgl", wglu, 2, 256); Pa = load_bf16(k, "Pa", p_a, 2, 1024); Pb = load_bf16(k, "Pb", p_b, 6, 1024)
                Wo = load_bf16(k, "Wo", w_out, 8, 1024); Wr = load_bf16(k, "Wr", w_r, 8, 64)
                brt = load_f32(k, "brt", b_r, [128, 64])
                rows = {}
                for nm_, idx in (("shift1", 0), ("scale1", 1), ("g1", 2), ("shift2", 3), ("scale2", 4)):
                    rows[nm_] = k.sb(nm_, [128, 1024]); k.dma(rows[nm_][:, :], modr[idx], R=[modrT[idx]], W=[rows[nm_]])
                X = k.ps("X", [128, 1024]); Y = k.ps("Y", [128, 1024]); Z = k.ps("Z", [128, 1024]); Wp = k.ps("Wp", [128, 1024])
                prod = Producer(k, identb, X, nbuf=2)
                hT = k.sb("hT", [128, 8, 128], BF16)
                yT = k.sb("yT", [128, 2, 128], BF16); sgl = k.sb("sgl", [128, 2, 128]); yaT = k.sb("yaT", [128, 2, 128], BF16)
                ont = k.sb("ont", [128, 768]); sgo = k.sb("sgo", [128, 768]); ybb = k.sb("ybb", [128, 768], BF16)
                ybT = k.sb("ybT", [128, 6, 128], BF16)
                sga = k.sb("sga", [128, 1024]); t1 = k.sb("t1", [128, 1024]); mb = k.sb("mb", [128, 1024], BF16)
                mT = k.sb("mT", [128, 8, 128], BF16)
                x2 = k.sb("x2", [128, 1024]); h2b = k.sb("h2b", [128, 1024], BF16); h2T = k.sb("h2T", [128, 8, 128], BF16)
                ss2 = k.sb("ss2", [128, 1])
                rs = k.sb("rs", [128, 64]); bz = k.sb("bz", [128, 64]); eq = k.sb("eq", [128, 64]); m1 = k.sb("m1g", [128, 8])
                m2 = k.sb("m2g", [128, 8]); top8 = k.sb("top8", [128, 8]); gok = k.sb("gok", [128, 8]); msk = k.sb("msk", [128, 64])
                tq = k.sb("tq", [128, 64]); den = k.sb("den", [128, 1])
                for t in range(32):
                    r0 = t * 128
                    xt = prod.make(xf[r0:r0 + 128, :], rows["scale1"], rows["shift1"], hT)
                    k.dma(yT[:, :, :], yT_d[:, :, r0:r0 + 128], R=[yT_dT[t]], W=[yT])
                    for mh in range(2):
                        for kh in range(2):
                            k.mm(Wp[:, mh * 128:(mh + 1) * 128], Wgl[:, kh, mh * 128:(mh + 1) * 128], yT[:, kh, :], kh == 0, kh == 1,
                                 R=[Wgl, yT], W=[Wp])
                    k.act(sgl[:, :, :], Wp[:, 0:256].rearrange("p (a b) -> p a b", a=2), AF.Sigmoid, R=[Wp], W=[sgl])
                    k.tt('dve', yaT[:, :, :], yT[:, :, :], sgl[:, :, :], ALU.mult, R=[yT, sgl], W=[yaT])
                    for (a, b) in ((0, 512), (512, 768)):
                        for kt in range(8):
                            k.mm(Y[:, a:b], hT[:, kt, :], Wgo[:, kt, a:b], kt == 0, kt == 7, R=[hT, Wgo], W=[Y])
                    k.act(sgo[:, :], Y[:, 0:768], AF.Silu, R=[Y], W=[sgo])
                    k.dma(ont[:, :], on_d[r0:r0 + 128, :], R=on_dT, W=[ont])
                    k.tt('dve', ybb[:, :], ont[:, :], sgo[:, :], ALU.mult, R=[ont, sgo], W=[ybb])
                    for j in range(6):
                        k.mm(X[:, j * 128:(j + 1) * 128], ybb[:, j * 128:(j + 1) * 128], identb[:, :], True, True, R=[ybb, identb], W=[X])
                    k.act(ybT[:, :, :], X[:, 0:768].rearrange("p (a b) -> p a b", a=6), AF.Copy, R=[X], W=[ybT])
                    for hf in range(2):
                        cs = slice(hf * 512, (hf + 1) * 512)
                        for kt in range(8):
                            k.mm(Y[:, cs], hT[:, kt, :], Wga[:, kt, cs], kt == 0, kt == 7, R=[hT, Wga], W=[Y])
                    k.act(sga[:, :], Y[:, :], AF.Sigmoid, R=[Y], W=[sga])
                    for hf in range(2):
                        cs = slice(hf * 512, (hf + 1) * 512)
                        for kh in range(2):
                            k.mm(Z[:, cs], yaT[:, kh, :], Pa[:, kh, cs], kh == 0, kh == 1, R=[yaT, Pa], W=[Z])
                    k.tt('dve', t1[:, :], sga[:, :], Z[:, :], ALU.mult, R=[sga, Z], W=[t1])
                    for hf in range(2):
                        cs = slice(hf * 512, (hf + 1) * 512)
                        for kt in range(8):
                            k.mm(Y[:, cs], hT[:, kt, :], Wgb[:, kt, cs], kt == 0, kt == 7, R=[hT, Wgb], W=[Y])
                    k.act(sga[:, :], Y[:, :], AF.Sigmoid, R=[Y], W=[sga])
                    for hf in range(2):
                        cs = slice(hf * 512, (hf + 1) * 512)
                        for j in range(6):
                            k.mm(Z[:, cs], ybT[:, j, :], Pb[:, j, cs], j == 0, j == 5, R=[ybT, Pb], W=[Z])
                    k.tt('dve', sga[:, :], sga[:, :], Z[:, :], ALU.mult, R=[sga, Z], W=[sga])
                    k.tt('pool', mb[:, :], t1[:, :], sga[:, :], ALU.add, R=[t1, sga], W=[mb])
                    prod.transpose(mb, mT)
                    for hf in range(2):
                        cs = slice(hf * 512, (hf + 1) * 512)
                        for kt in range(8):
                            k.mm(Wp[:, cs], mT[:, kt, :], Wo[:, kt, cs], kt == 0, kt == 7, R=[mT, Wo], W=[Wp])
                    k.dma(x2[:, :], xf[r0:r0 + 128, :], R=[xt], W=[x2])
                    k.tt('dve', t1[:, :], Wp[:, :], rows["g1"][:, :], ALU.mult, R=[Wp, rows["g1"]], W=[t1])
                    k.tt('dve', x2[:, :], x2[:, :], t1[:, :], ALU.add, R=[x2, t1], W=[x2])
                    k.dma(x2_d[r0:r0 + 128, :], x2[:, :], R=[x2], W=[x2_dT[t]], q='act')
                    prod.rstd(x2, ss2)
                    k.stt('dve', t1[:, :], x2[:, :], ss2[:, 0:1], rows["scale2"][:, :], ALU.mult, ALU.mult, R=[x2, ss2, rows["scale2"]], W=[t1])
                    k.tt('pool', h2b[:, :], t1[:, :], rows["shift2"][:, :], ALU.add, R=[t1, rows["shift2"]], W=[h2b])
                    prod.transpose(h2b, h2T)
                    k.dma(h2T_d[:, :, r0:r0 + 128], h2T[:, :, :], R=[h2T], W=[h2T_dT[t]], q='act')
                    for kt in range(8):
                        k.mm(Wp[:, 0:64], h2T[:, kt, :], Wr[:, kt, :], kt == 0, kt == 7, R=[h2T, Wr], W=[Wp])
                    k.act(rs[:, :], Wp[:, 0:64], AF.Sigmoid, R=[Wp], W=[rs])
                    k.tt('dve', bz[:, :], rs[:, :], brt[:, :], ALU.add, R=[rs, brt], W=[bz])
                    bz3 = bz[:, :].rearrange("p (g e) -> p g e", g=8)
                    k.op('dve', lambda e: e.tensor_reduce(out=m1[:, :], in_=bz3, axis=AX.X, op=ALU.max), reads=[bz], writes=[m1])
                    k.tt('dve', eq[:, :].rearrange("p (g e) -> p g e", g=8), bz3, bview(m1[:, :, None], [128, 8, 8]), ALU.is_equal,
                         R=[bz, m1], W=[eq])
                    k.stt('dve', eq[:, :], eq[:, :], -1e9, bz[:, :], ALU.mult, ALU.add, R=[eq, bz], W=[eq])
                    k.op('dve', lambda e: e.tensor_reduce(out=m2[:, :], in_=eq[:, :].rearrange("p (g e) -> p g e", g=8), axis=AX.X, op=ALU.max),
                         reads=[eq], writes=[m2])
                    k.tt('dve', m1[:, :], m1[:, :], m2[:, :], ALU.add, R=[m1, m2], W=[m1])
                    k.op('dve', lambda e: e.max(out=top8[:, :], in_=m1[:, :]), reads=[m1], writes=[top8])
                    k.ts('dve', gok[:, :], m1[:, :], top8[:, 3:4], None, ALU.is_ge, None, R=[m1, top8], W=[gok])
                    gok3 = bview(gok[:, :, None], [128, 8, 8])
                    k.tt('dve', msk[:, :].rearrange("p (g e) -> p g e", g=8), bz3, gok3, ALU.mult, R=[bz, gok], W=[msk])
                    k.ts('dve', gok[:, :], gok[:, :], 1e9, -1e9, ALU.mult, ALU.add, R=[gok], W=[gok])
                    k.tt('dve', msk[:, :].rearrange("p (g e) -> p g e", g=8), msk[:, :].rearrange("p (g e) -> p g e", g=8), gok3, ALU.add,
                         R=[msk, gok], W=[msk])
                    k.op('dve', lambda e: e.max(out=top8[:, :], in_=msk[:, :]), reads=[msk], writes=[top8])
                    k.ts('dve', selA[:, t, :], msk[:, :], top8[:, 7:8], None, ALU.is_ge, None, R=[msk, top8], W=[selA])
                    k.tt('dve', tq[:, :], selA[:, t, :], rs[:, :], ALU.mult, R=[selA, rs], W=[tq])
                    k.op('dve', lambda e: e.reduce_sum(out=den[:, :], in_=tq[:, :], axis=AX.X), reads=[tq], writes=[den])
                    k.op('dve', lambda e: e.reciprocal(out=den[:, :], in_=den[:, :]), reads=[den], writes=[den])
                    k.ts('dve', gateA[:, t, :], tq[:, :], den[:, 0:1], 2.5, ALU.mult, ALU.mult, R=[tq, den], W=[gateA])
                    k.dma(h2tok_d[r0:r0 + 128, :], h2b[:, :], R=[h2b], W=[h2tok_dT[t]], q='act')
                zero_some(1000)

            with Phase(k):
                triu = load_f32(k, "triu", triu_d, [128, 128]); e127 = load_f32(k, "e127", e127_d, [128, 128])
                CUM = k.ps("CUM", [128, 2048]); TOT = k.ps("TOT", [128, 2048])
                cumA = k.sb("cumA", [128, 32, 64]); totA = k.sb("totA", [128, 32, 64]); base = k.sb("base", [128, 32, 64])
                for i in range(32):
                    k.mm(CUM[:, i * 64:(i + 1) * 64], triu[:, :], selA[:, i, :], True, True, R=[triu, selA], W=[CUM])
                k.cp('dve', cumA[:, :, :], CUM[:, :].rearrange("p (a b) -> p a b", a=32), R=[CUM], W=[cumA])
                cflat = cumA[:, :, :].rearrange("p a b -> p (a b)")
                for c in range(4):
                    k.mm(TOT[:, c * 512:(c + 1) * 512], e127[:, :], cflat[:, c * 512:(c + 1) * 512], True, True, R=[e127, cumA], W=[TOT])
                k.act(totA[:, :, :], TOT[:, :].rearrange("p (a b) -> p a b", a=32), AF.Copy, R=[TOT], W=[totA])
                k.memset('dve', base, base[:, 0, :], 0.0)
                for i in range(1, 32):
                    k.tt('dve', base[:, i, :], base[:, i - 1, :], totA[:, i - 1, :], ALU.add, R=[base, totA], W=[base])
                cnt = k.sb("cnt", [128, 64]); nbf = k.sb("nbf", [128, 64]); nbi = k.sb("nbi", [128, 64], I32)
                ones64 = k.sb("ones64", [128, 64]); pend = k.sb("pend", [128, 64]); pstart = k.sb("pstart", [128, 64])
                k.memset('pool', ones64, ones64[:, :], 1.0)
                k.tt('dve', cnt[:, :], base[:, 31, :], totA[:, 31, :], ALU.add, R=[base, totA], W=[cnt])
                k.ts('dve', nbf[:, :], cnt[:, :], 1.0 / 256, 255.0 / 256 - 0.5 + 1.0 / 512, ALU.mult, ALU.add, R=[cnt], W=[nbf])
                k.cp('dve', nbi[:, :], nbf[:, :], R=[nbf], W=[nbi])
                k.cp('dve', nbf[:, :], nbi[:, :], R=[nbi], W=[nbf])
                k.ts('dve', nbf[:, :], nbf[:, :], 256.0, None, ALU.mult, None, R=[nbf], W=[nbf])
                k.op('dve', lambda e: e.tensor_tensor_scan(out=pend[:, :], data0=ones64[:, :], data1=nbf[:, :], initial=0.0,
                                                           op0=ALU.mult, op1=ALU.add), reads=[ones64, nbf], writes=[pend])
                k.tt('dve', pstart[:, :], pend[:, :], nbf[:, :], ALU.subtract, R=[pend, nbf], W=[pstart])
                k.tt('dve', cumA[:, :, :], cumA[:, :, :], base[:, :, :], ALU.add, R=[cumA, base], W=[cumA])
                k.tt('dve', cumA[:, :, :], cumA[:, :, :], bview(pstart[:, None, :], [128, 32, 64]), ALU.add, R=[cumA, pstart], W=[cumA])
                k.tt('dve', cumA[:, :, :], cumA[:, :, :], selA[:, :, :], ALU.mult, R=[cumA, selA], W=[cumA])
                cmp = k.sb("cmp", [128, 192, 64]); bef = k.sb("bef", [128, 192]); pcol = load_f32(k, "pcol", pcol_d, [128, 1])
                thrb = k.sb("thrb", [128, 192]); k.dma(thrb[:, :], thr_d.broadcast_to([128, 192]), W=[thrb])
                k.tt('dve', cmp[:, :, :], bview(pend[:, None, :], [128, 192, 64]), bview(thrb[:, :, None], [128, 192, 64]), ALU.is_le,
                     R=[pend, thrb], W=[cmp])
                k.op('dve', lambda e: e.reduce_sum(out=bef[:, :], in_=cmp[:, :, :], axis=AX.X), reads=[cmp], writes=[bef])
                k.ts('dve', bef[:, :], bef[:, :], 63.0, 128.0, ALU.min, ALU.mult, R=[bef], W=[bef])
                k.ts('dve', bef[:, :], bef[:, :], pcol[:, 0:1], None, ALU.add, None, R=[bef, pcol], W=[bef])
                k.cp('dve', be_i[:, :], bef[:, :], R=[bef], W=[be_i])
                t8 = k.ring("t8", 2, [128, 8]); eqg = k.ring("eqg", 2, [128, 64]); hk = k.ring("hk", 3, [128, 1024], BF16)
                for i in range(32):
                    t8_ = t8[i % 2]; hk_ = hk[i % 3]
                    k.dma(hk_[:, :], h2tok_d[i * 128:(i + 1) * 128, :], R=[h2tok_dT[i]], W=[hk_])
                    k.op('dve', lambda e, t8_=t8_, i=i: e.max(out=t8_[:, :], in_=cumA[:, i, :]), reads=[cumA], writes=[t8_])
                    k.ts('dve', dest8A[:, i, :], t8_[:, :], -1.0, None, ALU.add, None, R=[t8_], W=[dest8A])
                    for kk in range(8):
                        eq_ = eqg[kk % 2]
                        k.stt('dve', eq_[:, :], cumA[:, i, :], t8_[:, kk:kk + 1], gateA[:, i, :], ALU.is_equal, ALU.mult,
                              R=[cumA, t8_, gateA], W=[eq_])
                        k.op('dve', lambda e, eq_=eq_, i=i, kk=kk: e.reduce_sum(out=w8A[:, i, kk:kk + 1], in_=eq_[:, :], axis=AX.X),
                             reads=[eq_], writes=[w8A])
                    for kk in range(8):
                        k.op('pool', lambda e, i=i, kk=kk, hk_=hk_: e.indirect_dma_start(
                            out=xg_d[:, :], out_offset=bass.IndirectOffsetOnAxis(ap=dest8A[:, i, kk:kk + 1], axis=0),
                            in_=hk_[:, :], in_offset=None, bounds_check=bcreg, oob_is_err=False),
                            reads=[hk_, dest8A], writes=[xgT], dma=True)

        with Phase(k):
            identb = k.sb("identb", [128, 128], BF16); k.dma(identb[:, :], ident_d, W=[identb], q='pool')
            W1r = k.ring("W1", 4, [128, 8, 256], BF16); W3r = k.ring("W3", 4, [128, 8, 256], BF16); W2r = k.ring("W2", 4, [128, 2, 1024], BF16)
            xblk = k.ring("xblk", 3, [128, 2, 1024], BF16); XTr = k.ring("XT", 2, [128, 8, 256], BF16)
            sr = k.ring("sr", 2, [128, 512]); hbr = k.ring("hbr", 2, [128, 2, 256], BF16); yblk = k.ring("yblk", 2, [128, 2, 1024], BF16)
            XTp = [k.ps("XTp", [128, 1024]) for _ in range(1)]
            H13 = k.ps("H13", [128, 1024]); OP = [k.ps("OP", [128, 1024]) for _ in range(2)]
            for j in range(192):
                W1 = W1r[j % 4]; W3 = W3r[j % 4]; W2 = W2r[j % 4]; xb_ = xblk[j % 3]; XT = XTr[j % 2]
                s_ = sr[j % 2]; hb_ = hbr[j % 2]; yb_ = yblk[j % 2]
                for (Wt, wsrc, wT_) in (((W1, wb_d[0], wbT[0]), (W3, wb_d[1], wbT[1]), (W2, wb_d[2], wbT[2])) if USE_CONV else ((W1, w1, wbT[0]), (W3, w3, wbT[1]), (W2, w2, wbT[2]))):
                    k.op('pool', lambda e, Wt=Wt, wsrc=wsrc, j=j: e.indirect_dma_start(
                        out=Wt[:, :, :].rearrange("p a b -> p (a b)"), out_offset=None, in_=wsrc[:, :],
                        in_offset=bass.IndirectOffsetOnAxis(ap=be_i[:, j:j + 1], axis=0),
                        bounds_check=bcreg2, oob_is_err=False), reads=[be_i, wT_], writes=[Wt], dma=True)
                k.dma(xb_[:, :, :], xg_d[j * 256:(j + 1) * 256, :].rearrange("(a p) c -> p a c", p=128), R=[xgT], W=[xb_])
                for a in range(2):
                    P_ = XTp[0]
                    for kt in range(8):
                        k.mm(P_[:, kt * 128:(kt + 1) * 128], xb_[:, a, kt * 128:(kt + 1) * 128], identb[:, :], True, True, R=[xb_, identb], W=[P_])
                    if a == 0:
                        k.act(XT[:, :, 0:128], P_[:, :].rearrange("p (a b) -> p a b", a=8), AF.Copy, R=[P_], W=[XT])
                    else:
                        k.cp('dve', XT[:, :, 128:256], P_[:, :].rearrange("p (a b) -> p a b", a=8), R=[P_], W=[XT])
                for (Wx, off) in ((W1, 0), (W3, 512)):
                    for mt in range(2):
                        for kt in range(8):
                            k.mm(H13[:, off + mt * 256: off + (mt + 1) * 256], Wx[:, kt, mt * 128:(mt + 1) * 128], XT[:, kt, :], kt == 0, kt == 7,
                                 R=[Wx, XT], W=[H13])
                k.act(s_[:, :], H13[:, 0:512], AF.Silu, R=[H13], W=[s_])
                k.tt('dve', hb_[:, :, :], s_[:, :].rearrange("p (a b) -> p a b", a=2), H13[:, 512:1024].rearrange("p (a b) -> p a b", a=2),
                     ALU.mult, R=[s_, H13], W=[hb_])
                for a in range(2):
                    O_ = OP[a]
                    for hf in range(2):
                        cs = slice(hf * 512, (hf + 1) * 512)
                        for ft in range(2):
                            k.mm(O_[:, cs], hb_[:, ft, a * 128:(a + 1) * 128], W2[:, ft, cs], ft == 0, ft == 1, R=[hb_, W2], W=[O_])
                    if a == 0:
                        k.act(yb_[:, 0, :], O_[:, :], AF.Copy, R=[O_], W=[yb_])
                    else:
                        k.cp('dve', yb_[:, 1, :], O_[:, :], R=[O_], W=[yb_])
                k.dma(yg_d[j * 256:(j + 1) * 256, :].rearrange("(a p) c -> p a c", p=128), yb_[:, :, :], R=[yb_], W=[ygT], q='act')

        with Phase(k):
            g2r = k.sb("g2r", [128, 1024]); k.dma(g2r[:, :], modr[5], R=[modrT[5]], W=[g2r])
            fg = load_f32(k, "fng", fng, [128, 1024])
            W1s = k.sb("W1s", [128, 8, 256], BF16); W3s = k.sb("W3s", [128, 8, 256], BF16); W2s = k.sb("W2s", [128, 2, 1024], BF16)
            for (Wt, wsrc) in ((W1s, w1), (W3s, w3), (W2s, w2)):
                k.dma(Wt[:, :, :].rearrange("p a b -> p (a b)"), wsrc[64 * 128:65 * 128, :], W=[Wt], q='pool')
            H1 = k.ps("H1", [128, 1024]); H3 = k.ps("H3", [128, 1024]); OPs = [k.ps("OPs", [128, 1024]) for _ in range(2)]
            h2s = k.ring("h2s", 2, [128, 8, 512], BF16)
            sl = k.ring("sl", 2, [128, 1024]); hg = k.ring("hg", 2, [128, 2, 512], BF16)
            acc = k.ring("acc", 2, [128, 1024]); Yk = k.ring("Yk", 6, [128, 1024], BF16)
            x3 = k.ring("x3", 2, [128, 1024]); junk = k.sb("junk4", [128, 1024], BF16); ssf = k.ring("ssf", 2, [128, 1])
            yi = 0
            for g4 in range(8):
                h2_ = h2s[g4 % 2]; s_ = sl[g4 % 2]; hg_ = hg[g4 % 2]
                k.dma(h2_[:, :, :], h2T_d[:, :, g4 * 512:(g4 + 1) * 512], R=h2T_dT, W=[h2_])
                for (Wx, Hx) in ((W1s, H1), (W3s, H3)):
                    for mt in range(2):
                        for kt in range(8):
                            k.mm(Hx[:, mt * 512:(mt + 1) * 512], Wx[:, kt, mt * 128:(mt + 1) * 128], h2_[:, kt, :], kt == 0, kt == 7,
                                 R=[Wx, h2_], W=[Hx])
                k.act(s_[:, :], H1[:, :], AF.Silu, R=[H1], W=[s_])
                k.tt('dve', hg_[:, :, :], s_[:, :].rearrange("p (a b) -> p a b", a=2), H3[:, :].rearrange("p (a b) -> p a b", a=2), ALU.mult,
                     R=[s_, H3], W=[hg_])
                for tt_ in range(4):
                    t = g4 * 4 + tt_
                    r0 = t * 128
                    O_ = OPs[t % 2]; ac = acc[t % 2]; x_ = x3[t % 2]; s_f = ssf[t % 2]
                    for hf in range(2):
                        cs = slice(hf * 512, (hf + 1) * 512)
                        for ft in range(2):
                            k.mm(O_[:, cs], hg_[:, ft, tt_ * 128:(tt_ + 1) * 128], W2s[:, ft, cs], ft == 0, ft == 1, R=[hg_, W2s], W=[O_])
                    k.act(ac[:, :], O_[:, :], AF.Copy, R=[O_], W=[ac])
                    for kk in range(8):
                        y_ = Yk[yi % 6]; yi += 1
                        k.op('pool', lambda e, y_=y_, t=t, kk=kk: e.indirect_dma_start(
                            out=y_[:, :], out_offset=None, in_=yg_d[:, :],
                            in_offset=bass.IndirectOffsetOnAxis(ap=dest8A[:, t, kk:kk + 1], axis=0),
                            bounds_check=bcreg, oob_is_err=False), reads=[ygT, dest8A], writes=[y_], dma=True)
                        k.stt('dve', ac[:, :], y_[:, :], w8A[:, t, kk:kk + 1], ac[:, :], ALU.mult, ALU.add, R=[y_, w8A, ac], W=[ac])
                    k.dma(x_[:, :], x2_d[r0:r0 + 128, :], R=[x2_dT[t]], W=[x_])
                    k.tt('dve', ac[:, :], ac[:, :], g2r[:, :], ALU.mult, R=[ac, g2r], W=[ac])
                    k.tt('pool', x_[:, :], x_[:, :], ac[:, :], ALU.add, R=[x_, ac], W=[x_])
                    k.act(junk[:, :], x_[:, :], AF.Square, R=[x_], W=[junk, s_f], accum_out=s_f[:, 0:1])
                    k.ts('dve', s_f[:, 0:1], s_f[:, 0:1], 1.0 / 1024, EPS, ALU.mult, ALU.add, R=[s_f], W=[s_f])
                    k.act(s_f[:, 0:1], s_f[:, 0:1], AF.Sqrt, R=[s_f], W=[s_f])
                    k.op('dve', lambda e, s_f=s_f: e.reciprocal(out=s_f[:, 0:1], in_=s_f[:, 0:1]), reads=[s_f], writes=[s_f])
                    k.stt('dve', x_[:, :], x_[:, :], s_f[:, 0:1], fg[:, :], ALU.mult, ALU.mult, R=[x_, s_f, fg], W=[x_])
                    k.dma(out[r0:r0 + 128, :], x_[:, :], R=[x_], W=[], q='act')
        k.finish()
        print("kernel build: ninst=%d nsem=%d" % (k.ninst, k.nsem))
    return nc


def _hg_mats():
    M = np.zeros((2, 3, 128, 128), np.float32)
    s = np.arange(128)[:, None]; t = np.arange(128)[None, :]
    same = (s // 64) == (t // 64)
    for d in range(2):
        if d == 0:
            inc = same & (s <= t); suf = same & (s > t); ref = (t // 64) * 64 + 32
        else:
            inc = same & (s >= t); suf = same & (s < t); ref = (t // 64) * 64 + 31
        inc = inc.astype(np.float32)
        M[d, 0] = inc
        M[d, 1] = inc - inc[np.arange(128)[:, None], ref]
        M[d, 2] = suf.astype(np.float32)
    return M


def _prep_core(inp, b, kf):
    f = np.float32
    rv = (lambda a: a[::-1]) if kf else (lambda a: a)
    d = {}
    d["xf"] = np.ascontiguousarray(rv(inp["x"][b]), f)
    d["ctxf"] = np.ascontiguousarray(rv(inp["ctx"][b]), f)
    rep = lambda v: np.ascontiguousarray(np.broadcast_to(v.reshape(8, 128).T[:, :, None], (128, 8, 128)), f)
    d["crep"] = rep(inp["c"][b]); d["ccrep"] = rep(inp["c_ctx"])
    d["w_ada"] = np.ascontiguousarray(inp["w_ada"][0], f); d["b_ada"] = np.ascontiguousarray(inp["b_ada"][0][None, :], f)
    rows = lambda v, n=128: np.ascontiguousarray(np.broadcast_to(v[None, :], (n, v.shape[0])), f)
    d["n1g"] = rows(inp["norm1_g"][0]); d["n2g"] = rows(inp["norm2_g"][0]); d["fng"] = rows(inp["final_norm_g"])
    w = inp["w_in"][0]
    sl = {"u": (0, 256), "q": (256, 1024), "ff": (1024, 1792), "fb": (1792, 2560), "i": (2560, 3328), "go": (3328, 4096),
          "ga": (4096, 5120), "gb": (5120, 6144)}
    cut = lambda n: np.ascontiguousarray(w[:, sl[n][0]:sl[n][1]], f)
    d["w_u"] = cut("u"); d["w_q"] = cut("q"); d["w_i"] = cut("i"); d["w_go"] = cut("go"); d["w_ga"] = cut("ga"); d["w_gb"] = cut("gb")
    d["w_fF"] = cut("fb" if kf else "ff"); d["w_fB"] = cut("ff" if kf else "fb")
    dirs = [1, 0] if kf else [0, 1]
    lam_re = inp["s5_lam_re"][0][dirs]; lam_im = inp["s5_lam_im"][0][dirs]; logdt = inp["s5_log_dt"][0][dirs]
    bre = inp["s5_b_re"][0][dirs]; bim = inp["s5_b_im"][0][dirs]
    A = lambda v: np.ascontiguousarray(np.concatenate([v.reshape(32, 64).T] * 2, axis=0), f)
    d["lamA_re"] = A(lam_re); d["lamA_im"] = A(lam_im)
    d["logdtA"] = np.ascontiguousarray(np.broadcast_to(logdt.reshape(1, 32), (128, 32)), f)
    Bb = lambda v: np.ascontiguousarray(np.broadcast_to(v.reshape(1, 32, 64), (128, 32, 64)), f)
    d["lamB_re"] = Bb(lam_re); d["lamB_im"] = Bb(lam_im)
    d["logdtB"] = np.ascontiguousarray(np.broadcast_to(logdt.reshape(1, 32, 1), (128, 32, 64)), f)
    brp = np.zeros((128, 32, 64), f); bip = np.zeros((128, 32, 64), f)
    for fd in range(2):
        for g in range(16):
            r0 = (g % 8) * 16
            brp[r0:r0 + 16, fd * 16 + g, :] = bre[fd, g].T
            bip[r0:r0 + 16, fd * 16 + g, :] = bim[fd, g].T
    d["Bre_pad"] = brp; d["Bim_pad"] = bip
    cre = inp["s5_c_re"][0]; cim = inp["s5_c_im"][0]
    ca = np.zeros((128, 16, 128), f); cb = np.zeros((128, 16, 128), f)
    for g in range(16):
        c0 = (g % 8) * 16
        ca[0:64, g, c0:c0 + 16] = cre[g].T; ca[64:128, g, c0:c0 + 16] = cim[g].T
        cb[0:64, g, c0:c0 + 16] = cim[g].T; cb[64:128, g, c0:c0 + 16] = cre[g].T
    d["CA"] = ca; d["CB"] = cb
    d["dcol"] = np.ascontiguousarray(inp["s5_d"][0].reshape(2, 128).T, f)
    sg = np.ones((128, 1), f); sg[64:] = -1; d["sgn"] = sg
    sw = np.zeros((128, 128), f); sw[np.arange(128), (np.arange(128) + 64) % 128] = 1; d["swapm"] = sw
    d["iota256"] = np.ascontiguousarray(np.broadcast_to(np.arange(1, 257, dtype=f)[None, :], (128, 256)), f)
    d["lbl"] = np.ascontiguousarray(np.broadcast_to(inp["hg_lb_logits"][None, :, :], (128, 2, 768)), f)
    d["hgg6"] = rows(np.tile(inp["hg_norm_g"][0], 6))
    d["hgM"] = _hg_mats()
    ci = np.zeros((128, 2), f); ci[:64, 0] = 1; ci[64:, 1] = 1; d["cind"] = ci
    d["wglu"] = np.ascontiguousarray(inp["s5_w_glu"][0], f); d["p_a"] = np.ascontiguousarray(inp["p_a"][0], f)
    d["p_b"] = np.ascontiguousarray(inp["p_b"][0], f); d["w_out"] = np.ascontiguousarray(inp["w_out"][0], f)
    d["w_r"] = np.ascontiguousarray(inp["moe_w_router"][0], f); d["b_r"] = rows(inp["moe_b_router"][0])
    d["ident"] = np.eye(128, dtype=f)
    d["triu"] = np.triu(np.ones((128, 128), f))
    e127 = np.zeros((128, 128), f); e127[127, :] = 1; d["e127"] = e127
    d["thr"] = (np.arange(192, dtype=f) * 256)[None, :]
    d["pcol"] = np.arange(128, dtype=f)[:, None]
    return d


_SHARED = {}


def kernel(**inp):
    inp = {k_: np.asarray(v) for k_, v in inp.items()}
    f = np.float32
    def relay(w, ws):
        a = np.concatenate([w, ws[None]], 0)
        e_, r_, n_ = a.shape
        return np.ascontiguousarray(a.reshape(e_, r_ // 128, 128, n_).transpose(0, 2, 1, 3).reshape(e_ * 128, (r_ // 128) * n_), f)
    w1 = relay(inp["moe_w1"][0], inp["moe_ws1"][0])
    w3 = relay(inp["moe_w3"][0], inp["moe_ws3"][0])
    w2 = relay(inp["moe_w2"][0], inp["moe_ws2"][0])
    in_maps = []
    for core in range(8):
        b, kf = core // 2, core % 2
        d = _prep_core(inp, b, kf)
        d["w1"] = w1; d["w3"] = w3; d["w2"] = w2
        in_maps.append(d)
    nc = build(DEBUG)
    res = run_bass_kernel_spmd(nc, in_maps, core_ids=list(range(8)))
    outp = np.zeros((4, 8192, 1024), f)
    for core in range(8):
        b, kf = core // 2, core % 2
        o = res.results[core]["out"]
        if kf == 0:
            outp[b, :4096] = o
        else:
            outp[b, 4096:] = o[::-1]
    if DEBUG:
        _SHARED["res"] = res
    return outp
```
